# Optimizing a Trainium2 kernel written in Bass

```python
import jax, jax.numpy as jnp
from jax import lax
import numpy as np

D_MODEL = 1024
BATCH = 4
SEQ = 8192
DEPTH = 2

GRID_W = 64
CTX_LEN = 256
ROPE_THETA = 10000.0
LN_EPS = 1e-6
NEG_INF = -1e30
DN_ALPHA = (2 * DEPTH) ** 0.25
DN_BETA = (8 * DEPTH) ** -0.25
N_EVEN = (DEPTH + 1) // 2
N_ODD = DEPTH // 2

LRU_WIDTH = 512
LRU_BLOCKS = 8
LRU_BLOCK_DIM = LRU_WIDTH // LRU_BLOCKS
LRU_CONV_W = 4
LRU_C = 8.0
SWA_HEADS = 8
SWA_KV_HEADS = 2
SWA_HEAD_DIM = 64
WINDOW = 128
Q_BLOCK = 128
EVEN_IN = 2 * LRU_WIDTH + (SWA_HEADS + 2 * SWA_KV_HEADS) * SWA_HEAD_DIM
EVEN_MIX = LRU_WIDTH + SWA_HEADS * SWA_HEAD_DIM
MLA_HEADS = 8
MLA_Q_RANK = 256
MLA_KV_RANK = 128
MLA_NOPE = 64
MLA_ROPE = 32
MLA_V = 64
CONF_CH = 512
CONF_K = 31
ODD_IN = MLA_Q_RANK + MLA_KV_RANK + MLA_ROPE + 2 * CONF_CH
ODD_MIX = MLA_HEADS * MLA_V + CONF_CH
N_GROUPS = 4
EXPERTS_PER_GROUP = 8
N_EXPERTS = N_GROUPS * EXPERTS_PER_GROUP
TOP_K = 2
D_EXPERT = 512
MOE_BLOCK = 128

kernel_name = "hybrid_rglru_swa_mla_conformer_hmoe_dit"


def _layer_norm(x, g, b):
    xf = x.astype(jnp.float32)
    mu = jnp.mean(xf, axis=-1, keepdims=True)
    var = jnp.mean(jnp.square(xf - mu), axis=-1, keepdims=True)
    return ((xf - mu) * lax.rsqrt(var + LN_EPS) * g + b).astype(x.dtype)


def _rms_norm(x, g):
    xf = x.astype(jnp.float32)
    return (xf * lax.rsqrt(jnp.mean(jnp.square(xf), axis=-1, keepdims=True) + LN_EPS) * g).astype(x.dtype)


def _grid_positions(n):
    rows = n // GRID_W
    row = jnp.repeat(jnp.arange(rows, dtype=jnp.int32), GRID_W)
    col = jnp.tile(jnp.arange(GRID_W, dtype=jnp.int32), rows)
    return row, col


def _rope_1d(x, pos):
    half = x.shape[-1] // 2
    inv_freq = ROPE_THETA ** (-jnp.arange(half, dtype=jnp.float32) / half)
    ang = pos.astype(jnp.float32)[:, None] * inv_freq[None, :]
    cos = jnp.cos(ang)[:, None, :]
    sin = jnp.sin(ang)[:, None, :]
    xf = x.astype(jnp.float32)
    x1, x2 = xf[..., :half], xf[..., half:]
    return jnp.concatenate([x1 * cos - x2 * sin, x1 * sin + x2 * cos], axis=-1).astype(x.dtype)


def _rope_2d(x, row, col):
    d = x.shape[-1] // 2
    return jnp.concatenate([_rope_1d(x[..., :d], row), _rope_1d(x[..., d:], col)], axis=-1)


def _depthwise_conv(x, w, b):
    k, ch = w.shape
    y = lax.conv_general_dilated(x, w[:, None, :].astype(x.dtype), (1,), [((k - 1) // 2, k // 2)],
                                 dimension_numbers=('NWC', 'WIO', 'NWC'), feature_group_count=ch)
    return y + b


def _modulation(cond, w, b):
    m = (jax.nn.silu(cond) @ w + b)[..., None, :]
    return jnp.split(m, 6, axis=-1)


def _modulate(x, shift, scale):
    return x * (1.0 + scale) + shift


def _rglru_coeffs(u, wa, ba, wx, bx, lam):
    bsz, n, w = u.shape
    ub = u.reshape(bsz, n, LRU_BLOCKS, LRU_BLOCK_DIM)
    r = jax.nn.sigmoid((jnp.einsum('bnkc,kcd->bnkd', ub, wa).reshape(bsz, n, w) + ba).astype(jnp.float32))
    i = jax.nn.sigmoid((jnp.einsum('bnkc,kcd->bnkd', ub, wx).reshape(bsz, n, w) + bx).astype(jnp.float32))
    log_a = -LRU_C * r * jax.nn.softplus(-lam.astype(jnp.float32))
    a = jnp.exp(log_a)
    b = jnp.sqrt(-jnp.expm1(2.0 * log_a)) * (i * u.astype(jnp.float32))
    return a, b


def _linear_scan(a, b, h0):
    b = b.at[:, 0].add(a[:, 0] * h0)

    def combine(left, right):
        a_l, b_l = left
        a_r, b_r = right
        return a_l * a_r, a_r * b_l + b_r

    return lax.associative_scan(combine, (a, b), axis=1)[1]


def _rglru_bidir(u_ctx, u_lat, wa, ba, wx, bx, lam):
    h_ctx = 0.0
    h_lat = 0.0
    for d in range(2):
        a_c, b_c = _rglru_coeffs(u_ctx, wa[d], ba[d], wx[d], bx[d], lam[d])
        a_l, b_l = _rglru_coeffs(u_lat, wa[d], ba[d], wx[d], bx[d], lam[d])
        if d == 1:
            a_c, b_c, a_l, b_l = [jnp.flip(t, axis=1) for t in (a_c, b_c, a_l, b_l)]
        s_c = _linear_scan(a_c, b_c, jnp.zeros_like(a_c[:, 0]))
        s_l = _linear_scan(a_l, b_l, s_c[:, -1])
        if d == 1:
            s_c, s_l = jnp.flip(s_c, axis=1), jnp.flip(s_l, axis=1)
        h_ctx = h_ctx + s_c
        h_lat = h_lat + s_l
    return h_ctx, h_lat


def _sink_column(sink, kvh, g, shape):
    return jnp.broadcast_to(sink.astype(jnp.float32).reshape(kvh, g)[:, :, None, None], shape)


def _swa_latent(q, k, v, k_ctx, v_ctx, sink):
    bsz, n, h, hd = q.shape
    kvh = k.shape[2]
    g = h // kvh
    nb = n // Q_BLOCK
    qb = q.reshape(bsz, nb, Q_BLOCK, kvh, g, hd)

    def band(t):
        tb = jnp.pad(t.reshape(bsz, nb, Q_BLOCK, kvh, hd), ((0, 0), (1, 1), (0, 0), (0, 0), (0, 0)))
        return jnp.concatenate([tb[:, :-2], tb[:, 1:-1], tb[:, 2:]], axis=2)

    kb, vb = band(k), band(v)
    scale = hd ** -0.5
    s_win = jnp.einsum('bnqkgd,bnmkd->bnkgqm', qb, kb).astype(jnp.float32) * scale
    qpos = jnp.arange(nb)[:, None] * Q_BLOCK + jnp.arange(Q_BLOCK)[None, :]
    kpos = (jnp.arange(nb)[:, None] - 1) * Q_BLOCK + jnp.arange(3 * Q_BLOCK)[None, :]
    valid = (jnp.abs(qpos[:, :, None] - kpos[:, None, :]) <= WINDOW) & (kpos[:, None, :] >= 0) & (kpos[:, None, :] < n)
    s_win = jnp.where(valid[None, :, None, None], s_win, NEG_INF)
    s_ctx = jnp.einsum('bnqkgd,bckd->bnkgqc', qb, k_ctx).astype(jnp.float32) * scale
    s_sink = _sink_column(sink, kvh, g, s_win.shape[:-1] + (1,))
    p = jax.nn.softmax(jnp.concatenate([s_win, s_ctx, s_sink], axis=-1), axis=-1)
    nw = 3 * Q_BLOCK
    p_win = p[..., :nw].astype(v.dtype)
    p_ctx = p[..., nw:nw + k_ctx.shape[1]].astype(v.dtype)
    out = jnp.einsum('bnkgqm,bnmkd->bnqkgd', p_win, vb) + jnp.einsum('bnkgqc,bckd->bnqkgd', p_ctx, v_ctx)
    return out.reshape(bsz, n, h * hd)


def _gqa_sink_dense(q, k, v, sink):
    bsz, n, h, hd = q.shape
    kvh = k.shape[2]
    g = h // kvh
    qg = q.reshape(bsz, n, kvh, g, hd)
    s = jnp.einsum('bqkgd,bckd->bkgqc', qg, k).astype(jnp.float32) * hd ** -0.5
    s_sink = _sink_column(sink, kvh, g, s.shape[:-1] + (1,))
    p = jax.nn.softmax(jnp.concatenate([s, s_sink], axis=-1), axis=-1)[..., :-1]
    return jnp.einsum('bkgqc,bckd->bqkgd', p.astype(v.dtype), v).reshape(bsz, n, h * hd)


def _dense_attn(q, k, v):
    s = jnp.einsum('bqhd,bkhd->bhqk', q, k).astype(jnp.float32) * q.shape[-1] ** -0.5
    p = jax.nn.softmax(s, axis=-1).astype(v.dtype)
    return jnp.einsum('bhqk,bkhd->bqhd', p, v)


def _mla_q(cq, q_norm, w_uq, pos):
    bsz, n, _ = cq.shape
    q = (_rms_norm(cq, q_norm) @ w_uq).reshape(bsz, n, MLA_HEADS, MLA_NOPE + MLA_ROPE)
    q_pe = q[..., MLA_NOPE:]
    if pos is not None:
        q_pe = _rope_2d(q_pe, *pos)
    return jnp.concatenate([q[..., :MLA_NOPE], q_pe], axis=-1)


def _mla_kv(ckv, kpe, kv_norm, w_uk, w_uv, pos):
    bsz, n, _ = ckv.shape
    c_n = _rms_norm(ckv, kv_norm)
    k_nope = (c_n @ w_uk).reshape(bsz, n, MLA_HEADS, MLA_NOPE)
    v = (c_n @ w_uv).reshape(bsz, n, MLA_HEADS, MLA_V)
    k_pe = kpe[:, :, None, :]
    if pos is not None:
        k_pe = _rope_2d(k_pe, *pos)
    k = jnp.concatenate([k_nope, jnp.broadcast_to(k_pe, (bsz, n, MLA_HEADS, MLA_ROPE))], axis=-1)
    return k, v


def _mla_latent(q, k, v, k_ctx, v_ctx):
    bsz, n, h, dq = q.shape
    keys = jnp.concatenate([k_ctx, k], axis=1)
    vals = jnp.concatenate([v_ctx, v], axis=1)
    qb = jnp.moveaxis(q.reshape(bsz, n // Q_BLOCK, Q_BLOCK, h, dq), 1, 0)
    out = lax.map(lambda qi: _dense_attn(qi, keys, vals), qb)
    return jnp.moveaxis(out, 0, 1).reshape(bsz, n, h * v.shape[-1])


def _conformer_conv(u, dw_w, dw_b, ln_g, ln_b):
    a, gate = jnp.split(u, 2, axis=-1)
    y = _depthwise_conv(a * jax.nn.sigmoid(gate), dw_w, dw_b)
    return jax.nn.silu(_layer_norm(y, ln_g, ln_b))


def _even_mixer(h_lat, h_ctx, pos, need_ctx, w_in, b_in, conv_w, conv_b, wa, ba, wx, bx, lam, sink, w_out, b_out):
    splits = np.cumsum([LRU_WIDTH, LRU_WIDTH, SWA_HEADS * SWA_HEAD_DIM, SWA_KV_HEADS * SWA_HEAD_DIM]).tolist()

    def project(h):
        bsz, n, _ = h.shape
        g, u, q, k, v = jnp.split(h @ w_in + b_in, splits, axis=-1)
        return (g, u, q.reshape(bsz, n, SWA_HEADS, SWA_HEAD_DIM),
                k.reshape(bsz, n, SWA_KV_HEADS, SWA_HEAD_DIM), v.reshape(bsz, n, SWA_KV_HEADS, SWA_HEAD_DIM))

    g_l, u_l, q_l, k_l, v_l = project(h_lat)
    g_c, u_c, q_c, k_c, v_c = project(h_ctx)
    rec_c, rec_l = _rglru_bidir(_depthwise_conv(u_c, conv_w, conv_b), _depthwise_conv(u_l, conv_w, conv_b),
                                wa, ba, wx, bx, lam)
    att_l = _swa_latent(_rope_2d(q_l, *pos), _rope_2d(k_l, *pos), v_l, k_c, v_c, sink)
    y_l = jnp.concatenate([jax.nn.gelu(g_l) * rec_l.astype(g_l.dtype), att_l], axis=-1) @ w_out + b_out
    y_c = None
    if need_ctx:
        att_c = _gqa_sink_dense(q_c, k_c, v_c, sink)
        y_c = jnp.concatenate([jax.nn.gelu(g_c) * rec_c.astype(g_c.dtype), att_c], axis=-1) @ w_out + b_out
    return y_l, y_c


def _odd_mixer(h_lat, h_ctx, pos, need_ctx, w_in, b_in, q_norm, kv_norm, w_uq, w_uk, w_uv,
               dw_w, dw_b, cln_g, cln_b, w_out, b_out):
    splits = np.cumsum([MLA_Q_RANK, MLA_KV_RANK, MLA_ROPE]).tolist()
    cq_l, ckv_l, kpe_l, cv_l = jnp.split(h_lat @ w_in + b_in, splits, axis=-1)
    cq_c, ckv_c, kpe_c, cv_c = jnp.split(h_ctx @ w_in + b_in, splits, axis=-1)
    k_c, v_c = _mla_kv(ckv_c, kpe_c, kv_norm, w_uk, w_uv, None)
    k_l, v_l = _mla_kv(ckv_l, kpe_l, kv_norm, w_uk, w_uv, pos)
    att_l = _mla_latent(_mla_q(cq_l, q_norm, w_uq, pos), k_l, v_l, k_c, v_c)
    conv_l = _conformer_conv(cv_l, dw_w, dw_b, cln_g, cln_b)
    y_l = jnp.concatenate([att_l, conv_l], axis=-1) @ w_out + b_out
    y_c = None
    if need_ctx:
        bsz, n, _ = cq_c.shape
        att_c = _dense_attn(_mla_q(cq_c, q_norm, w_uq, None), k_c, v_c).reshape(bsz, n, MLA_HEADS * MLA_V)
        conv_c = _conformer_conv(cv_c, dw_w, dw_b, cln_g, cln_b)
        y_c = jnp.concatenate([att_c, conv_c], axis=-1) @ w_out + b_out
    return y_l, y_c


def _hier_moe(h, w_group, b_group, w_router, b_router, w1, w3, w2):
    n_tok, d = h.shape
    g_logits = (h @ w_group + b_group).astype(jnp.float32)
    g_idx = jnp.argmax(g_logits, axis=-1)
    g_prob = jnp.take_along_axis(jax.nn.softmax(g_logits, axis=-1), g_idx[:, None], axis=-1)
    e_logits = (h @ w_router + b_router).astype(jnp.float32).reshape(n_tok, N_GROUPS, EXPERTS_PER_GROUP)
    e_logits = jnp.take_along_axis(e_logits, g_idx[:, None, None], axis=1)[:, 0]
    top_p, top_i = lax.top_k(jax.nn.softmax(e_logits, axis=-1), TOP_K)
    gates = g_prob * top_p / jnp.sum(top_p, axis=-1, keepdims=True)
    expert = (g_idx[:, None] * EXPERTS_PER_GROUP + top_i).reshape(-1)
    token = jnp.repeat(jnp.arange(n_tok, dtype=jnp.int32), TOP_K)
    gate = gates.reshape(-1)
    n_assign = n_tok * TOP_K
    order = jnp.argsort(expert)
    e_s, t_s, g_s = expert[order], token[order], gate[order]
    counts = jnp.bincount(expert, length=N_EXPERTS)
    starts = jnp.cumsum(counts) - counts
    padded = (counts + MOE_BLOCK - 1) // MOE_BLOCK * MOE_BLOCK
    p_ends = jnp.cumsum(padded)
    p_starts = p_ends - padded
    dest = p_starts[e_s] + jnp.arange(n_assign) - starts[e_s]
    n_blocks = -(-(n_assign + N_EXPERTS * (MOE_BLOCK - 1)) // MOE_BLOCK)
    slot_tok = jnp.zeros((n_blocks * MOE_BLOCK,), jnp.int32).at[dest].set(t_s)
    slot_gate = jnp.zeros((n_blocks * MOE_BLOCK,), jnp.float32).at[dest].set(g_s)
    block_expert = jnp.minimum(jnp.searchsorted(p_ends, jnp.arange(n_blocks) * MOE_BLOCK, side='right'), N_EXPERTS - 1)
    xb = h[slot_tok].reshape(n_blocks, MOE_BLOCK, d)

    def expert_block(args):
        xi, e = args
        return (jax.nn.silu(xi @ w1[e]) * (xi @ w3[e])) @ w2[e]

    yb = lax.map(expert_block, (xb, block_expert)).reshape(-1, d)
    return jax.ops.segment_sum(yb * slot_gate[:, None].astype(yb.dtype), slot_tok, num_segments=n_tok)


def setup_inputs(seed: int = 0) -> dict:
    key = jax.random.key(seed)
    keys = jax.random.split(key, 64)
    counter = [0]

    def nxt():
        k = keys[counter[0]]
        counter[0] += 1
        return k

    def nrm(shape, scale):
        return jax.random.normal(nxt(), shape, jnp.float32) * scale

    def gain(shape):
        return 1.0 + nrm(shape, 0.05)

    D = D_MODEL
    u = jax.random.uniform(nxt(), (N_EVEN, 2, LRU_WIDTH), jnp.float32, 0.9, 0.999)
    a0 = u ** (1.0 / LRU_C)
    lam = jnp.log(a0) - jnp.log1p(-a0)
    return {
        "x": nrm((BATCH, SEQ, D), 1.0),
        "c": nrm((BATCH, D), 1.0),
        "ctx": nrm((BATCH, CTX_LEN, D), 1.0),
        "c_ctx": nrm((D,), 1.0),
        "w_mod": nrm((DEPTH, D, 6 * D), 0.5 * D ** -0.5),
        "b_mod": nrm((DEPTH, 6 * D), 0.02),
        "ln_g": gain((DEPTH, 2, D)),
        "ln_b": nrm((DEPTH, 2, D), 0.02),
        "e_w_in": nrm((N_EVEN, D, EVEN_IN), D ** -0.5),
        "e_b_in": nrm((N_EVEN, EVEN_IN), 0.02),
        "e_conv_w": nrm((N_EVEN, LRU_CONV_W, LRU_WIDTH), LRU_CONV_W ** -0.5),
        "e_conv_b": nrm((N_EVEN, LRU_WIDTH), 0.02),
        "e_lru_wa": nrm((N_EVEN, 2, LRU_BLOCKS, LRU_BLOCK_DIM, LRU_BLOCK_DIM), LRU_BLOCK_DIM ** -0.5),
        "e_lru_ba": nrm((N_EVEN, 2, LRU_WIDTH), 0.02),
        "e_lru_wx": nrm((N_EVEN, 2, LRU_BLOCKS, LRU_BLOCK_DIM, LRU_BLOCK_DIM), LRU_BLOCK_DIM ** -0.5),
        "e_lru_bx": nrm((N_EVEN, 2, LRU_WIDTH), 0.02),
        "e_lru_lambda": lam,
        "e_sink": nrm((N_EVEN, SWA_HEADS), 0.5),
        "e_w_out": nrm((N_EVEN, EVEN_MIX, D), EVEN_MIX ** -0.5 * DN_BETA),
        "e_b_out": nrm((N_EVEN, D), 0.02),
        "o_w_in": nrm((N_ODD, D, ODD_IN), D ** -0.5),
        "o_b_in": nrm((N_ODD, ODD_IN), 0.02),
        "o_q_norm": gain((N_ODD, MLA_Q_RANK)),
        "o_kv_norm": gain((N_ODD, MLA_KV_RANK)),
        "o_w_uq": nrm((N_ODD, MLA_Q_RANK, MLA_HEADS * (MLA_NOPE + MLA_ROPE)), MLA_Q_RANK ** -0.5),
        "o_w_uk": nrm((N_ODD, MLA_KV_RANK, MLA_HEADS * MLA_NOPE), MLA_KV_RANK ** -0.5),
        "o_w_uv": nrm((N_ODD, MLA_KV_RANK, MLA_HEADS * MLA_V), MLA_KV_RANK ** -0.5),
        "o_dw_w": nrm((N_ODD, CONF_K, CONF_CH), CONF_K ** -0.5),
        "o_dw_b": nrm((N_ODD, CONF_CH), 0.02),
        "o_cln_g": gain((N_ODD, CONF_CH)),
        "o_cln_b": nrm((N_ODD, CONF_CH), 0.02),
        "o_w_out": nrm((N_ODD, ODD_MIX, D), ODD_MIX ** -0.5 * DN_BETA),
        "o_b_out": nrm((N_ODD, D), 0.02),
        "moe_w_group": nrm((DEPTH, D, N_GROUPS), D ** -0.5),
        "moe_b_group": nrm((DEPTH, N_GROUPS), 0.01),
        "moe_w_router": nrm((DEPTH, D, N_EXPERTS), D ** -0.5),
        "moe_b_router": nrm((DEPTH, N_EXPERTS), 0.01),
        "moe_w1": nrm((DEPTH, N_EXPERTS, D, D_EXPERT), D ** -0.5),
        "moe_w3": nrm((DEPTH, N_EXPERTS, D, D_EXPERT), D ** -0.5),
        "moe_w2": nrm((DEPTH, N_EXPERTS, D_EXPERT, D), D_EXPERT ** -0.5 * DN_BETA),
    }


def reference(x, c, ctx, c_ctx, w_mod, b_mod, ln_g, ln_b,
              e_w_in, e_b_in, e_conv_w, e_conv_b, e_lru_wa, e_lru_ba, e_lru_wx, e_lru_bx, e_lru_lambda,
              e_sink, e_w_out, e_b_out,
              o_w_in, o_b_in, o_q_norm, o_kv_norm, o_w_uq, o_w_uk, o_w_uv, o_dw_w, o_dw_b, o_cln_g, o_cln_b,
              o_w_out, o_b_out,
              moe_w_group, moe_b_group, moe_w_router, moe_b_router, moe_w1, moe_w3, moe_w2):
    pos = _grid_positions(x.shape[1])
    x_lat, x_ctx = x, ctx
    for layer in range(DEPTH):
        need_ctx = layer < DEPTH - 1
        m_l = _modulation(c, w_mod[layer], b_mod[layer])
        m_c = _modulation(c_ctx, w_mod[layer], b_mod[layer])
        h_l = _modulate(x_lat, m_l[0], m_l[1])
        h_c = _modulate(x_ctx, m_c[0], m_c[1])
        p = layer // 2
        if layer % 2 == 0:
            y_l, y_c = _even_mixer(h_l, h_c, pos, need_ctx, e_w_in[p], e_b_in[p], e_conv_w[p], e_conv_b[p],
                                   e_lru_wa[p], e_lru_ba[p], e_lru_wx[p], e_lru_bx[p], e_lru_lambda[p],
                                   e_sink[p], e_w_out[p], e_b_out[p])
        else:
            y_l, y_c = _odd_mixer(h_l, h_c, pos, need_ctx, o_w_in[p], o_b_in[p], o_q_norm[p], o_kv_norm[p],
                                  o_w_uq[p], o_w_uk[p], o_w_uv[p], o_dw_w[p], o_dw_b[p], o_cln_g[p], o_cln_b[p],
                                  o_w_out[p], o_b_out[p])
        x_lat = _layer_norm(DN_ALPHA * x_lat + m_l[2] * y_l, ln_g[layer, 0], ln_b[layer, 0])
        f_l = _modulate(x_lat, m_l[3], m_l[4])
        n_lat = f_l.shape[0] * f_l.shape[1]
        if need_ctx:
            x_ctx = _layer_norm(DN_ALPHA * x_ctx + m_c[2] * y_c, ln_g[layer, 0], ln_b[layer, 0])
            f_c = _modulate(x_ctx, m_c[3], m_c[4])
            tokens = jnp.concatenate([f_l.reshape(n_lat, -1), f_c.reshape(-1, f_c.shape[-1])], axis=0)
        else:
            tokens = f_l.reshape(n_lat, -1)
        ffn = _hier_moe(tokens, moe_w_group[layer], moe_b_group[layer], moe_w_router[layer], moe_b_router[layer],
                        moe_w1[layer], moe_w3[layer], moe_w2[layer])
        x_lat = _layer_norm(DN_ALPHA * x_lat + m_l[5] * ffn[:n_lat].reshape(x_lat.shape), ln_g[layer, 1], ln_b[layer, 1])
        if need_ctx:
            x_ctx = _layer_norm(DN_ALPHA * x_ctx + m_c[5] * ffn[n_lat:].reshape(x_ctx.shape), ln_g[layer, 1], ln_b[layer, 1])
    return x_lat
```

```python
import contextlib
import numpy as np
import concourse.bass as bass
import concourse.mybir as mybir
from concourse.bass_utils import run_bass_kernel_spmd

F32 = mybir.dt.float32
BF16 = mybir.dt.bfloat16
I32 = mybir.dt.int32
AF = mybir.ActivationFunctionType
ALU = mybir.AluOpType
AX = mybir.AxisListType

D = 1024
SEM_LIMIT = 30000
DN_ALPHA = 4.0 ** 0.25
LN_EPS = 1e-6


class _Rec:
    def __getattr__(self, name):
        def f(*a, **kw):
            self.call = (name, a, kw)
            return self
        return f


class Prog:
    ENGS = ("tensor", "vector", "scalar", "gpsimd", "sync")

    def __init__(self, nc, n_dma_sems=14):
        self.nc = nc
        self.ops = {e: [] for e in self.ENGS}
        self.sems = {}
        self.sem_order = []
        self.cnt = {e: 0 for e in self.ENGS}
        self.epoch = {e: 0 for e in self.ENGS}
        self.known = {e: {} for e in self.ENGS}
        self.last_w = {}
        self.readers = {}
        self.dma_pool = {e: [[f"d_{e}_{i}", 0] for i in range(n_dma_sems)] for e in ("sync", "gpsimd", "scalar")}
        self.dma_rr = {e: 0 for e in ("sync", "gpsimd", "scalar")}
        self.out_tokens = []
        self.last_tok = {}
        self.nops = 0

    def _sem(self, name):
        if name not in self.sems:
            self.sems[name] = None
            self.sem_order.append(name)
        return name

    limit = None

    def op(self, eng, fn, reads=(), writes=(), dma=False, final=False, late=False):
        if Prog.limit is not None and self.nops >= Prog.limit:
            return None
        pr = [b for b in reads if b.startswith("pb")]
        if pr:
            reads = [b for b in reads if not b.startswith("pb")]
            writes = list(writes) + pr
        deps = {}

        def need(tok):
            if tok is None:
                return
            s, v, e = tok
            if eng == "tensor" and e == "tensor" and not dma:
                return
            if deps.get(s, 0) < v:
                deps[s] = v

        for b in reads:
            need(self.last_w.get(b))
        for b in writes:
            need(self.last_w.get(b))
            for t in self.readers.get(b, ()):
                need(t)
        if dma:
            pool = self.dma_pool[eng]
            i = self.dma_rr[eng]
            self.dma_rr[eng] = (i + 1) % len(pool)
            ent = pool[i]
            if ent[1] > 0:
                need((ent[0], ent[1], "dma"))
            ent[1] += 16
            tok = (self._sem(ent[0]), ent[1], "dma")
            inc = (ent[0], 16)
            self.last_tok[ent[0]] = tok
        else:
            if self.cnt[eng] >= SEM_LIMIT:
                self.epoch[eng] += 1
                self.cnt[eng] = 0
            self.cnt[eng] += 1
            sname = self._sem(f"e_{eng}_{self.epoch[eng]}")
            tok = (sname, self.cnt[eng], eng)
            inc = (sname, 1)
            self.last_tok[sname] = tok
        kn = self.known[eng]
        waits = []
        for s, v in deps.items():
            if kn.get(s, 0) < v:
                waits.append((s, v))
                kn[s] = v
        if late:
            self.ops[eng].append((waits, fn, inc))
        else:
            rec = _Rec()
            fn(rec)
            self.ops[eng].append((waits, rec.call, inc))
        for b in writes:
            self.last_w[b] = tok
            self.readers[b] = []
        for b in reads:
            self.readers.setdefault(b, []).append(tok)
        if final:
            self.out_tokens.append(tok)
        self.nops += 1
        return tok

    def dma(self, out, in_, reads=(), writes=(), eng="sync", final=False, **kw):
        if out.dtype != in_.dtype:
            eng = "gpsimd"
        return self.op(eng, lambda e: e.dma_start(out=out, in_=in_, **kw), reads, writes, dma=True, final=final)

    def vload(self, eng, name, ap, lo, hi, reads):
        kn = self.known[eng]
        waits = []
        for b in reads:
            t = self.last_w.get(b)
            if t is not None and kn.get(t[0], 0) < t[1]:
                waits.append((t[0], t[1]))
                kn[t[0]] = t[1]
        self.ops[eng].append((waits, ("__vload__", name, ap, lo, hi), None))

    def barrier(self):
        toks = list(self.last_tok.values())
        for eng in self.ENGS:
            kn = self.known[eng]
            waits = []
            for s, v, _ in toks:
                if kn.get(s, 0) < v:
                    waits.append((s, v))
                    kn[s] = v
            if waits:
                self.ops[eng].append((waits, None, None))
        self.last_w = {}
        self.readers = {}

    def emit(self):
        nc = self.nc
        with contextlib.ExitStack() as st:
            for name in self.sem_order:
                self.sems[name] = st.enter_context(nc.semaphore(name))
            block = st.enter_context(nc.Block())
            sems = self.sems
            out_tokens = self.out_tokens

            def run(engname):
                def body(e):
                    env = {}
                    for waits, fn, inc in self.ops[engname]:
                        for s, v in waits:
                            e.wait_ge(sems[s], v)
                        if fn is None:
                            continue
                        if callable(fn):
                            fn(e, env).then_inc(sems[inc[0]], inc[1])
                        elif fn[0] == "__vload__":
                            env[fn[1]] = e.value_load(fn[2], min_val=fn[3], max_val=fn[4])
                        else:
                            getattr(e, fn[0])(*fn[1], **fn[2]).then_inc(sems[inc[0]], inc[1])
                    if engname == "sync":
                        for s, v, _ in out_tokens:
                            e.wait_ge(sems[s], v)
                return body

            block.tensor(run("tensor"))
            block.vector(run("vector"))
            block.scalar(run("scalar"))
            block.gpsimd(run("gpsimd"))
            block.sync(run("sync"))


class K:
    def __init__(self, S, C, debug=False):
        self.S, self.C, self.T = S, C, S + C
        self.debug = debug
        self.nc = bass.Bass("TRN2", target_bir_lowering=False)
        self.P = Prog(self.nc)
        self.inputs = {}
        self.scr = {}
        self.pb_rr = 0
        self.uid = 0
        self.debug_barrier = False

    def inp(self, name, shape, dt=F32):
        ap = self.nc.dram_tensor(name, list(shape), dt, kind="ExternalInput").ap()
        self.inputs[name] = ap
        return ap

    def scratch(self, name, shape, dt=F32):
        kind = "ExternalOutput" if self.debug else "Internal"
        ap = self.nc.dram_tensor(name, list(shape), dt, kind=kind).ap()
        self.scr[name] = ap
        return ap


def build(S, C, debug=False, stop_after=None, skip_l0=False):
    k = K(S, C, debug)
    nc, P = k.nc, k.P
    T = S + C
    NT_L, NT_C = S // 128, C // 128
    SQ = S // 2
    SH = SQ + 128

    x_d = k.inp("x", [S, D])
    ctx_d = k.inp("ctx", [C, D])
    cc_d = k.inp("cvec", [2, D])
    ident_d = k.inp("ident", [128, 128])
    w_mod_d = k.inp("w_mod", [2, D, 6 * D])
    b_mod_d = k.inp("b_mod", [2, 6 * D])
    ln_g_d = k.inp("ln_g", [2, 2, D])
    ln_b_d = k.inp("ln_b", [2, 2, D])
    e_w_in_d = k.inp("e_w_in", [D, 2432])
    e_b_in_d = k.inp("e_b_in", [2432])
    e_conv_w_d = k.inp("e_conv_w", [4, 512])
    e_conv_b_d = k.inp("e_conv_b", [512])
    e_wa_d = k.inp("e_lru_wa", [2, 8, 64, 64])
    e_ba_d = k.inp("e_lru_ba", [2, 512])
    e_wx_d = k.inp("e_lru_wx", [2, 8, 64, 64])
    e_bx_d = k.inp("e_lru_bx", [2, 512])
    e_lam_d = k.inp("e_lru_lambda", [2, 512])
    e_sink_d = k.inp("e_sink", [8])
    e_w_out_d = k.inp("e_w_out", [D, D])
    e_b_out_d = k.inp("e_b_out", [D])
    rope_e_d = k.inp("rope_e", [2, 128, S])
    mask_d = k.inp("swa_mask", [2, 128, 512])
    o_w_in_d = k.inp("o_w_in", [D, 1440])
    o_b_in_d = k.inp("o_b_in", [1440])
    o_q_norm_d = k.inp("o_q_norm", [256])
    o_kv_norm_d = k.inp("o_kv_norm", [128])
    o_w_uq_d = k.inp("o_w_uq", [256, 8, 192])
    o_w_uk_d = k.inp("o_w_uk", [128, 512])
    o_w_uv_d = k.inp("o_w_uv", [128, 512])
    o_w_kpe_rot_d = k.inp("o_w_kpe_rot", [D, 32])
    o_b_kpe_rot_d = k.inp("o_b_kpe_rot", [32])
    rope_q_d = k.inp("rope_q", [2, 96, SH])
    rope_k_d = k.inp("rope_k", [2, 32, S])
    xh_idx_d = k.inp("xh_idx", [SH], I32)
    zmask_d = k.inp("zmask", [SH])
    o_dw_w_d = k.inp("o_dw_w", [31, 512])
    o_dw_b_d = k.inp("o_dw_b", [512])
    o_cln_g_d = k.inp("o_cln_g", [512])
    o_cln_b_d = k.inp("o_cln_b", [512])
    o_w_out_d = k.inp("o_w_out", [D, D])
    o_b_out_d = k.inp("o_b_out", [D])
    moe_wg_d = k.inp("moe_w_gr", [2, D, 36])
    moe_bg_d = k.inp("moe_b_gr", [2, 36])
    moe_w1_d = k.inp("moe_w1", [2, 32, D, 512])
    moe_w3_d = k.inp("moe_w3", [2, 32, D, 512])
    moe_w2_d = k.inp("moe_w2", [2, 32, 512, D])
    out_d = nc.dram_tensor("out", [SQ, D], F32, kind="ExternalOutput").ap()

    m_scr = k.scratch("m_scr", [2, 2, 6 * D])
    XA = k.scratch("XA", [T, D])
    XBp = k.scratch("XB", [128 + T, D])
    XB = XBp[128:, :]
    XH = k.scratch("XH", [SH, D])
    FT = k.scratch("FT", [D, T], BF16)
    GATE = k.scratch("GATE", [T, 32])
    G_s = k.scratch("G_s", [512, T], BF16)
    U_s = k.scratch("U_s", [512, T])
    HF_s = k.scratch("HF_s", [512, T])
    MIXA = k.scratch("MIXA", [512, T], BF16)
    QT_s = k.scratch("QT_s", [8, 64, T], BF16)
    KT_s = k.scratch("KT_s", [2, 64, T], BF16)
    V_s = k.scratch("V_s", [T, 2, 65], BF16)
    ATT = k.scratch("ATT", [T, 512])
    QM = k.scratch("QM", [8, 96, SH], BF16)
    KM = k.scratch("KM", [8, 96, T], BF16)
    VM = k.scratch("VM", [T, 8, 65], BF16)
    ZC = k.scratch("ZC", [512, SH], BF16)
    CONV = k.scratch("CONV", [SQ, 512])

    st_all = contextlib.ExitStack()
    ps = st_all.enter_context(nc.psum_tensor("ps", [128, 4096], F32))
    SB_WORDS = 52000
    big = st_all.enter_context(nc.sbuf_tensor("big", [128, SB_WORDS], F32))
    k.sb_ptr = 0

    def pbank(n=1):
        if n == 2 and k.pb_rr % 2 == 1:
            k.pb_rr += 1
        i = k.pb_rr % 8
        k.pb_rr += n
        return ps[:, i * 512:(i + n) * 512], [f"pb{i + j}" for j in range(n)]

    class scope:
        def __enter__(self):
            self.mark = k.sb_ptr
            return self

        def __exit__(self, *a):
            k.sb_ptr = self.mark
            return False

    def sb(st, name, shape, dt=F32):
        k.uid += 1
        nm = f"{name}_{k.uid}"
        nfree = int(np.prod(shape[1:]))
        nwords = nfree if dt == F32 else (nfree + 1) // 2
        off = k.sb_ptr
        k.sb_ptr += nwords
        assert k.sb_ptr <= SB_WORDS, (name, k.sb_ptr)
        ap = big[:, off:off + nwords]
        if dt != F32:
            ap = ap.bitcast(dt)[:, 0:nfree]
        ap = ap[0:shape[0]]
        if len(shape) == 3:
            ap = ap.rearrange("p (a b) -> p a b", a=shape[1])
        elif len(shape) == 4:
            ap = ap.rearrange("p (a b c) -> p a b c", a=shape[1], b=shape[2])
        elif len(shape) == 5:
            ap = ap.rearrange("p (a b c d) -> p a b c d", a=shape[1], b=shape[2], c=shape[3])
        return ap, nm

    V, A, G, TE = "vector", "scalar", "gpsimd", "tensor"

    def ring(st, name, shape, dt=F32, n=2, init=None):
        tiles = [sb(st, name, shape, dt) for _ in range(n)]
        if init is not None:
            for t_, k_ in tiles:
                P.op(V, lambda e, t_=t_: e.memset(t_[:], init), [], [k_])
        cnt = [0]

        def nxt():
            cnt[0] += 1
            return tiles[(cnt[0] - 1) % n]
        return nxt

    ident, ident_k = sb(None, "ident", [128, 128])
    P.dma(ident[:], ident_d, writes=[ident_k])
    ones_r, ones_k = sb(None, "ones", [1, 128])
    P.op(V, lambda e: e.memset(ones_r[:], 1.0), [], [ones_k])
    mcol, mcol_k = sb(None, "mcol", [128, 2, 2, 4, 8])

    def col_dma(dst_ap, src_ap, keys_w, eng="gpsimd", reads=()):
        P.dma(dst_ap, src_ap, writes=keys_w, reads=reads, eng=eng, allow_slow_non_contiguous=True)

    with scope() as st:
        csT, csT_k = sb(st, "csT", [128, 8, 2])
        craw, craw_k = sb(st, "craw", [128, 8, 2])
        for s_ in range(2):
            col_dma(craw[:, :, s_], cc_d[s_].rearrange("(c p) -> p c", p=128), [craw_k])
        P.op(A, lambda e: e.activation(out=csT[:], in_=craw[:], func=AF.Silu), [craw_k], [csT_k])
        bm, bm_k = sb(st, "bm", [2, 6 * D])
        mrow, mrow_k = sb(st, "mrow", [2, 6 * D])
        wm = [sb(st, f"wm{i}", [128, 8, 512]) for i in range(2)]
        for l in range(2):
            P.dma(bm[:], b_mod_d[l].partition_broadcast(2), writes=[bm_k], reads=[])
            for n in range(12):
                wt, wk = wm[n % 2]
                P.dma(wt[:], w_mod_d[l, :, n * 512:(n + 1) * 512].rearrange("(kc p) n -> p kc n", p=128), writes=[wk],
                      eng="sync" if n % 2 == 0 else "gpsimd")
                pb, pk = pbank()
                for kc in range(8):
                    P.op(TE, lambda e, kc=kc, wt=wt, pb=pb: e.matmul(pb[0:2, :], lhsT=csT[:, kc, :], rhs=wt[:, kc, :],
                                                                   start=(kc == 0), stop=(kc == 7)),
                         [csT_k, wk], pk)
                P.op(V, lambda e, pb=pb, n=n: e.tensor_tensor(out=mrow[:, n * 512:(n + 1) * 512], in0=pb[0:2, :],
                                                               in1=bm[:, n * 512:(n + 1) * 512], op=ALU.add),
                     pk + [bm_k], [mrow_k])
            P.dma(m_scr[l], mrow[:], reads=[mrow_k], writes=["m_scr"], eng="gpsimd")
        zpad, zpadk = sb(st, "zpad", [64, D])
        P.op(V, lambda e: e.memset(zpad[:], 0.0), [], [zpadk])
        P.dma(XBp[64:128, :], zpad[:], reads=[zpadk], writes=["XBpad"], eng="gpsimd")
        for l in range(2):
            for s_ in range(2):
                for j, idx in enumerate((0, 1, 3, 4)):
                    col_dma(mcol[:, l, s_, j, :], m_scr[l, s_, idx * D:(idx + 1) * D].rearrange("(c p) -> p c", p=128),
                            [mcol_k], reads=["m_scr"])
        P.op(V, lambda e: e.tensor_scalar_add(out=mcol[:, :, :, 1, :], in0=mcol[:, :, :, 1, :], scalar1=1.0), [mcol_k], [mcol_k])
        P.op(V, lambda e: e.tensor_scalar_add(out=mcol[:, :, :, 3, :], in0=mcol[:, :, :, 3, :], scalar1=1.0), [mcol_k], [mcol_k])
    P.barrier()

    def load_bc(st, name, row_ap, n):
        t, tk = sb(st, name, [128, n])
        P.dma(t[:], row_ap.partition_broadcast(128), writes=[tk], eng="gpsimd")
        return t, tk

    def tok_src(l_idx, which):
        raise NotImplementedError

    def transpose_mod(xt, xk, dst, dk, col_sc, col_sh, ck, tcol, out_parity=0):
        pb, pk = pbank(2)
        for kc in range(8):
            P.op(TE, lambda e, kc=kc, pb=pb: e.transpose(pb[:, kc * 128:(kc + 1) * 128], xt[:, kc * 128:(kc + 1) * 128], ident[:]),
                 [xk, ident_k], pk)
        for kc in range(8):
            if kc % 2 == 0:
                P.op(V, lambda e, kc=kc, pb=pb: e.tensor_scalar(out=dst[:, kc, tcol:tcol + 128], in0=pb[:, kc * 128:(kc + 1) * 128],
                                                               scalar1=col_sc[:, kc:kc + 1], scalar2=col_sh[:, kc:kc + 1],
                                                               op0=ALU.mult, op1=ALU.add), pk + [ck], [dk])
            else:
                P.op(A, lambda e, kc=kc, pb=pb: e.activation(out=dst[:, kc, tcol:tcol + 128], in_=pb[:, kc * 128:(kc + 1) * 128],
                                                            func=AF.Identity, scale=col_sc[:, kc:kc + 1], bias=col_sh[:, kc:kc + 1]),
                     pk + [ck], [dk])

    def layer_norm_tile(st_tiles, z, zk, g_bc, gk, b_bc, bk, out, ok, width=D):
        stats, sk = st_tiles["stats"]
        mv, mvk = st_tiles["mv"]
        nchunk = width // 512
        for c_ in range(nchunk):
            P.op(V, lambda e, c_=c_: e.bn_stats(out=stats[:, c_, :], in_=z[:, c_ * 512:(c_ + 1) * 512]), [zk], [sk])
        P.op(V, lambda e: e.bn_aggr(out=mv[:, 0:2], in_=stats[:, 0:nchunk, :]), [sk], [mvk])
        P.op(V, lambda e: e.tensor_scalar_add(out=mv[:, 2:3], in0=mv[:, 1:2], scalar1=LN_EPS), [mvk], [mvk])
        P.op(A, lambda e: e.sqrt(out=mv[:, 3:4], in_=mv[:, 2:3]), [mvk], [mvk])
        P.op(V, lambda e: e.reciprocal(out=mv[:, 4:5], in_=mv[:, 3:4]), [mvk], [mvk])
        P.op(V, lambda e: e.scalar_tensor_tensor(out=mv[:, 5:6], in0=mv[:, 0:1], scalar=-1.0, in1=mv[:, 4:5],
                                                 op0=ALU.mult, op1=ALU.mult), [mvk], [mvk])
        P.op(A, lambda e: e.activation(out=out[:, 0:width], in_=z[:, 0:width], func=AF.Identity, scale=mv[:, 4:5], bias=mv[:, 5:6]),
             [zk, mvk], [ok])
        P.op(G, lambda e: e.tensor_tensor(out=out[:, 0:width], in0=out[:, 0:width], in1=g_bc[:, 0:width], op=ALU.mult), [ok, gk], [ok])
        P.op(V, lambda e: e.tensor_tensor(out=out[:, 0:width], in0=out[:, 0:width], in1=b_bc[:, 0:width], op=ALU.add), [ok, bk], [ok])

    def tok_tiles(n_lat_only=False, n_lat=None):
        r = [(i * 128, 0) for i in range(NT_L if n_lat is None else n_lat // 128)]
        if not n_lat_only:
            r += [(S + i * 128, 1) for i in range(NT_C)]
        return r

    def mixer_epilogue(l, w_out_d, b_out_d, x_src, mixT_loader, with_ctx, n_lat=None):
        with scope() as st:
            wo, wok = sb(st, "wo", [128, 8, D], BF16)
            P.dma(wo[:], w_out_d.rearrange("(kc p) n -> p kc n", p=128), writes=[wok], eng="gpsimd")
            wr, wrk = sb(st, "wr", [128, 8, 36])
            P.dma(wr[:], moe_wg_d[l].rearrange("(kc p) n -> p kc n", p=128), writes=[wrk])
            br, brk = load_bc(st, "br", moe_bg_d[l], 36)
            lng, lngk = load_bc(st, "lng", ln_g_d[l, 0], D)
            lnb, lnbk = load_bc(st, "lnb", ln_b_d[l, 0], D)
            streams = (0, 1) if with_ctx else (0,)
            gate_bc, gb_bc = {}, {}
            bo, bok = load_bc(st, "bo", b_out_d, D)
            for s_ in streams:
                gate_bc[s_] = load_bc(st, f"gate{s_}", m_scr[l, s_, 2 * D:3 * D], D)
                gb_bc[s_] = sb(st, f"gb{s_}", [128, D])
                P.op(V, lambda e, s_=s_: e.tensor_tensor(out=gb_bc[s_][0][:], in0=gate_bc[s_][0][:], in1=bo[:], op=ALU.mult),
                     [gate_bc[s_][1], bok], [gb_bc[s_][1]])
            NBUF = 2
            bufs = []
            for bi in range(NBUF):
                bufs.append(dict(
                    xt=sb(st, "xt", [128, D]), mixT=sb(st, "mixT", [128, 8, 128], BF16), tmp=sb(st, "tmp", [128, D]),
                    z=sb(st, "z", [128, D]), x1=sb(st, "x1", [128, D]), fT=sb(st, "fT", [128, 8, 128]),
                    fTb=sb(st, "fTb", [128, 8, 128], BF16),
                    small={"stats": sb(st, "stats", [128, 2, 6]), "mv": sb(st, "mv", [128, 8])},
                    lg=sb(st, "lg", [128, 36]), rt=sb(st, "rt", [128, 64]), gt=sb(st, "gt", [128, 32]), ld={}))
            for ti_, (t0, s_) in enumerate(tok_tiles(not with_ctx, n_lat)):
                B_ = bufs[ti_ % NBUF]
                xt, xk = B_["xt"]; mixT, mixk = B_["mixT"]; tmp, tmpk = B_["tmp"]; z, zk = B_["z"]; x1, x1k = B_["x1"]
                fT, fTk = B_["fT"]; fTb, fTbk = B_["fTb"]; small = B_["small"]; lg, lgk = B_["lg"]; rt, rtk = B_["rt"]; gt, gtk = B_["gt"]
                st.ld = B_["ld"]
                P.dma(xt[:], x_src(t0, s_), writes=[xk])
                mixT_loader(t0, s_, mixT, mixk, st)
                pb, pk = pbank(2)
                for n in range(2):
                    for kc in range(8):
                        P.op(TE, lambda e, kc=kc, n=n, pb=pb: e.matmul(pb[:, n * 512:(n + 1) * 512], lhsT=mixT[:, kc, :],
                                                                       rhs=wo[:, kc, n * 512:(n + 1) * 512], start=(kc == 0), stop=(kc == 7)),
                             [mixk, wok], pk)
                gbc, gbck = gate_bc[s_]
                P.op(V, lambda e, pb=pb, gbc=gbc: e.tensor_tensor(out=tmp[:], in0=pb, in1=gbc[:], op=ALU.mult), pk + [gbck], [tmpk])
                P.op(V, lambda e: e.scalar_tensor_tensor(out=z[:], in0=xt[:], scalar=DN_ALPHA, in1=tmp[:], op0=ALU.mult, op1=ALU.add),
                     [xk, tmpk], [zk])
                P.op(G, lambda e, s_=s_: e.tensor_tensor(out=z[:], in0=z[:], in1=gb_bc[s_][0][:], op=ALU.add), [zk, gb_bc[s_][1]], [zk])
                layer_norm_tile(small, z, zk, lng, lngk, lnb, lnbk, x1, x1k)
                P.dma(XA[t0:t0 + 128, :], x1[:], reads=[x1k], writes=["XA"], eng="gpsimd")
                transpose_mod(x1, x1k, fT, fTk, mcol[:, l, s_, 3, :], mcol[:, l, s_, 2, :], mcol_k, 0)
                P.op(G, lambda e: e.tensor_copy(out=fTb[:], in_=fT[:]), [fTk], [fTbk])
                P.dma(FT[:, t0:t0 + 128].rearrange("(kc p) t -> p kc t", p=128), fTb[:], reads=[fTbk], writes=["FT"], eng="gpsimd")
                pb2, pk2 = pbank()
                for kc in range(8):
                    P.op(TE, lambda e, kc=kc, pb2=pb2: e.matmul(pb2[:, 0:36], lhsT=fT[:, kc, :], rhs=wr[:, kc, :], start=(kc == 0), stop=(kc == 7)),
                         [fTk, wrk], pk2)
                P.op(V, lambda e, pb2=pb2: e.tensor_tensor(out=lg[:], in0=pb2[:, 0:36], in1=br[:], op=ALU.add), pk2 + [brk], [lgk])
                routing(lg, lgk, rt, rtk, gt, gtk)
                P.dma(GATE[t0:t0 + 128, :], gt[:], reads=[gtk], writes=["GATE"], eng="gpsimd")
        P.barrier()

    def routing(lg, lgk, rt, rtk, gt, gtk):
        ops = P.op
        ops(V, lambda e: e.reduce_max(out=rt[:, 0:1], in_=lg[:, 0:4], axis=AX.X), [lgk], [rtk])
        ops(V, lambda e: e.tensor_scalar_mul(out=rt[:, 1:2], in0=rt[:, 0:1], scalar1=-1.0), [rtk], [rtk])
        ops(A, lambda e: e.activation(out=rt[:, 56:60], in_=lg[:, 0:4], func=AF.Exp, bias=rt[:, 1:2], scale=1.0, accum_out=rt[:, 2:3]),
            [lgk, rtk], [rtk])
        ops(V, lambda e: e.reciprocal(out=rt[:, 3:4], in_=rt[:, 2:3]), [rtk], [rtk])
        ops(V, lambda e: e.tensor_scalar(out=rt[:, 4:8], in0=lg[:, 0:4], scalar1=rt[:, 0:1], scalar2=None, op0=ALU.is_ge), [lgk, rtk], [rtk])
        ops(V, lambda e: e.tensor_scalar_mul(out=rt[:, 8:16], in0=lg[:, 4:12], scalar1=rt[:, 4:5]), [lgk, rtk], [rtk])
        for g_ in range(1, 4):
            ops(V, lambda e, g_=g_: e.scalar_tensor_tensor(out=rt[:, 8:16], in0=lg[:, 4 + 8 * g_:12 + 8 * g_], scalar=rt[:, 4 + g_:5 + g_],
                                                           in1=rt[:, 8:16], op0=ALU.mult, op1=ALU.add), [lgk, rtk], [rtk])
        ops(V, lambda e: e.max(out=rt[:, 16:24], in_=rt[:, 8:16]), [rtk], [rtk])
        ops(V, lambda e: e.tensor_tensor(out=rt[:, 24:25], in0=rt[:, 17:18], in1=rt[:, 16:17], op=ALU.subtract), [rtk], [rtk])
        ops(A, lambda e: e.activation(out=rt[:, 24:25], in_=rt[:, 24:25], func=AF.Exp), [rtk], [rtk])
        ops(V, lambda e: e.tensor_scalar_add(out=rt[:, 25:26], in0=rt[:, 24:25], scalar1=1.0), [rtk], [rtk])
        ops(V, lambda e: e.reciprocal(out=rt[:, 26:27], in_=rt[:, 25:26]), [rtk], [rtk])
        ops(V, lambda e: e.tensor_tensor(out=rt[:, 27:28], in0=rt[:, 26:27], in1=rt[:, 3:4], op=ALU.mult), [rtk], [rtk])
        ops(V, lambda e: e.tensor_tensor(out=rt[:, 28:29], in0=rt[:, 27:28], in1=rt[:, 24:25], op=ALU.mult), [rtk], [rtk])
        ops(V, lambda e: e.tensor_scalar(out=rt[:, 32:40], in0=rt[:, 8:16], scalar1=rt[:, 16:17], scalar2=None, op0=ALU.is_ge), [rtk], [rtk])
        ops(V, lambda e: e.tensor_scalar(out=rt[:, 40:48], in0=rt[:, 8:16], scalar1=rt[:, 17:18], scalar2=None, op0=ALU.is_ge), [rtk], [rtk])
        ops(V, lambda e: e.tensor_tensor(out=rt[:, 40:48], in0=rt[:, 40:48], in1=rt[:, 32:40], op=ALU.subtract), [rtk], [rtk])
        ops(V, lambda e: e.tensor_scalar_mul(out=rt[:, 48:56], in0=rt[:, 32:40], scalar1=rt[:, 27:28]), [rtk], [rtk])
        ops(V, lambda e: e.scalar_tensor_tensor(out=rt[:, 48:56], in0=rt[:, 40:48], scalar=rt[:, 28:29], in1=rt[:, 48:56],
                                                op0=ALU.mult, op1=ALU.add), [rtk], [rtk])
        for g_ in range(4):
            ops(V, lambda e, g_=g_: e.tensor_scalar_mul(out=gt[:, 8 * g_:8 * g_ + 8], in0=rt[:, 48:56], scalar1=rt[:, 4 + g_:5 + g_]),
                [rtk], [gtk])

    def moe_phase(l, with_ctx, dst_fn, final, n_lat=None):
        Tn = T if with_ctx else (S if n_lat is None else n_lat)
        TG = 1024
        with scope() as st:
            lng, lngk = load_bc(st, "lng2", ln_g_d[l, 1], D)
            lnb, lnbk = load_bc(st, "lnb2", ln_b_d[l, 1], D)
            g5 = {0: load_bc(st, "g5l", m_scr[l, 0, 5 * D:6 * D], D)}
            if with_ctx:
                g5[1] = load_bc(st, "g5c", m_scr[l, 1, 5 * D:6 * D], D)
            ftg, ftgk = sb(st, "ftg", [128, 8, TG], BF16)
            gts, gtsk = sb(st, "gts", [128, 8, 32])
            acc, acck = sb(st, "acc", [128, 8, D])
            w1 = [sb(st, f"w1_{i}", [128, 8, 512], BF16) for i in range(2)]
            w3 = [sb(st, f"w3_{i}", [128, 8, 512], BF16) for i in range(2)]
            w2 = [sb(st, f"w2_{i}", [128, 4, D], BF16) for i in range(2)]
            hm, hmk = sb(st, "hm", [128, 4, TG], BF16)
            sil, silk = sb(st, "sil", [128, 512])
            xt, xk = sb(st, "xt2", [128, D])
            z, zk = sb(st, "z2", [128, D])
            xo, xok = sb(st, "xo", [128, D])
            small = {"stats": sb(st, "stats2", [128, 2, 6]), "mv": sb(st, "mv2", [128, 8])}
            for g0 in range(0, Tn, TG):
                ng = min(TG, Tn - g0)
                ntt = ng // 128
                P.dma(ftg[:, :, 0:ng], FT[:, g0:g0 + ng].rearrange("(kc p) t -> p kc t", p=128), reads=["FT"], writes=[ftgk])
                P.dma(gts[:, 0:ntt, :], GATE[g0:g0 + ng, :].rearrange("(a p) e -> p a e", p=128), reads=["GATE"], writes=[gtsk], eng="gpsimd")
                for ex in range(32):
                    w1t, w1k = w1[ex % 2]
                    w3t, w3k = w3[ex % 2]
                    w2t, w2k = w2[ex % 2]
                    P.dma(w1t[:], moe_w1_d[l, ex].rearrange("(kc p) n -> p kc n", p=128), writes=[w1k], eng="sync")
                    P.dma(w3t[:], moe_w3_d[l, ex].rearrange("(kc p) n -> p kc n", p=128), writes=[w3k], eng="sync")
                    P.dma(w2t[:], moe_w2_d[l, ex].rearrange("(kc p) n -> p kc n", p=128), writes=[w2k], eng="sync")
                    for n0 in range(0, ng, 512):
                        nn = min(512, ng - n0)
                        for oc in range(4):
                            p1, p1k = pbank()
                            p3, p3k = pbank()
                            for kc in range(8):
                                P.op(TE, lambda e, kc=kc, oc=oc, p1=p1, w1t=w1t, n0=n0, nn=nn: e.matmul(
                                    p1[:, 0:nn], lhsT=w1t[:, kc, oc * 128:(oc + 1) * 128], rhs=ftg[:, kc, n0:n0 + nn],
                                    start=(kc == 0), stop=(kc == 7)), [w1k, ftgk], p1k)
                            for kc in range(8):
                                P.op(TE, lambda e, kc=kc, oc=oc, p3=p3, w3t=w3t, n0=n0, nn=nn: e.matmul(
                                    p3[:, 0:nn], lhsT=w3t[:, kc, oc * 128:(oc + 1) * 128], rhs=ftg[:, kc, n0:n0 + nn],
                                    start=(kc == 0), stop=(kc == 7)), [w3k, ftgk], p3k)
                            P.op(A, lambda e, p1=p1, nn=nn: e.activation(out=sil[:, 0:nn], in_=p1[:, 0:nn], func=AF.Silu), p1k, [silk])
                            P.op(V, lambda e, p3=p3, nn=nn, oc=oc, n0=n0: e.tensor_tensor(out=hm[:, oc, n0:n0 + nn], in0=p3[:, 0:nn],
                                                                                         in1=sil[:, 0:nn], op=ALU.mult), p3k + [silk], [hmk])
                    for tt in range(ntt):
                        py, pyk = pbank(2)
                        for n in range(2):
                            for kc in range(4):
                                P.op(TE, lambda e, kc=kc, n=n, tt=tt, py=py, w2t=w2t: e.matmul(
                                    py[:, n * 512:(n + 1) * 512], lhsT=hm[:, kc, tt * 128:(tt + 1) * 128],
                                    rhs=w2t[:, kc, n * 512:(n + 1) * 512], start=(kc == 0), stop=(kc == 3)), [hmk, w2k], pyk)
                        eng_ = V if tt % 4 != 3 else G
                        if ex == 0:
                            P.op(V, lambda e, tt=tt, py=py, ex=ex: e.tensor_scalar_mul(out=acc[:, tt, :], in0=py, scalar1=gts[:, tt, ex:ex + 1]),
                                 pyk + [gtsk], [acck + str(tt)])
                        else:
                            P.op(V, lambda e, tt=tt, py=py, ex=ex: e.scalar_tensor_tensor(
                                out=acc[:, tt, :], in0=py, scalar=gts[:, tt, ex:ex + 1], in1=acc[:, tt, :], op0=ALU.mult, op1=ALU.add),
                                 pyk + [gtsk, acck + str(tt)], [acck + str(tt)])
                for tt in range(ntt):
                    t0 = g0 + tt * 128
                    s_ = 0 if t0 < S else 1
                    P.dma(xt[:], XA[t0:t0 + 128, :], reads=["XA"], writes=[xk])
                    P.op(G, lambda e, tt=tt, s_=s_: e.tensor_tensor(out=z[:], in0=acc[:, tt, :], in1=g5[s_][0][:], op=ALU.mult),
                         [acck + str(tt), g5[s_][1]], [zk])
                    P.op(V, lambda e: e.scalar_tensor_tensor(out=z[:], in0=xt[:], scalar=DN_ALPHA, in1=z[:], op0=ALU.mult, op1=ALU.add),
                         [xk, zk], [zk])
                    layer_norm_tile(small, z, zk, lng, lngk, lnb, lnbk, xo, xok)
                    dst = dst_fn(t0)
                    P.dma(dst, xo[:], reads=[xok], writes=["XB"], eng="gpsimd", final=final)
        P.barrier()

    def x0_src(t0, s_):
        return x_d[t0:t0 + 128, :] if s_ == 0 else ctx_d[t0 - S:t0 - S + 128, :]

    with scope() as st:
        win, wink = sb(st, "win", [128, 8, 2432], BF16)
        for kc in range(8):
            P.dma(win[:, kc, :], e_w_in_d[kc * 128:(kc + 1) * 128, :], writes=[wink], eng="sync" if kc % 2 else "gpsimd")
        bcol, bcolk = sb(st, "bcol", [128, 19])
        fm_cols = [(i * 128) for i in range(13)] + [1792 + i * 128 for i in range(5)]
        for j, c0 in enumerate(fm_cols):
            col_dma(bcol[:, j:j + 1], e_b_in_d[c0:c0 + 128].rearrange("(p o) -> p o", o=1), [bcolk])
        bv, bvk = load_bc(st, "bv", e_b_in_d[1664:1792], 128)
        xt_r = ring(st, "xt", [128, D])
        hT_r = ring(st, "hT", [128, 8, 512], BF16)
        gtmp_r = ring(st, "gtmp", [128, 512], BF16)
        utmp_r = ring(st, "utmp", [128, 512])
        cosT, cosk = sb(st, "cosT", [128, 512])
        sinT, sink = sb(st, "sinT", [128, 512])
        q1_r = ring(st, "q1", [128, 512])
        q2_r = ring(st, "q2", [128, 512])
        qo_r = ring(st, "qo", [128, 512], BF16, n=3)
        vt_r = ring(st, "vt", [128, 2, 65], BF16, init=1.0)
        groups = [(g0, 0, min(512, S - g0)) for g0 in range(0, S, 512)] + [(S + g0, 1, min(512, C - g0)) for g0 in range(0, C, 512)]
        for (g0, s_, ng) in groups:
            hT, hTk = hT_r()
            for tt in range(ng // 128):
                xt, xk = xt_r()
                P.dma(xt[:], x0_src(g0 + tt * 128, s_), writes=[xk], eng="sync")
                transpose_mod(xt, xk, hT, hTk, mcol[:, 0, s_, 1, :], mcol[:, 0, s_, 0, :], mcol_k, tt * 128)
            if k.debug_barrier:
                P.barrier()
            if s_ == 0:
                P.dma(cosT[:, 0:ng], rope_e_d[0, :, g0:g0 + ng], writes=[cosk], eng="gpsimd")
                P.dma(sinT[:, 0:ng], rope_e_d[1, :, g0:g0 + ng], writes=[sink], eng="gpsimd")

            def mm_chunk(c0, pb, pk):
                for kc in range(8):
                    P.op(TE, lambda e, kc=kc: e.matmul(pb[:, 0:ng], lhsT=win[:, kc, c0:c0 + 128], rhs=hT[:, kc, 0:ng],
                                                       start=(kc == 0), stop=(kc == 7)), [wink, hTk], pk)
            for j in range(4):
                pb, pk = pbank()
                mm_chunk(j * 128, pb, pk)
                gtmp, gtmpk = gtmp_r()
                P.op(A, lambda e, pb=pb, j=j: e.activation(out=gtmp[:, 0:ng], in_=pb[:, 0:ng], func=AF.Gelu_apprx_tanh,
                                                          bias=bcol[:, j:j + 1], scale=1.0), pk + [bcolk], [gtmpk])
                P.dma(G_s[j * 128:(j + 1) * 128, g0:g0 + ng], gtmp[:, 0:ng], reads=[gtmpk], writes=["G_s"], eng="gpsimd")
            for j in range(4):
                pb, pk = pbank()
                mm_chunk(512 + j * 128, pb, pk)
                utmp, utmpk = utmp_r()
                P.op(A, lambda e, pb=pb, j=j: e.activation(out=utmp[:, 0:ng], in_=pb[:, 0:ng], func=AF.Identity,
                                                          bias=bcol[:, 4 + j:5 + j], scale=1.0), pk + [bcolk], [utmpk])
                P.dma(U_s[j * 128:(j + 1) * 128, g0:g0 + ng], utmp[:, 0:ng], reads=[utmpk], writes=["U_s"], eng="gpsimd")
            for j in range(5):
                pb, pk = pbank()
                mm_chunk(1024 + j * 128, pb, pk)
                q1, q1k = q1_r()
                q2, q2k = q2_r()
                qo, qok = qo_r()
                if s_ == 0:
                    pr, prk = pbank()
                    mm_chunk(1792 + j * 128, pr, prk)
                    P.op(V, lambda e, pb=pb, j=j: e.scalar_tensor_tensor(out=q1[:, 0:ng], in0=pb[:, 0:ng], scalar=bcol[:, 8 + j:9 + j],
                                                                         in1=cosT[:, 0:ng], op0=ALU.add, op1=ALU.mult),
                         pk + [bcolk, cosk], [q1k])
                    P.op(V, lambda e, pr=pr, j=j: e.scalar_tensor_tensor(out=q2[:, 0:ng], in0=pr[:, 0:ng], scalar=bcol[:, 13 + j:14 + j],
                                                                         in1=sinT[:, 0:ng], op0=ALU.add, op1=ALU.mult),
                         prk + [bcolk, sink], [q2k])
                    P.op(G, lambda e: e.tensor_tensor(out=qo[:, 0:ng], in0=q1[:, 0:ng], in1=q2[:, 0:ng], op=ALU.add), [q1k, q2k], [qok])
                else:
                    P.op(A, lambda e, pb=pb, j=j: e.activation(out=qo[:, 0:ng], in_=pb[:, 0:ng], func=AF.Identity,
                                                              bias=bcol[:, 8 + j:9 + j], scale=1.0), pk + [bcolk], [qok])
                if j < 4:
                    dst = QT_s[2 * j:2 * j + 2, :, g0:g0 + ng].rearrange("h d t -> (h d) t")
                else:
                    dst = KT_s[:, :, g0:g0 + ng].rearrange("h d t -> (h d) t")
                P.dma(dst, qo[:, 0:ng], reads=[qok], writes=["QKT"], eng="gpsimd")
            for tt in range(ng // 128):
                vt, vtk = vt_r()
                pb, pk = pbank()
                for kc in range(8):
                    P.op(TE, lambda e, kc=kc, tt=tt, pb=pb: e.matmul(pb[:, 0:128], lhsT=hT[:, kc, tt * 128:(tt + 1) * 128],
                                                                     rhs=win[:, kc, 1664:1792], start=(kc == 0), stop=(kc == 7)),
                         [wink, hTk], pk)
                P.op(V, lambda e, pb=pb: e.tensor_tensor(out=vt[:, :, 0:64], in0=pb[:, 0:128].rearrange("p (h d) -> p h d", h=2),
                                                         in1=bv[:].rearrange("p (h d) -> p h d", h=2), op=ALU.add), pk + [bvk], [vtk])
                t0 = g0 + tt * 128
                P.dma(V_s[t0:t0 + 128], vt[:], reads=[vtk], writes=["V_s"], eng="gpsimd")
    P.barrier()
    if stop_after == "P1":
        return finish(k, st_all)

    SEG = 1024 if S % 1024 == 0 else 512
    with scope() as st:
        wbd32, wbd32k = sb(st, "wbd32", [128, 16, 128])
        wbd, wbdk = sb(st, "wbd", [128, 16, 128], BF16)
        P.op(V, lambda e: e.memset(wbd32[:], 0.0), [], [wbd32k])
        for d_ in range(2):
            for cc in range(4):
                for ai, wd in enumerate((e_wa_d, e_wx_d)):
                    ix = (d_ * 4 + cc) * 2 + ai
                    for hb in range(2):
                        P.dma(wbd32[hb * 64:(hb + 1) * 64, ix, hb * 64:(hb + 1) * 64], wd[d_, 2 * cc + hb], writes=[wbd32k], eng="gpsimd")
        P.op(V, lambda e: e.tensor_copy(out=wbd[:], in_=wbd32[:]), [wbd32k], [wbdk])
        cols, colsk = sb(st, "cols", [128, 64])
        col_dma(cols[:, 0:8], e_lam_d.rearrange("d (c p) -> p (d c)", p=128), [colsk])
        col_dma(cols[:, 8:16], e_ba_d.rearrange("d (c p) -> p (d c)", p=128), [colsk])
        col_dma(cols[:, 16:24], e_bx_d.rearrange("d (c p) -> p (d c)", p=128), [colsk])
        col_dma(cols[:, 24:28], e_conv_b_d.rearrange("(c p) -> p c", p=128), [colsk])
        for cc in range(4):
            col_dma(cols[:, 28 + 4 * cc:32 + 4 * cc], e_conv_w_d[:, cc * 128:(cc + 1) * 128].rearrange("k p -> p k"), [colsk])
        P.op(A, lambda e: e.activation(out=cols[:, 44:52], in_=cols[:, 0:8], func=AF.Exp, scale=-1.0), [colsk], [colsk])
        P.op(A, lambda e: e.activation(out=cols[:, 44:52], in_=cols[:, 44:52], func=AF.Ln, bias=1.0, scale=1.0), [colsk], [colsk])
        P.op(V, lambda e: e.tensor_scalar_mul(out=cols[:, 52:60], in0=cols[:, 44:52], scalar1=-16.0), [colsk], [colsk])
        P.op(V, lambda e: e.tensor_scalar_mul(out=cols[:, 44:52], in0=cols[:, 44:52], scalar1=-8.0), [colsk], [colsk])
        rings_ = {nm: ring(st, nm, [128, SEG + (3 if nm == "uh" else 0)], BF16 if nm in ("ucb", "gg", "mx") else F32)
                  for nm in ("uh", "uc", "ucb", "r", "i", "a", "b", "h", "hf", "gg", "mx")}
        state, statek = sb(st, "state", [128, 1])
        segs_l = [(s0, min(SEG, S - s0), 0) for s0 in range(0, S, SEG)]
        seg_c = (S, C, 1)
        for cc in range(4):
            rows = slice(cc * 128, (cc + 1) * 128)
            for d_ in range(2):
                order = [seg_c] + (segs_l if d_ == 0 else segs_l[::-1])
                P.op(V, lambda e: e.memset(state[:], 0.0), [], [statek])
                for (s0, n, s_) in order:
                    uh, uhk = rings_["uh"](); uc, uck = rings_["uc"](); ucb, ucbk = rings_["ucb"](); r_, rk = rings_["r"]()
                    i_, ik = rings_["i"](); a_, ak = rings_["a"](); b_, bk = rings_["b"](); h_, hk = rings_["h"]()
                    hf, hfk = rings_["hf"](); gg, ggk = rings_["gg"](); mx, mxk = rings_["mx"]()
                    lo = S if s_ == 1 else 0
                    hi = T if s_ == 1 else S
                    a0, a1 = max(lo, s0 - 1), min(hi, s0 + n + 2)
                    P.op(V, lambda e: e.memset(uh[:], 0.0), [], [uhk])
                    P.dma(uh[:, a0 - (s0 - 1):a1 - (s0 - 1)], U_s[rows, a0:a1], reads=["U_s"], writes=[uhk])
                    cw = 28 + 4 * cc
                    P.op(V, lambda e, n=n, cw=cw, cc=cc: e.tensor_scalar(out=uc[:, 0:n], in0=uh[:, 0:n], scalar1=cols[:, cw:cw + 1],
                                                                        scalar2=cols[:, 24 + cc:25 + cc], op0=ALU.mult, op1=ALU.add),
                         [uhk, colsk], [uck])
                    for kk in range(1, 4):
                        P.op(V, lambda e, n=n, cw=cw, kk=kk: e.scalar_tensor_tensor(out=uc[:, 0:n], in0=uh[:, kk:kk + n],
                                                                                    scalar=cols[:, cw + kk:cw + kk + 1], in1=uc[:, 0:n],
                                                                                    op0=ALU.mult, op1=ALU.add), [uhk, colsk, uck], [uck])
                    P.op(A, lambda e, n=n: e.copy(out=ucb[:, 0:n], in_=uc[:, 0:n]), [uck], [ucbk])
                    dc = d_ * 4 + cc
                    for n0 in range(0, n, 512):
                        nn = min(512, n - n0)
                        pa, pak = pbank()
                        px, pxk = pbank()
                        P.op(TE, lambda e, pa=pa, n0=n0, nn=nn, dc=dc: e.matmul(pa[:, 0:nn], lhsT=wbd[:, dc * 2, :], rhs=ucb[:, n0:n0 + nn],
                                                                                start=True, stop=True), [wbdk, ucbk], pak)
                        P.op(TE, lambda e, px=px, n0=n0, nn=nn, dc=dc: e.matmul(px[:, 0:nn], lhsT=wbd[:, dc * 2 + 1, :], rhs=ucb[:, n0:n0 + nn],
                                                                                start=True, stop=True), [wbdk, ucbk], pxk)
                        P.op(A, lambda e, pa=pa, n0=n0, nn=nn, dc=dc: e.activation(out=r_[:, n0:n0 + nn], in_=pa[:, 0:nn], func=AF.Sigmoid,
                                                                                   bias=cols[:, 8 + dc:9 + dc], scale=1.0), pak + [colsk], [rk])
                        P.op(A, lambda e, px=px, n0=n0, nn=nn, dc=dc: e.activation(out=i_[:, n0:n0 + nn], in_=px[:, 0:nn], func=AF.Sigmoid,
                                                                                   bias=cols[:, 16 + dc:17 + dc], scale=1.0), pxk + [colsk], [ik])
                    P.op(A, lambda e, n=n, dc=dc: e.activation(out=a_[:, 0:n], in_=r_[:, 0:n], func=AF.Exp, scale=cols[:, 44 + dc:45 + dc]),
                         [rk, colsk], [ak])
                    P.op(A, lambda e, n=n, dc=dc: e.activation(out=b_[:, 0:n], in_=r_[:, 0:n], func=AF.Exp, scale=cols[:, 52 + dc:53 + dc]),
                         [rk, colsk], [bk])
                    P.op(V, lambda e, n=n: e.tensor_scalar(out=b_[:, 0:n], in0=b_[:, 0:n], scalar1=-1.0, scalar2=1.0, op0=ALU.mult, op1=ALU.add),
                         [bk], [bk])
                    P.op(A, lambda e, n=n: e.sqrt(out=b_[:, 0:n], in_=b_[:, 0:n]), [bk], [bk])
                    P.op(V, lambda e, n=n: e.tensor_tensor(out=b_[:, 0:n], in0=b_[:, 0:n], in1=i_[:, 0:n], op=ALU.mult), [bk, ik], [bk])
                    P.op(G, lambda e, n=n: e.tensor_tensor(out=b_[:, 0:n], in0=b_[:, 0:n], in1=uc[:, 0:n], op=ALU.mult), [bk, uck], [bk])
                    if d_ == 0:
                        P.op(V, lambda e, n=n: e.tensor_tensor_scan(out=h_[:, 0:n], data0=a_[:, 0:n], data1=b_[:, 0:n], initial=state[:, 0:1],
                                                                    op0=ALU.mult, op1=ALU.add), [ak, bk, statek], [hk])
                        P.op(V, lambda e, n=n: e.tensor_copy(out=state[:], in_=h_[:, n - 1:n]), [hk], [statek])
                        P.dma(HF_s[rows, s0:s0 + n], h_[:, 0:n], reads=[hk], writes=["HF_s"], eng="gpsimd")
                    else:
                        P.op(V, lambda e, n=n: e.tensor_tensor_scan(out=h_[:, 0:n][:, ::-1], data0=a_[:, 0:n][:, ::-1], data1=b_[:, 0:n][:, ::-1],
                                                                    initial=state[:, 0:1], op0=ALU.mult, op1=ALU.add), [ak, bk, statek], [hk])
                        P.op(V, lambda e: e.tensor_copy(out=state[:], in_=h_[:, 0:1]), [hk], [statek])
                        P.dma(hf[:, 0:n], HF_s[rows, s0:s0 + n], reads=["HF_s"], writes=[hfk])
                        P.dma(gg[:, 0:n], G_s[rows, s0:s0 + n], reads=["G_s"], writes=[ggk])
                        P.op(G, lambda e, n=n: e.tensor_tensor(out=hf[:, 0:n], in0=hf[:, 0:n], in1=h_[:, 0:n], op=ALU.add), [hfk, hk], [hfk])
                        P.op(V, lambda e, n=n: e.tensor_tensor(out=mx[:, 0:n], in0=hf[:, 0:n], in1=gg[:, 0:n], op=ALU.mult), [hfk, ggk], [mxk])
                        P.dma(MIXA[rows, s0:s0 + n], mx[:, 0:n], reads=[mxk], writes=["MIXA"], eng="gpsimd")
    P.barrier()
    if stop_after == "P2":
        return finish(k, st_all)

    def attn_block(qT_ap, qk, keyts, nsub, dv, scale, pT, pTk, on_out):
        nk = len(keyts)
        nq = nsub * 128
        for i, (kT_ap, kkeys, v_ap, vkeys, mask) in enumerate(keyts):
            pb, pk = pbank()
            P.op(TE, lambda e, pb=pb, kT_ap=kT_ap: e.matmul(pb[:, 0:nq], lhsT=kT_ap, rhs=qT_ap, start=True, stop=True), kkeys + qk, pk)
            P.op(A, lambda e, pb=pb, i=i: e.activation(out=pT[:, i, 0:nq], in_=pb[:, 0:nq], func=AF.Exp, scale=scale), pk, [pTk + str(i)])
            if mask is not None:
                P.op(G, lambda e, i=i, mask=mask: e.tensor_tensor(out=pT[:, i, 0:nq], in0=pT[:, i, 0:nq], in1=mask[0][:, 0:nq], op=ALU.mult),
                     [pTk + str(i), mask[1]], [pTk + str(i)])
        for sub in range(nsub):
            po, pok = pbank()
            for i, (kT_ap, kkeys, v_ap, vkeys, mask) in enumerate(keyts):
                P.op(TE, lambda e, po=po, i=i, sub=sub, v_ap=v_ap: e.matmul(po[:, 0:dv + 1], lhsT=pT[:, i, sub * 128:(sub + 1) * 128], rhs=v_ap,
                                                                           start=(i == 0), stop=(i == nk - 1)), [pTk + str(i)] + vkeys, pok)
            on_out(sub, po[:, 0:dv + 1], pok)

    with scope() as st:
        es, esk = load_bc(st, "es", e_sink_d, 8)
        P.op(A, lambda e: e.activation(out=es[:], in_=es[:], func=AF.Exp), [esk], [esk])
        mprev, mprevk = sb(st, "mprev", [128, 512], BF16)
        mnext, mnextk = sb(st, "mnext", [128, 512], BF16)
        P.dma(mprev[:], mask_d[0], writes=[mprevk], eng="gpsimd")
        P.dma(mnext[:], mask_d[1], writes=[mnextk], eng="gpsimd")
        kt_sb, ktk = sb(st, "kt_sb", [64, T], BF16)
        v_sb, vk_ = sb(st, "v_sb", [128, T // 128, 65], BF16)
        qt_sb, qtk = sb(st, "qt_sb", [64, 4, 128], BF16)
        pT, pTk = sb(st, "pT", [128, 5, 512], BF16)
        den, denk = sb(st, "den", [128, 8])
        att, attk = sb(st, "att", [128, 256])
        for j in range(2):
            P.dma(kt_sb[:], KT_s[j], reads=["QKT"], writes=[ktk])
            P.dma(v_sb[:], V_s[:, j, :].rearrange("(a p) d -> p a d", p=128), reads=["V_s"], writes=[vk_], eng="gpsimd")
            for (t0, s_) in tok_tiles():
                P.dma(qt_sb[:], QT_s[4 * j:4 * j + 4, :, t0:t0 + 128].rearrange("h d t -> d h t"), reads=["QKT"], writes=[qtk])
                keyts = []

                def kt(tile_idx, mask):
                    keyts.append((kt_sb[:, tile_idx * 128:(tile_idx + 1) * 128], [ktk], v_sb[:, tile_idx, :], [vk_], mask))
                if s_ == 0:
                    n = t0 // 128
                    if n > 0:
                        kt(n - 1, (mprev, mprevk))
                    kt(n, None)
                    if n < NT_L - 1:
                        kt(n + 1, (mnext, mnextk))
                for c_ in range(NT_C):
                    kt(NT_L + c_, None)

                def on_out(sub, po, pok):
                    hh = 4 * j + sub
                    P.op(V, lambda e: e.tensor_tensor(out=den[:, 0:1], in0=po[:, 64:65], in1=es[:, hh:hh + 1], op=ALU.add), pok + [esk], [denk])
                    P.op(V, lambda e: e.reciprocal(out=den[:, 1:2], in_=den[:, 0:1]), [denk], [denk])
                    P.op(A, lambda e: e.activation(out=att[:, sub * 64:(sub + 1) * 64], in_=po[:, 0:64], func=AF.Identity, scale=den[:, 1:2]),
                         pok + [denk], [attk])
                attn_block(qt_sb[:].rearrange("d h t -> d (h t)"), [qtk], keyts, 4, 64, 0.125, pT, pTk, on_out)
                P.dma(ATT[t0:t0 + 128, j * 256:(j + 1) * 256], att[:], reads=[attk], writes=["ATT"], eng="gpsimd")
    P.barrier()
    if stop_after == "P3":
        return finish(k, st_all)

    def mix_loader_even(t0, s_, mixT, mixk, st):
        if "att" not in st.ld:
            st.ld["att"] = sb(st, "attin", [128, 512])
        att_in, attink = st.ld["att"]
        P.dma(mixT[:, 0:4, :], MIXA[:, t0:t0 + 128].rearrange("(c p) t -> p c t", p=128), reads=["MIXA"], writes=[mixk])
        P.dma(att_in[:], ATT[t0:t0 + 128, :], reads=["ATT"], writes=[attink], eng="gpsimd")
        pb, pk = pbank()
        for c_ in range(4):
            P.op(TE, lambda e, c_=c_, pb=pb: e.transpose(pb[:, c_ * 128:(c_ + 1) * 128], att_in[:, c_ * 128:(c_ + 1) * 128], ident[:]),
                 [attink, ident_k], pk)
        P.op(V, lambda e, pb=pb: e.tensor_copy(out=mixT[:, 4:8, :], in_=pb[:, 0:512].rearrange("p (c t) -> p c t", c=4)), pk, [mixk])

    mixer_epilogue(0, e_w_out_d, e_b_out_d, x0_src, mix_loader_even, True)
    if stop_after == "P4":
        return finish(k, st_all)
    moe_phase(0, True, lambda t0: XB[t0:t0 + 128, :], False)
    if stop_after == "P5":
        return finish(k, st_all)

    with scope() as st:
        NA = SH // 128
        xi_f, xik = sb(st, "xi", [128, NA])
        xi = xi_f.bitcast(I32)
        col_dma(xi, xh_idx_d.rearrange("(a p) -> p a", p=128), [xik])
        xg = [sb(st, f"xg{i}", [128, D]) for i in range(2)]
        for a in range(NA):
            gt_, gk_ = xg[a % 2]
            P.op("gpsimd", lambda e, a=a, gt_=gt_: e.indirect_dma_start(
                out=gt_[:, :], out_offset=None, in_=XBp[:, :],
                in_offset=bass.IndirectOffsetOnAxis(ap=xi[:, a:a + 1], axis=0),
                bounds_check=128 + T - 1, oob_is_err=False), [xik, "XB", "XBpad"], [gk_], dma=True)
            P.dma(XH[a * 128:(a + 1) * 128, :], gt_[:], reads=[gk_], writes=["XH"], eng="sync")
    P.barrier()

    def x1_src(t0, s_):
        return XH[64 + t0:64 + t0 + 128, :]

    MSCALE = 96.0 ** -0.5
    with scope() as st:
        owin, owink = sb(st, "owin", [128, 8, 1440], BF16)
        for kc in range(8):
            P.dma(owin[:, kc, :], o_w_in_d[kc * 128:(kc + 1) * 128, :], writes=[owink])
        wkr, wkrk = sb(st, "wkr", [128, 8, 32], BF16)
        P.dma(wkr[:], o_w_kpe_rot_d.rearrange("(kc p) n -> p kc n", p=128), writes=[wkrk])
        wuq, wuqk = sb(st, "wuq", [128, 2, 8, 192], BF16)
        P.dma(wuq[:], o_w_uq_d.rearrange("(kc p) h n -> p kc h n", p=128), writes=[wuqk])
        wuk, wukk = sb(st, "wuk", [128, 512], BF16)
        P.dma(wuk[:], o_w_uk_d, writes=[wukk])
        wuv, wuvk = sb(st, "wuv", [128, 512], BF16)
        P.dma(wuv[:], o_w_uv_d, writes=[wuvk])
        bq, bqk = load_bc(st, "bq", o_b_in_d[0:384], 384)
        qn, qnk = load_bc(st, "qn", o_q_norm_d, 256)
        kvn, kvnk = load_bc(st, "kvn", o_kv_norm_d, 128)
        zmk, zmkk = load_bc(st, "zmk", zmask_d, SH)
        oc, ock = sb(st, "oc", [128, 12])
        col_dma(oc[0:32, 0:1], o_b_in_d[384:416].rearrange("(p o) -> p o", o=1), [ock])
        col_dma(oc[0:32, 1:2], o_b_kpe_rot_d.rearrange("(p o) -> p o", o=1), [ock])
        col_dma(oc[:, 2:10], o_b_in_d[416:1440].rearrange("(c p) -> p c", p=128), [ock])
        xt_r = ring(st, "xt", [128, D])
        hT_r = ring(st, "hT", [128, 8, 512], BF16)
        tq_r = ring(st, "tq", [128, 384])
        sq_r = ring(st, "sq", [128, 384])
        rs_r = ring(st, "rs", [128, 8])
        cqnT_r = ring(st, "cqnT", [128, 2, 512], BF16)
        ckvT_r = ring(st, "ckvT", [128, 512], BF16)
        cos96, cos96k = sb(st, "cos96", [96, 512])
        sin96, sin96k = sb(st, "sin96", [96, 512])
        cos32, cos32k = sb(st, "cos32", [32, 512])
        sin32, sin32k = sb(st, "sin32", [32, 512])
        f1_r = ring(st, "f1", [128, 512])
        f2_r = ring(st, "f2", [128, 512])
        ob_r = ring(st, "ob", [128, 512], BF16, n=3)
        vt_r = ring(st, "vt1", [128, 8, 65], BF16, init=1.0)

        def norm_part(c0, c1, rcol, ncol, nbc, nbck):
            w = c1 - c0
            P.op(A, lambda e: e.activation(out=sq[:, c0:c1], in_=tq[:, c0:c1], func=AF.Square, accum_out=rs[:, rcol:rcol + 1]), [tqk], [sqk, rsk])
            P.op(V, lambda e: e.tensor_scalar(out=rs[:, rcol + 2:rcol + 3], in0=rs[:, rcol:rcol + 1], scalar1=1.0 / w, scalar2=LN_EPS,
                                              op0=ALU.mult, op1=ALU.add), [rsk], [rsk])
            P.op(A, lambda e: e.sqrt(out=rs[:, rcol + 4:rcol + 5], in_=rs[:, rcol + 2:rcol + 3]), [rsk], [rsk])
            P.op(V, lambda e: e.reciprocal(out=rs[:, rcol + 6:rcol + 7], in_=rs[:, rcol + 4:rcol + 5]), [rsk], [rsk])
            P.op(V, lambda e: e.scalar_tensor_tensor(out=sq[:, c0:c1], in0=tq[:, c0:c1], scalar=rs[:, rcol + 6:rcol + 7], in1=nbc[:],
                                                     op0=ALU.mult, op1=ALU.mult), [tqk, rsk, nbck], [sqk])

        groups = [(g0, 0, min(512, S - g0)) for g0 in range(0, S, 512)] + [(S + g0, 1, min(512, C - g0)) for g0 in range(0, C, 512)]
        for (g0, s_, ng) in groups:
            hT, hTk = hT_r()
            ckvT, ckvTk = ckvT_r()
            for tt in range(ng // 128):
                t0 = g0 + tt * 128
                xt, xk = xt_r()
                tq, tqk = tq_r()
                sq, sqk = sq_r()
                rs, rsk = rs_r()
                vt, vtk = vt_r()
                P.dma(xt[:], XB[t0:t0 + 128, :], reads=["XB"], writes=[xk])
                transpose_mod(xt, xk, hT, hTk, mcol[:, 1, s_, 1, :], mcol[:, 1, s_, 0, :], mcol_k, tt * 128)
                pb, pk = pbank()
                for kc in range(8):
                    P.op(TE, lambda e, kc=kc: e.matmul(pb[:, 0:128], lhsT=hT[:, kc, tt * 128:(tt + 1) * 128], rhs=owin[:, kc, 256:384],
                                                       start=(kc == 0), stop=(kc == 7)), [hTk, owink], pk)
                P.op(V, lambda e: e.tensor_tensor(out=tq[:, 256:384], in0=pb[:, 0:128], in1=bq[:, 256:384], op=ALU.add), pk + [bqk], [tqk])
                norm_part(256, 384, 1, 128, kvn, kvnk)
                pt, ptk = pbank()
                P.op(TE, lambda e: e.transpose(pt[:, 0:128], sq[:, 256:384], ident[:]), [sqk, ident_k], ptk)
                P.op(A, lambda e: e.copy(out=ckvT[:, tt * 128:(tt + 1) * 128], in_=pt[:, 0:128]), ptk, [ckvTk])
                pv, pvk = pbank()
                P.op(TE, lambda e: e.matmul(pv[:, 0:512], lhsT=ckvT[:, tt * 128:(tt + 1) * 128], rhs=wuv[:], start=True, stop=True), [ckvTk, wuvk], pvk)
                P.op(V, lambda e: e.tensor_copy(out=vt[:, :, 0:64], in_=pv[:, 0:512].rearrange("p (h d) -> p h d", h=8)), pvk, [vtk])
                P.dma(VM[t0:t0 + 128], vt[:], reads=[vtk], writes=["VM"], eng="gpsimd")
            for c_ in range(4):
                pkn, pknk = pbank()
                ob, obk = ob_r()
                P.op(TE, lambda e, c_=c_: e.matmul(pkn[:, 0:ng], lhsT=wuk[:, c_ * 128:(c_ + 1) * 128], rhs=ckvT[:, 0:ng], start=True, stop=True),
                     [wukk, ckvTk], pknk)
                P.op(A, lambda e: e.copy(out=ob[:, 0:ng], in_=pkn[:, 0:ng]), pknk, [obk])
                for hh in range(2):
                    P.dma(KM[2 * c_ + hh, 0:64, g0:g0 + ng], ob[hh * 64:(hh + 1) * 64, 0:ng], reads=[obk], writes=["KM"], eng="gpsimd")
            pp, ppk = pbank()
            ob, obk = ob_r()
            f1, f1k = f1_r()
            f2, f2k = f2_r()
            for kc in range(8):
                P.op(TE, lambda e, kc=kc: e.matmul(pp[0:32, 0:ng], lhsT=owin[:, kc, 384:416], rhs=hT[:, kc, 0:ng], start=(kc == 0), stop=(kc == 7)),
                     [owink, hTk], ppk)
            if s_ == 0:
                P.dma(cos32[:, 0:ng], rope_k_d[0, :, g0:g0 + ng], writes=[cos32k])
                P.dma(sin32[:, 0:ng], rope_k_d[1, :, g0:g0 + ng], writes=[sin32k])
                pr, prk = pbank()
                for kc in range(8):
                    P.op(TE, lambda e, kc=kc: e.matmul(pr[0:32, 0:ng], lhsT=wkr[:, kc, :], rhs=hT[:, kc, 0:ng], start=(kc == 0), stop=(kc == 7)),
                         [wkrk, hTk], prk)
                P.op(V, lambda e: e.scalar_tensor_tensor(out=f1[0:32, 0:ng], in0=pp[0:32, 0:ng], scalar=oc[0:32, 0:1], in1=cos32[:, 0:ng],
                                                         op0=ALU.add, op1=ALU.mult), ppk + [ock, cos32k], [f1k])
                P.op(V, lambda e: e.scalar_tensor_tensor(out=f2[0:32, 0:ng], in0=pr[0:32, 0:ng], scalar=oc[0:32, 1:2], in1=sin32[:, 0:ng],
                                                         op0=ALU.add, op1=ALU.mult), prk + [ock, sin32k], [f2k])
                P.op(G, lambda e: e.tensor_tensor(out=ob[0:32, 0:ng], in0=f1[0:32, 0:ng], in1=f2[0:32, 0:ng], op=ALU.add), [f1k, f2k], [obk])
            else:
                P.op(A, lambda e: e.activation(out=ob[0:32, 0:ng], in_=pp[0:32, 0:ng], func=AF.Identity, bias=oc[0:32, 0:1], scale=1.0),
                     ppk + [ock], [obk])
            for h in range(8):
                P.dma(KM[h, 64:96, g0:g0 + ng], ob[0:32, 0:ng], reads=[obk], writes=["KM"], eng="gpsimd")

        for g0 in range(0, SH, 512):
            ng = min(512, SH - g0)
            hT, hTk = hT_r()
            cqnT, cqnTk = cqnT_r()
            for tt in range(ng // 128):
                r0 = g0 + tt * 128
                xt, xk = xt_r()
                tq, tqk = tq_r()
                sq, sqk = sq_r()
                rs, rsk = rs_r()
                P.dma(xt[:], XH[r0:r0 + 128, :], reads=["XH"], writes=[xk])
                transpose_mod(xt, xk, hT, hTk, mcol[:, 1, 0, 1, :], mcol[:, 1, 0, 0, :], mcol_k, tt * 128)
                pb, pk = pbank()
                for kc in range(8):
                    P.op(TE, lambda e, kc=kc: e.matmul(pb[:, 0:256], lhsT=hT[:, kc, tt * 128:(tt + 1) * 128], rhs=owin[:, kc, 0:256],
                                                       start=(kc == 0), stop=(kc == 7)), [hTk, owink], pk)
                P.op(V, lambda e: e.tensor_tensor(out=tq[:, 0:256], in0=pb[:, 0:256], in1=bq[:, 0:256], op=ALU.add), pk + [bqk], [tqk])
                norm_part(0, 256, 0, 256, qn, qnk)
                pt, ptk = pbank()
                for c_ in range(2):
                    P.op(TE, lambda e, c_=c_: e.transpose(pt[:, c_ * 128:(c_ + 1) * 128], sq[:, c_ * 128:(c_ + 1) * 128], ident[:]), [sqk, ident_k], ptk)
                P.op(V, lambda e: e.tensor_copy(out=cqnT[:, :, tt * 128:(tt + 1) * 128], in_=pt[:, 0:256].rearrange("p (c t) -> p c t", c=2)),
                     ptk, [cqnTk])
            P.dma(cos96[:, 0:ng], rope_q_d[0, :, g0:g0 + ng], writes=[cos96k])
            P.dma(sin96[:, 0:ng], rope_q_d[1, :, g0:g0 + ng], writes=[sin96k])
            for h in range(8):
                pq, pqk = pbank()
                pr, prk = pbank()
                f1, f1k = f1_r()
                f2, f2k = f2_r()
                ob, obk = ob_r()
                for kc in range(2):
                    P.op(TE, lambda e, kc=kc: e.matmul(pq[0:96, 0:ng], lhsT=wuq[:, kc, h, 0:96], rhs=cqnT[:, kc, 0:ng], start=(kc == 0), stop=(kc == 1)),
                         [wuqk, cqnTk], pqk)
                for kc in range(2):
                    P.op(TE, lambda e, kc=kc: e.matmul(pr[0:96, 0:ng], lhsT=wuq[:, kc, h, 96:192], rhs=cqnT[:, kc, 0:ng], start=(kc == 0), stop=(kc == 1)),
                         [wuqk, cqnTk], prk)
                P.op(V, lambda e: e.tensor_tensor(out=f1[0:96, 0:ng], in0=pq[0:96, 0:ng], in1=cos96[:, 0:ng], op=ALU.mult), pqk + [cos96k], [f1k])
                P.op(V, lambda e: e.tensor_tensor(out=f2[0:96, 0:ng], in0=pr[0:96, 0:ng], in1=sin96[:, 0:ng], op=ALU.mult), prk + [sin96k], [f2k])
                P.op(G, lambda e: e.tensor_tensor(out=ob[0:96, 0:ng], in0=f1[0:96, 0:ng], in1=f2[0:96, 0:ng], op=ALU.add), [f1k, f2k], [obk])
                P.dma(QM[h, :, g0:g0 + ng], ob[0:96, 0:ng], reads=[obk], writes=["QM"], eng="gpsimd")
            for j in range(4):
                pa, pak = pbank()
                pg, pgk = pbank()
                f1, f1k = f1_r()
                f2, f2k = f2_r()
                ob, obk = ob_r()
                for kc in range(8):
                    P.op(TE, lambda e, kc=kc: e.matmul(pa[:, 0:ng], lhsT=owin[:, kc, 416 + j * 128:544 + j * 128], rhs=hT[:, kc, 0:ng],
                                                       start=(kc == 0), stop=(kc == 7)), [owink, hTk], pak)
                for kc in range(8):
                    P.op(TE, lambda e, kc=kc: e.matmul(pg[:, 0:ng], lhsT=owin[:, kc, 928 + j * 128:1056 + j * 128], rhs=hT[:, kc, 0:ng],
                                                       start=(kc == 0), stop=(kc == 7)), [owink, hTk], pgk)
                P.op(A, lambda e: e.activation(out=f1[:, 0:ng], in_=pg[:, 0:ng], func=AF.Sigmoid, bias=oc[:, 6 + j:7 + j], scale=1.0),
                     pgk + [ock], [f1k])
                P.op(V, lambda e: e.scalar_tensor_tensor(out=f2[:, 0:ng], in0=pa[:, 0:ng], scalar=oc[:, 2 + j:3 + j], in1=f1[:, 0:ng],
                                                         op0=ALU.add, op1=ALU.mult), pak + [ock, f1k], [f2k])
                P.op(G, lambda e: e.tensor_tensor(out=ob[:, 0:ng], in0=f2[:, 0:ng], in1=zmk[:, g0:g0 + ng], op=ALU.mult), [f2k, zmkk], [obk])
                P.dma(ZC[j * 128:(j + 1) * 128, g0:g0 + ng], ob[:, 0:ng], reads=[obk], writes=["ZC"], eng="gpsimd")
    P.barrier()
    if stop_after == "Q1":
        return finish(k, st_all)

    with scope() as st:
        identb, identbk = sb(st, "identb", [128, 128], BF16)
        P.op(V, lambda e: e.tensor_copy(out=identb[:], in_=ident[:]), [ident_k], [identbk])
        dwc, dwck = sb(st, "dwc", [128, 4, 32])
        for j in range(4):
            col_dma(dwc[:, j, 0:31], o_dw_w_d[:, j * 128:(j + 1) * 128].rearrange("k p -> p k"), [dwck])
        col_dma(dwc[:, :, 31], o_dw_b_d.rearrange("(c p) -> p c", p=128), [dwck])
        dg, dgk = sb(st, "dg", [128, 4, 31, 128], BF16)
        for j in range(4):
            for kk in range(31):
                P.op(V if kk % 2 else G, lambda e: e.tensor_scalar_mul(out=dg[:, j, kk, :], in0=identb[:], scalar1=dwc[:, j, kk:kk + 1]),
                     [identbk, dwck], [dgk])
        cg, cgk = load_bc(st, "cg", o_cln_g_d, 512)
        cb, cbk = load_bc(st, "cb", o_cln_b_d, 512)
        zw, zwk = sb(st, "zw", [128, 512 + 30], BF16)
        yc, yck = sb(st, "yc", [128, 4, 512])
        yt, ytk = sb(st, "yt", [128, 512])
        yo, yok = sb(st, "yo", [128, 512])
        small = {"stats": sb(st, "stats3", [128, 2, 6]), "mv": sb(st, "mv3", [128, 8])}
        for g0 in range(0, SQ, 512):
            ng = min(512, SQ - g0)
            for j in range(4):
                P.dma(zw[:, 0:ng + 30], ZC[j * 128:(j + 1) * 128, 49 + g0:49 + g0 + ng + 30], reads=["ZC"], writes=[zwk])
                pc, pck = pbank()
                for kk in range(31):
                    P.op(TE, lambda e, kk=kk: e.matmul(pc[:, 0:ng], lhsT=dg[:, j, kk, :], rhs=zw[:, kk:kk + ng], start=(kk == 0), stop=(kk == 30)),
                         [dgk, zwk], pck)
                P.op(A, lambda e: e.activation(out=yc[:, j, 0:ng], in_=pc[:, 0:ng], func=AF.Identity, bias=dwc[:, j, 31:32], scale=1.0),
                     pck + [dwck], [yck])
            for tt in range(ng // 128):
                pt, ptk = pbank()
                for j in range(4):
                    P.op(TE, lambda e, j=j: e.transpose(pt[:, j * 128:(j + 1) * 128], yc[:, j, tt * 128:(tt + 1) * 128], ident[:]), [yck, ident_k], ptk)
                P.op(V, lambda e: e.tensor_copy(out=yt[:], in_=pt[:, 0:512]), ptk, [ytk])
                layer_norm_tile(small, yt, ytk, cg, cgk, cb, cbk, yo, yok, width=512)
                P.op(A, lambda e: e.activation(out=yo[:], in_=yo[:], func=AF.Silu), [yok], [yok])
                t0 = g0 + tt * 128
                P.dma(CONV[t0:t0 + 128, :], yo[:], reads=[yok], writes=["CONV"], eng="gpsimd")
    P.barrier()
    if stop_after == "Q2":
        return finish(k, st_all)

    with scope() as st:
        NKT = T // 128
        km, kmk = sb(st, "km", [96, T], BF16)
        vm, vmk = sb(st, "vm", [128, NKT, 65], BF16)
        qm, qmk = sb(st, "qm", [96, 512], BF16)
        pT, pTk = sb(st, "pTm", [128, NKT, 512], BF16)
        den, denk = sb(st, "den1", [128, 2])
        att, attk = sb(st, "att1", [128, 64])
        for h in range(8):
            P.dma(km[:], KM[h], reads=["KM"], writes=[kmk])
            P.dma(vm[:], VM[:, h, :].rearrange("(a p) d -> p a d", p=128), reads=["VM"], writes=[vmk], eng="gpsimd")
            for g0 in range(0, SQ, 512):
                ng = min(512, SQ - g0)
                P.dma(qm[:, 0:ng], QM[h, :, 64 + g0:64 + g0 + ng], reads=["QM"], writes=[qmk])
                keyts = [(km[:, i * 128:(i + 1) * 128], [kmk], vm[:, i, :], [vmk], None) for i in range(NKT)]

                def on_out(sub, po, pok):
                    P.op(V, lambda e: e.reciprocal(out=den[:, 0:1], in_=po[:, 64:65]), pok, [denk])
                    P.op(A, lambda e: e.activation(out=att[:], in_=po[:, 0:64], func=AF.Identity, scale=den[:, 0:1]), pok + [denk], [attk])
                    t0 = g0 + sub * 128
                    P.dma(ATT[t0:t0 + 128, h * 64:(h + 1) * 64], att[:], reads=[attk], writes=["ATT"], eng="gpsimd")
                attn_block(qm[:, 0:ng], [qmk], keyts, ng // 128, 64, MSCALE, pT, pTk, on_out)
    P.barrier()
    if stop_after == "Q3":
        return finish(k, st_all)

    def mix_loader_odd(t0, s_, mixT, mixk, st):
        if "t" not in st.ld:
            st.ld["t"] = sb(st, "mixin", [128, D])
        mi, mik = st.ld["t"]
        P.dma(mi[:, 0:512], ATT[t0:t0 + 128, :], reads=["ATT"], writes=[mik])
        P.dma(mi[:, 512:1024], CONV[t0:t0 + 128, :], reads=["CONV"], writes=[mik], eng="gpsimd")
        pb, pk = pbank(2)
        for c_ in range(8):
            P.op(TE, lambda e, c_=c_: e.transpose(pb[:, c_ * 128:(c_ + 1) * 128], mi[:, c_ * 128:(c_ + 1) * 128], ident[:]), [mik, ident_k], pk)
        P.op(V, lambda e: e.tensor_copy(out=mixT[:, 0:4, :], in_=pb[:, 0:512].rearrange("p (c t) -> p c t", c=4)), pk, [mixk])
        P.op(A, lambda e: e.copy(out=mixT[:, 4:8, :], in_=pb[:, 512:1024].rearrange("p (c t) -> p c t", c=4)), pk, [mixk])

    mixer_epilogue(1, o_w_out_d, o_b_out_d, x1_src, mix_loader_odd, False, n_lat=SQ)
    if stop_after == "Q4":
        return finish(k, st_all)
    moe_phase(1, False, lambda t0: out_d[t0:t0 + 128, :], True, n_lat=SQ)
    return finish(k, st_all)


def finish(k, st_all):
    k.P.emit()
    st_all.close()
    return k


GRID_W = 64
ROPE_THETA = 10000.0


def _rot_perm(n_heads, hd):
    q = hd // 4
    idx = []
    for h in range(n_heads):
        b = h * hd
        idx += list(range(b + q, b + 2 * q)) + list(range(b, b + q)) + list(range(b + 3 * q, b + 4 * q)) + list(range(b + 2 * q, b + 3 * q))
    return np.array(idx)


def _rope_tables(S, hd, t=None):
    q = hd // 4
    if t is None:
        t = np.arange(S)
    S = len(t)
    row, col = t // GRID_W, t % GRID_W
    invf = ROPE_THETA ** (-np.arange(q, dtype=np.float64) / q)
    cos = np.zeros((hd, S)); sin = np.zeros((hd, S))
    for d in range(hd):
        pos = row if d < 2 * q else col
        dd = d % (2 * q)
        j = dd % q
        ang = pos.astype(np.float32).astype(np.float64) * np.float32(invf[j]).astype(np.float64)
        cos[d] = np.cos(ang)
        sin[d] = np.sin(ang) * (-1.0 if dd < q else 1.0)
    return cos.astype(np.float32), sin.astype(np.float32)


def prep_core_inputs(inp, b, S, h=0):
    f = lambda a: np.ascontiguousarray(np.asarray(a, dtype=np.float32))
    m = {}
    m["x"] = f(inp["x"][b])
    m["ctx"] = f(inp["ctx"][b])
    m["cvec"] = f(np.stack([inp["c"][b], inp["c_ctx"]]))
    m["ident"] = np.eye(128, dtype=np.float32)
    m["w_mod"] = f(inp["w_mod"]); m["b_mod"] = f(inp["b_mod"])
    m["ln_g"] = f(inp["ln_g"]); m["ln_b"] = f(inp["ln_b"])
    w_in = np.asarray(inp["e_w_in"][0]); b_in = np.asarray(inp["e_b_in"][0])
    perm = _rot_perm(10, 64) + 1024
    m["e_w_in"] = f(np.concatenate([w_in, w_in[:, perm]], axis=1))
    m["e_b_in"] = f(np.concatenate([b_in, b_in[perm]]))
    m["e_conv_w"] = f(inp["e_conv_w"][0]); m["e_conv_b"] = f(inp["e_conv_b"][0])
    m["e_lru_wa"] = f(inp["e_lru_wa"][0]); m["e_lru_ba"] = f(inp["e_lru_ba"][0])
    m["e_lru_wx"] = f(inp["e_lru_wx"][0]); m["e_lru_bx"] = f(inp["e_lru_bx"][0])
    m["e_lru_lambda"] = f(inp["e_lru_lambda"][0]); m["e_sink"] = f(inp["e_sink"][0])
    m["e_w_out"] = f(inp["e_w_out"][0]); m["e_b_out"] = f(inp["e_b_out"][0])
    c64, s64 = _rope_tables(S, 64)
    m["rope_e"] = f(np.stack([np.concatenate([c64, c64]), np.concatenate([s64, s64])]))
    j = np.arange(128)[:, None]; i = np.arange(128)[None, :]
    mp = (j >= i).astype(np.float32); mn = (j <= i).astype(np.float32)
    m["swa_mask"] = f(np.stack([np.tile(mp, (1, 4)), np.tile(mn, (1, 4))]))
    ow = np.asarray(inp["o_w_in"][0]); ob = np.asarray(inp["o_b_in"][0])
    m["o_w_in"] = f(ow); m["o_b_in"] = f(ob)
    m["o_q_norm"] = f(inp["o_q_norm"][0]); m["o_kv_norm"] = f(inp["o_kv_norm"][0])
    wuq = np.asarray(inp["o_w_uq"][0]).reshape(256, 8, 96)
    p32 = _rot_perm(1, 32)
    ext = np.zeros((256, 8, 192), np.float32)
    ext[:, :, 0:96] = wuq
    ext[:, :, 160:192] = wuq[:, :, 64:96][:, :, p32]
    m["o_w_uq"] = f(ext)
    m["o_w_uk"] = f(inp["o_w_uk"][0]); m["o_w_uv"] = f(inp["o_w_uv"][0])
    m["o_w_kpe_rot"] = f(ow[:, 384:416][:, p32]); m["o_b_kpe_rot"] = f(ob[384:416][p32])
    c32, s32 = _rope_tables(S, 32)
    m["rope_k"] = f(np.stack([c32, s32]))
    SQ = S // 2
    SH = SQ + 128
    tok = h * SQ - 64 + np.arange(SH)
    inside = (tok >= 0) & (tok < S)
    cq, sq_ = _rope_tables(S, 32, np.clip(tok, 0, S - 1))
    m["rope_q"] = f(np.stack([np.concatenate([np.ones((64, SH), np.float32), cq]), np.concatenate([np.zeros((64, SH), np.float32), sq_])]))
    m["zmask"] = f(inside.astype(np.float32))
    m["xh_idx"] = (64 + h * SQ + np.arange(SH)).astype(np.int32)
    m["o_dw_w"] = f(inp["o_dw_w"][0]); m["o_dw_b"] = f(inp["o_dw_b"][0])
    m["o_cln_g"] = f(inp["o_cln_g"][0]); m["o_cln_b"] = f(inp["o_cln_b"][0])
    m["o_w_out"] = f(inp["o_w_out"][0]); m["o_b_out"] = f(inp["o_b_out"][0])
    m["moe_w_gr"] = f(np.concatenate([inp["moe_w_group"], inp["moe_w_router"]], axis=2))
    m["moe_b_gr"] = f(np.concatenate([inp["moe_b_group"], inp["moe_b_router"]], axis=1))
    m["moe_w1"] = f(inp["moe_w1"]); m["moe_w3"] = f(inp["moe_w3"]); m["moe_w2"] = f(inp["moe_w2"])
    return m


_CACHE = {}


def kernel(**inputs):
    B, S, _ = inputs["x"].shape
    C = inputs["ctx"].shape[1]
    key = (S, C)
    if key not in _CACHE:
        _CACHE[key] = build(S, C)
    kk = _CACHE[key]
    shared = None
    in_maps = []
    for core in range(8):
        b, h = core % B, core // B
        m = prep_core_inputs(inputs, b, S, h)
        in_maps.append(m)
    res = run_bass_kernel_spmd(kk.nc, in_maps, core_ids=list(range(8)))
    SQ = S // 2
    out = np.empty((B, S, D), np.float32)
    for core in range(8):
        b, h = core % B, core // B
        out[b, h * SQ:(h + 1) * SQ] = np.asarray(res.results[core]["out"], dtype=np.float32)
    return out
```

```python
import contextlib
import numpy as np
import concourse.bass as bass
import concourse.mybir as mybir
from concourse.bass_utils import run_bass_kernel_spmd

F32 = mybir.dt.float32
BF16 = mybir.dt.bfloat16
I32 = mybir.dt.int32
AF = mybir.ActivationFunctionType
ALU = mybir.AluOpType
AX = mybir.AxisListType

D = 1024
SEM_LIMIT = 30000
DN_ALPHA = 4.0 ** 0.25
LN_EPS = 1e-6


class _Rec:
    def __getattr__(self, name):
        def f(*a, **kw):
            self.call = (name, a, kw)
            return self
        return f


class Prog:
    ENGS = ("tensor", "vector", "scalar", "gpsimd", "sync")

    def __init__(self, nc, n_dma_sems=14):
        self.nc = nc
        self.ops = {e: [] for e in self.ENGS}
        self.sems = {}
        self.sem_order = []
        self.cnt = {e: 0 for e in self.ENGS}
        self.epoch = {e: 0 for e in self.ENGS}
        self.known = {e: {} for e in self.ENGS}
        self.last_w = {}
        self.readers = {}
        self.dma_pool = {e: [[f"d_{e}_{i}", 0] for i in range(n_dma_sems)] for e in ("sync", "gpsimd", "scalar")}
        self.dma_rr = {e: 0 for e in ("sync", "gpsimd", "scalar")}
        self.out_tokens = []
        self.last_tok = {}
        self.nops = 0

    def _sem(self, name):
        if name not in self.sems:
            self.sems[name] = None
            self.sem_order.append(name)
        return name

    limit = None

    def op(self, eng, fn, reads=(), writes=(), dma=False, final=False, late=False):
        if Prog.limit is not None and self.nops >= Prog.limit:
            return None
        if eng == "gpsimd" and not dma:
            eng = "vector"
        pr = [b for b in reads if b.startswith("pb")]
        if pr:
            reads = [b for b in reads if not b.startswith("pb")]
            writes = list(writes) + pr
        deps = {}

        def need(tok):
            if tok is None:
                return
            s, v, e = tok
            if eng == "tensor" and e == "tensor" and not dma:
                return
            if deps.get(s, 0) < v:
                deps[s] = v

        for b in reads:
            need(self.last_w.get(b))
        for b in writes:
            need(self.last_w.get(b))
            for t in self.readers.get(b, ()):
                need(t)
        if dma:
            pool = self.dma_pool[eng]
            i = self.dma_rr[eng]
            self.dma_rr[eng] = (i + 1) % len(pool)
            ent = pool[i]
            if ent[1] > 0:
                need((ent[0], ent[1], "dma"))
            ent[1] += 16
            tok = (self._sem(ent[0]), ent[1], "dma")
            inc = (ent[0], 16)
            self.last_tok[ent[0]] = tok
        else:
            if self.cnt[eng] >= SEM_LIMIT:
                self.epoch[eng] += 1
                self.cnt[eng] = 0
            self.cnt[eng] += 1
            sname = self._sem(f"e_{eng}_{self.epoch[eng]}")
            tok = (sname, self.cnt[eng], eng)
            inc = (sname, 1)
            self.last_tok[sname] = tok
        kn = self.known[eng]
        waits = []
        for s, v in deps.items():
            if kn.get(s, 0) < v:
                waits.append((s, v))
                kn[s] = v
        if late:
            self.ops[eng].append((waits, fn, inc))
        else:
            rec = _Rec()
            fn(rec)
            self.ops[eng].append((waits, rec.call, inc))
        for b in writes:
            self.last_w[b] = tok
            self.readers[b] = []
        for b in reads:
            self.readers.setdefault(b, []).append(tok)
        if final:
            self.out_tokens.append(tok)
        self.nops += 1
        return tok

    def dma(self, out, in_, reads=(), writes=(), eng="sync", final=False, **kw):
        eng = "gpsimd" if "DRam" in type(out.tensor).__name__ else "sync"
        if out.dtype != in_.dtype:
            eng = "gpsimd"
        return self.op(eng, lambda e: e.dma_start(out=out, in_=in_, **kw), reads, writes, dma=True, final=final)

    def vload(self, eng, name, ap, lo, hi, reads):
        kn = self.known[eng]
        waits = []
        for b in reads:
            t = self.last_w.get(b)
            if t is not None and kn.get(t[0], 0) < t[1]:
                waits.append((t[0], t[1]))
                kn[t[0]] = t[1]
        self.ops[eng].append((waits, ("__vload__", name, ap, lo, hi), None))

    def barrier(self):
        toks = list(self.last_tok.values())
        for eng in self.ENGS:
            kn = self.known[eng]
            waits = []
            for s, v, _ in toks:
                if kn.get(s, 0) < v:
                    waits.append((s, v))
                    kn[s] = v
            if waits:
                self.ops[eng].append((waits, None, None))
        self.last_w = {}
        self.readers = {}

    def emit(self):
        nc = self.nc
        with contextlib.ExitStack() as st:
            for name in self.sem_order:
                self.sems[name] = st.enter_context(nc.semaphore(name))
            block = st.enter_context(nc.Block())
            sems = self.sems
            out_tokens = self.out_tokens

            def run(engname):
                def body(e):
                    env = {}
                    for waits, fn, inc in self.ops[engname]:
                        for s, v in waits:
                            e.wait_ge(sems[s], v)
                        if fn is None:
                            continue
                        if callable(fn):
                            fn(e, env).then_inc(sems[inc[0]], inc[1])
                        elif fn[0] == "__vload__":
                            env[fn[1]] = e.value_load(fn[2], min_val=fn[3], max_val=fn[4])
                        else:
                            getattr(e, fn[0])(*fn[1], **fn[2]).then_inc(sems[inc[0]], inc[1])
                    if engname == "sync":
                        for s, v, _ in out_tokens:
                            e.wait_ge(sems[s], v)
                return body

            block.tensor(run("tensor"))
            block.vector(run("vector"))
            block.scalar(run("scalar"))
            block.gpsimd(run("gpsimd"))
            block.sync(run("sync"))


class K:
    def __init__(self, S, C, debug=False):
        self.S, self.C, self.T = S, C, S + C
        self.debug = debug
        self.nc = bass.Bass("TRN2", target_bir_lowering=False)
        self.P = Prog(self.nc)
        self.inputs = {}
        self.scr = {}
        self.pb_rr = 0
        self.uid = 0
        self.debug_barrier = False

    def inp(self, name, shape, dt=F32):
        ap = self.nc.dram_tensor(name, list(shape), dt, kind="ExternalInput").ap()
        self.inputs[name] = ap
        return ap

    def scratch(self, name, shape, dt=F32):
        kind = "ExternalOutput" if self.debug else "Internal"
        ap = self.nc.dram_tensor(name, list(shape), dt, kind=kind).ap()
        self.scr[name] = ap
        return ap


def build(S, C, debug=False, stop_after=None, skip_l0=False):
    k = K(S, C, debug)
    nc, P = k.nc, k.P
    T = S + C
    NT_L, NT_C = S // 128, C // 128
    SQ = S // 2
    SH = SQ + 128

    x_d = k.inp("x", [S, D])
    ctx_d = k.inp("ctx", [C, D])
    cc_d = k.inp("cvec", [2, D])
    ident_d = k.inp("ident", [128, 128])
    w_mod_d = k.inp("w_mod", [2, D, 6 * D])
    b_mod_d = k.inp("b_mod", [2, 6 * D])
    ln_g_d = k.inp("ln_g", [2, 2, D])
    ln_b_d = k.inp("ln_b", [2, 2, D])
    e_w_in_d = k.inp("e_w_in", [D, 2432])
    e_b_in_d = k.inp("e_b_in", [2432])
    e_conv_w_d = k.inp("e_conv_w", [4, 512])
    e_conv_b_d = k.inp("e_conv_b", [512])
    e_wa_d = k.inp("e_lru_wa", [2, 8, 64, 64])
    e_ba_d = k.inp("e_lru_ba", [2, 512])
    e_wx_d = k.inp("e_lru_wx", [2, 8, 64, 64])
    e_bx_d = k.inp("e_lru_bx", [2, 512])
    e_lam_d = k.inp("e_lru_lambda", [2, 512])
    e_sink_d = k.inp("e_sink", [8])
    e_w_out_d = k.inp("e_w_out", [D, D])
    e_b_out_d = k.inp("e_b_out", [D])
    rope_e_d = k.inp("rope_e", [2, 128, S])
    mask_d = k.inp("swa_mask", [2, 128, 512])
    o_w_in_d = k.inp("o_w_in", [D, 1440])
    o_b_in_d = k.inp("o_b_in", [1440])
    o_q_norm_d = k.inp("o_q_norm", [256])
    o_kv_norm_d = k.inp("o_kv_norm", [128])
    o_w_uq_d = k.inp("o_w_uq", [256, 8, 192])
    o_w_uk_d = k.inp("o_w_uk", [128, 512])
    o_w_uv_d = k.inp("o_w_uv", [128, 512])
    o_w_kpe_rot_d = k.inp("o_w_kpe_rot", [D, 32])
    o_b_kpe_rot_d = k.inp("o_b_kpe_rot", [32])
    rope_q_d = k.inp("rope_q", [2, 96, SH])
    rope_k_d = k.inp("rope_k", [2, 32, S])
    xh_idx_d = k.inp("xh_idx", [SH], I32)
    zmask_d = k.inp("zmask", [SH])
    o_dw_w_d = k.inp("o_dw_w", [31, 512])
    o_dw_b_d = k.inp("o_dw_b", [512])
    o_cln_g_d = k.inp("o_cln_g", [512])
    o_cln_b_d = k.inp("o_cln_b", [512])
    o_w_out_d = k.inp("o_w_out", [D, D])
    o_b_out_d = k.inp("o_b_out", [D])
    moe_wg_d = k.inp("moe_w_gr", [2, D, 36])
    moe_bg_d = k.inp("moe_b_gr", [2, 36])
    moe_w1_d = k.inp("moe_w1", [2, 32, D, 512])
    moe_w3_d = k.inp("moe_w3", [2, 32, D, 512])
    moe_w2_d = k.inp("moe_w2", [2, 32, 512, D])
    out_d = nc.dram_tensor("out", [SQ, D], F32, kind="ExternalOutput").ap()

    m_scr = k.scratch("m_scr", [2, 2, 6 * D])
    XA = k.scratch("XA", [T, D])
    XBp = k.scratch("XB", [128 + T, D])
    XB = XBp[128:, :]
    XH = k.scratch("XH", [SH, D])
    FT = k.scratch("FT", [D, T], BF16)
    GATE = k.scratch("GATE", [T, 32])
    G_s = k.scratch("G_s", [512, T], BF16)
    U_s = k.scratch("U_s", [512, T])
    HF_s = k.scratch("HF_s", [512, T])
    MIXA = k.scratch("MIXA", [512, T], BF16)
    QT_s = k.scratch("QT_s", [8, 64, T], BF16)
    KT_s = k.scratch("KT_s", [2, 64, T], BF16)
    V_s = k.scratch("V_s", [T, 2, 65], BF16)
    ATT = k.scratch("ATT", [T, 512])
    QM = k.scratch("QM", [8, 96, SH], BF16)
    KM = k.scratch("KM", [8, 96, T], BF16)
    VM = k.scratch("VM", [T, 8, 65], BF16)
    ZC = k.scratch("ZC", [512, SH], BF16)
    CONV = k.scratch("CONV", [SQ, 512])

    st_all = contextlib.ExitStack()
    ps = st_all.enter_context(nc.psum_tensor("ps", [128, 4096], F32))
    SB_WORDS = 52000
    big = st_all.enter_context(nc.sbuf_tensor("big", [128, SB_WORDS], F32))
    k.sb_ptr = 0

    def pbank(n=1):
        if n == 2 and k.pb_rr % 2 == 1:
            k.pb_rr += 1
        i = k.pb_rr % 8
        k.pb_rr += n
        return ps[:, i * 512:(i + n) * 512], [f"pb{i + j}" for j in range(n)]

    class scope:
        def __enter__(self):
            self.mark = k.sb_ptr
            return self

        def __exit__(self, *a):
            k.sb_ptr = self.mark
            return False

    def sb(st, name, shape, dt=F32):
        k.uid += 1
        nm = f"{name}_{k.uid}"
        nfree = int(np.prod(shape[1:]))
        nwords = nfree if dt == F32 else (nfree + 1) // 2
        off = k.sb_ptr
        k.sb_ptr += nwords
        assert k.sb_ptr <= SB_WORDS, (name, k.sb_ptr)
        ap = big[:, off:off + nwords]
        if dt != F32:
            ap = ap.bitcast(dt)[:, 0:nfree]
        ap = ap[0:shape[0]]
        if len(shape) == 3:
            ap = ap.rearrange("p (a b) -> p a b", a=shape[1])
        elif len(shape) == 4:
            ap = ap.rearrange("p (a b c) -> p a b c", a=shape[1], b=shape[2])
        elif len(shape) == 5:
            ap = ap.rearrange("p (a b c d) -> p a b c d", a=shape[1], b=shape[2], c=shape[3])
        return ap, nm

    V, A, G, TE = "vector", "scalar", "gpsimd", "tensor"

    def ring(st, name, shape, dt=F32, n=2, init=None):
        tiles = [sb(st, name, shape, dt) for _ in range(n)]
        if init is not None:
            for t_, k_ in tiles:
                P.op(V, lambda e, t_=t_: e.memset(t_[:], init), [], [k_])
        cnt = [0]

        def nxt():
            cnt[0] += 1
            return tiles[(cnt[0] - 1) % n]
        return nxt

    ident, ident_k = sb(None, "ident", [128, 128])
    P.dma(ident[:], ident_d, writes=[ident_k])
    ones_r, ones_k = sb(None, "ones", [1, 128])
    P.op(V, lambda e: e.memset(ones_r[:], 1.0), [], [ones_k])
    mcol, mcol_k = sb(None, "mcol", [128, 2, 2, 4, 8])

    def col_dma(dst_ap, src_ap, keys_w, eng="gpsimd", reads=()):
        P.dma(dst_ap, src_ap, writes=keys_w, reads=reads, eng=eng, allow_slow_non_contiguous=True)

    with scope() as st:
        csT, csT_k = sb(st, "csT", [128, 8, 2])
        craw, craw_k = sb(st, "craw", [128, 8, 2])
        for s_ in range(2):
            col_dma(craw[:, :, s_], cc_d[s_].rearrange("(c p) -> p c", p=128), [craw_k])
        P.op(A, lambda e: e.activation(out=csT[:], in_=craw[:], func=AF.Silu), [craw_k], [csT_k])
        bm, bm_k = sb(st, "bm", [2, 6 * D])
        mrow, mrow_k = sb(st, "mrow", [2, 6 * D])
        wm = [sb(st, f"wm{i}", [128, 8, 512]) for i in range(2)]
        for l in range(2):
            P.dma(bm[:], b_mod_d[l].partition_broadcast(2), writes=[bm_k], reads=[])
            for n in range(12):
                wt, wk = wm[n % 2]
                P.dma(wt[:], w_mod_d[l, :, n * 512:(n + 1) * 512].rearrange("(kc p) n -> p kc n", p=128), writes=[wk],
                      eng="sync" if n % 2 == 0 else "gpsimd")
                pb, pk = pbank()
                for kc in range(8):
                    P.op(TE, lambda e, kc=kc, wt=wt, pb=pb: e.matmul(pb[0:2, :], lhsT=csT[:, kc, :], rhs=wt[:, kc, :],
                                                                   start=(kc == 0), stop=(kc == 7)),
                         [csT_k, wk], pk)
                P.op(V, lambda e, pb=pb, n=n: e.tensor_tensor(out=mrow[:, n * 512:(n + 1) * 512], in0=pb[0:2, :],
                                                               in1=bm[:, n * 512:(n + 1) * 512], op=ALU.add),
                     pk + [bm_k], [mrow_k])
            P.dma(m_scr[l], mrow[:], reads=[mrow_k], writes=["m_scr"], eng="gpsimd")
        zpad, zpadk = sb(st, "zpad", [64, D])
        P.op(V, lambda e: e.memset(zpad[:], 0.0), [], [zpadk])
        P.dma(XBp[64:128, :], zpad[:], reads=[zpadk], writes=["XBpad"], eng="gpsimd")
        for l in range(2):
            for s_ in range(2):
                for j, idx in enumerate((0, 1, 3, 4)):
                    col_dma(mcol[:, l, s_, j, :], m_scr[l, s_, idx * D:(idx + 1) * D].rearrange("(c p) -> p c", p=128),
                            [mcol_k], reads=["m_scr"])
        P.op(V, lambda e: e.tensor_scalar_add(out=mcol[:, :, :, 1, :], in0=mcol[:, :, :, 1, :], scalar1=1.0), [mcol_k], [mcol_k])
        P.op(V, lambda e: e.tensor_scalar_add(out=mcol[:, :, :, 3, :], in0=mcol[:, :, :, 3, :], scalar1=1.0), [mcol_k], [mcol_k])
    P.barrier()

    def load_bc(st, name, row_ap, n):
        t, tk = sb(st, name, [128, n])
        P.dma(t[:], row_ap.partition_broadcast(128), writes=[tk], eng="gpsimd")
        return t, tk

    def tok_src(l_idx, which):
        raise NotImplementedError

    def transpose_mod(xt, xk, dst, dk, col_sc, col_sh, ck, tcol, out_parity=0):
        pb, pk = pbank(2)
        for kc in range(8):
            P.op(TE, lambda e, kc=kc, pb=pb: e.transpose(pb[:, kc * 128:(kc + 1) * 128], xt[:, kc * 128:(kc + 1) * 128], ident[:]),
                 [xk, ident_k], pk)
        for kc in range(8):
            if kc % 2 == 0:
                P.op(V, lambda e, kc=kc, pb=pb: e.tensor_scalar(out=dst[:, kc, tcol:tcol + 128], in0=pb[:, kc * 128:(kc + 1) * 128],
                                                               scalar1=col_sc[:, kc:kc + 1], scalar2=col_sh[:, kc:kc + 1],
                                                               op0=ALU.mult, op1=ALU.add), pk + [ck], [dk])
            else:
                P.op(A, lambda e, kc=kc, pb=pb: e.activation(out=dst[:, kc, tcol:tcol + 128], in_=pb[:, kc * 128:(kc + 1) * 128],
                                                            func=AF.Identity, scale=col_sc[:, kc:kc + 1], bias=col_sh[:, kc:kc + 1]),
                     pk + [ck], [dk])

    def layer_norm_tile(st_tiles, z, zk, g_bc, gk, b_bc, bk, out, ok, width=D):
        stats, sk = st_tiles["stats"]
        mv, mvk = st_tiles["mv"]
        nchunk = width // 512
        for c_ in range(nchunk):
            P.op(V, lambda e, c_=c_: e.bn_stats(out=stats[:, c_, :], in_=z[:, c_ * 512:(c_ + 1) * 512]), [zk], [sk])
        P.op(V, lambda e: e.bn_aggr(out=mv[:, 0:2], in_=stats[:, 0:nchunk, :]), [sk], [mvk])
        P.op(V, lambda e: e.tensor_scalar_add(out=mv[:, 2:3], in0=mv[:, 1:2], scalar1=LN_EPS), [mvk], [mvk])
        P.op(A, lambda e: e.sqrt(out=mv[:, 3:4], in_=mv[:, 2:3]), [mvk], [mvk])
        P.op(V, lambda e: e.reciprocal(out=mv[:, 4:5], in_=mv[:, 3:4]), [mvk], [mvk])
        P.op(V, lambda e: e.scalar_tensor_tensor(out=mv[:, 5:6], in0=mv[:, 0:1], scalar=-1.0, in1=mv[:, 4:5],
                                                 op0=ALU.mult, op1=ALU.mult), [mvk], [mvk])
        P.op(A, lambda e: e.activation(out=out[:, 0:width], in_=z[:, 0:width], func=AF.Identity, scale=mv[:, 4:5], bias=mv[:, 5:6]),
             [zk, mvk], [ok])
        P.op(G, lambda e: e.tensor_tensor(out=out[:, 0:width], in0=out[:, 0:width], in1=g_bc[:, 0:width], op=ALU.mult), [ok, gk], [ok])
        P.op(V, lambda e: e.tensor_tensor(out=out[:, 0:width], in0=out[:, 0:width], in1=b_bc[:, 0:width], op=ALU.add), [ok, bk], [ok])

    def tok_tiles(n_lat_only=False, n_lat=None):
        r = [(i * 128, 0) for i in range(NT_L if n_lat is None else n_lat // 128)]
        if not n_lat_only:
            r += [(S + i * 128, 1) for i in range(NT_C)]
        return r

    def mixer_epilogue(l, w_out_d, b_out_d, x_src, mixT_loader, with_ctx, n_lat=None):
        with scope() as st:
            wo, wok = sb(st, "wo", [128, 8, D], BF16)
            P.dma(wo[:], w_out_d.rearrange("(kc p) n -> p kc n", p=128), writes=[wok], eng="gpsimd")
            wr, wrk = sb(st, "wr", [128, 8, 36])
            P.dma(wr[:], moe_wg_d[l].rearrange("(kc p) n -> p kc n", p=128), writes=[wrk])
            br, brk = load_bc(st, "br", moe_bg_d[l], 36)
            lng, lngk = load_bc(st, "lng", ln_g_d[l, 0], D)
            lnb, lnbk = load_bc(st, "lnb", ln_b_d[l, 0], D)
            streams = (0, 1) if with_ctx else (0,)
            gate_bc, gb_bc = {}, {}
            bo, bok = load_bc(st, "bo", b_out_d, D)
            for s_ in streams:
                gate_bc[s_] = load_bc(st, f"gate{s_}", m_scr[l, s_, 2 * D:3 * D], D)
                gb_bc[s_] = sb(st, f"gb{s_}", [128, D])
                P.op(V, lambda e, s_=s_: e.tensor_tensor(out=gb_bc[s_][0][:], in0=gate_bc[s_][0][:], in1=bo[:], op=ALU.mult),
                     [gate_bc[s_][1], bok], [gb_bc[s_][1]])
            NBUF = 2
            bufs = []
            for bi in range(NBUF):
                bufs.append(dict(
                    xt=sb(st, "xt", [128, D]), mixT=sb(st, "mixT", [128, 8, 128], BF16), tmp=sb(st, "tmp", [128, D]),
                    z=sb(st, "z", [128, D]), x1=sb(st, "x1", [128, D]), fT=sb(st, "fT", [128, 8, 128]),
                    fTb=sb(st, "fTb", [128, 8, 128], BF16),
                    small={"stats": sb(st, "stats", [128, 2, 6]), "mv": sb(st, "mv", [128, 8])},
                    lg=sb(st, "lg", [128, 36]), rt=sb(st, "rt", [128, 64]), gt=sb(st, "gt", [128, 32]), ld={}))
            for ti_, (t0, s_) in enumerate(tok_tiles(not with_ctx, n_lat)):
                B_ = bufs[ti_ % NBUF]
                xt, xk = B_["xt"]; mixT, mixk = B_["mixT"]; tmp, tmpk = B_["tmp"]; z, zk = B_["z"]; x1, x1k = B_["x1"]
                fT, fTk = B_["fT"]; fTb, fTbk = B_["fTb"]; small = B_["small"]; lg, lgk = B_["lg"]; rt, rtk = B_["rt"]; gt, gtk = B_["gt"]
                st.ld = B_["ld"]
                P.dma(xt[:], x_src(t0, s_), writes=[xk])
                mixT_loader(t0, s_, mixT, mixk, st)
                pb, pk = pbank(2)
                for n in range(2):
                    for kc in range(8):
                        P.op(TE, lambda e, kc=kc, n=n, pb=pb: e.matmul(pb[:, n * 512:(n + 1) * 512], lhsT=mixT[:, kc, :],
                                                                       rhs=wo[:, kc, n * 512:(n + 1) * 512], start=(kc == 0), stop=(kc == 7)),
                             [mixk, wok], pk)
                gbc, gbck = gate_bc[s_]
                P.op(V, lambda e, pb=pb, gbc=gbc: e.tensor_tensor(out=tmp[:], in0=pb, in1=gbc[:], op=ALU.mult), pk + [gbck], [tmpk])
                P.op(V, lambda e: e.scalar_tensor_tensor(out=z[:], in0=xt[:], scalar=DN_ALPHA, in1=tmp[:], op0=ALU.mult, op1=ALU.add),
                     [xk, tmpk], [zk])
                P.op(G, lambda e, s_=s_: e.tensor_tensor(out=z[:], in0=z[:], in1=gb_bc[s_][0][:], op=ALU.add), [zk, gb_bc[s_][1]], [zk])
                layer_norm_tile(small, z, zk, lng, lngk, lnb, lnbk, x1, x1k)
                P.dma(XA[t0:t0 + 128, :], x1[:], reads=[x1k], writes=["XA"], eng="gpsimd")
                transpose_mod(x1, x1k, fT, fTk, mcol[:, l, s_, 3, :], mcol[:, l, s_, 2, :], mcol_k, 0)
                P.op(G, lambda e: e.tensor_copy(out=fTb[:], in_=fT[:]), [fTk], [fTbk])
                P.dma(FT[:, t0:t0 + 128].rearrange("(kc p) t -> p kc t", p=128), fTb[:], reads=[fTbk], writes=["FT"], eng="gpsimd")
                pb2, pk2 = pbank()
                for kc in range(8):
                    P.op(TE, lambda e, kc=kc, pb2=pb2: e.matmul(pb2[:, 0:36], lhsT=fT[:, kc, :], rhs=wr[:, kc, :], start=(kc == 0), stop=(kc == 7)),
                         [fTk, wrk], pk2)
                P.op(V, lambda e, pb2=pb2: e.tensor_tensor(out=lg[:], in0=pb2[:, 0:36], in1=br[:], op=ALU.add), pk2 + [brk], [lgk])
                routing(lg, lgk, rt, rtk, gt, gtk)
                P.dma(GATE[t0:t0 + 128, :], gt[:], reads=[gtk], writes=["GATE"], eng="gpsimd")
        P.barrier()

    def routing(lg, lgk, rt, rtk, gt, gtk):
        ops = P.op
        ops(V, lambda e: e.reduce_max(out=rt[:, 0:1], in_=lg[:, 0:4], axis=AX.X), [lgk], [rtk])
        ops(V, lambda e: e.tensor_scalar_mul(out=rt[:, 1:2], in0=rt[:, 0:1], scalar1=-1.0), [rtk], [rtk])
        ops(A, lambda e: e.activation(out=rt[:, 56:60], in_=lg[:, 0:4], func=AF.Exp, bias=rt[:, 1:2], scale=1.0, accum_out=rt[:, 2:3]),
            [lgk, rtk], [rtk])
        ops(V, lambda e: e.reciprocal(out=rt[:, 3:4], in_=rt[:, 2:3]), [rtk], [rtk])
        ops(V, lambda e: e.tensor_scalar(out=rt[:, 4:8], in0=lg[:, 0:4], scalar1=rt[:, 0:1], scalar2=None, op0=ALU.is_ge), [lgk, rtk], [rtk])
        ops(V, lambda e: e.tensor_scalar_mul(out=rt[:, 8:16], in0=lg[:, 4:12], scalar1=rt[:, 4:5]), [lgk, rtk], [rtk])
        for g_ in range(1, 4):
            ops(V, lambda e, g_=g_: e.scalar_tensor_tensor(out=rt[:, 8:16], in0=lg[:, 4 + 8 * g_:12 + 8 * g_], scalar=rt[:, 4 + g_:5 + g_],
                                                           in1=rt[:, 8:16], op0=ALU.mult, op1=ALU.add), [lgk, rtk], [rtk])
        ops(V, lambda e: e.max(out=rt[:, 16:24], in_=rt[:, 8:16]), [rtk], [rtk])
        ops(V, lambda e: e.tensor_tensor(out=rt[:, 24:25], in0=rt[:, 17:18], in1=rt[:, 16:17], op=ALU.subtract), [rtk], [rtk])
        ops(A, lambda e: e.activation(out=rt[:, 24:25], in_=rt[:, 24:25], func=AF.Exp), [rtk], [rtk])
        ops(V, lambda e: e.tensor_scalar_add(out=rt[:, 25:26], in0=rt[:, 24:25], scalar1=1.0), [rtk], [rtk])
        ops(V, lambda e: e.reciprocal(out=rt[:, 26:27], in_=rt[:, 25:26]), [rtk], [rtk])
        ops(V, lambda e: e.tensor_tensor(out=rt[:, 27:28], in0=rt[:, 26:27], in1=rt[:, 3:4], op=ALU.mult), [rtk], [rtk])
        ops(V, lambda e: e.tensor_tensor(out=rt[:, 28:29], in0=rt[:, 27:28], in1=rt[:, 24:25], op=ALU.mult), [rtk], [rtk])
        ops(V, lambda e: e.tensor_scalar(out=rt[:, 32:40], in0=rt[:, 8:16], scalar1=rt[:, 16:17], scalar2=None, op0=ALU.is_ge), [rtk], [rtk])
        ops(V, lambda e: e.tensor_scalar(out=rt[:, 40:48], in0=rt[:, 8:16], scalar1=rt[:, 17:18], scalar2=None, op0=ALU.is_ge), [rtk], [rtk])
        ops(V, lambda e: e.tensor_tensor(out=rt[:, 40:48], in0=rt[:, 40:48], in1=rt[:, 32:40], op=ALU.subtract), [rtk], [rtk])
        ops(V, lambda e: e.tensor_scalar_mul(out=rt[:, 48:56], in0=rt[:, 32:40], scalar1=rt[:, 27:28]), [rtk], [rtk])
        ops(V, lambda e: e.scalar_tensor_tensor(out=rt[:, 48:56], in0=rt[:, 40:48], scalar=rt[:, 28:29], in1=rt[:, 48:56],
                                                op0=ALU.mult, op1=ALU.add), [rtk], [rtk])
        for g_ in range(4):
            ops(V, lambda e, g_=g_: e.tensor_scalar_mul(out=gt[:, 8 * g_:8 * g_ + 8], in0=rt[:, 48:56], scalar1=rt[:, 4 + g_:5 + g_]),
                [rtk], [gtk])

    def moe_phase(l, with_ctx, dst_fn, final, n_lat=None):
        Tn = T if with_ctx else (S if n_lat is None else n_lat)
        TG = 1024
        with scope() as st:
            lng, lngk = load_bc(st, "lng2", ln_g_d[l, 1], D)
            lnb, lnbk = load_bc(st, "lnb2", ln_b_d[l, 1], D)
            g5 = {0: load_bc(st, "g5l", m_scr[l, 0, 5 * D:6 * D], D)}
            if with_ctx:
                g5[1] = load_bc(st, "g5c", m_scr[l, 1, 5 * D:6 * D], D)
            ftg, ftgk = sb(st, "ftg", [128, 8, TG], BF16)
            gts, gtsk = sb(st, "gts", [128, 8, 32])
            acc, acck = sb(st, "acc", [128, 8, D])
            w1 = [sb(st, f"w1_{i}", [128, 8, 512], BF16) for i in range(2)]
            w3 = [sb(st, f"w3_{i}", [128, 8, 512], BF16) for i in range(2)]
            w2 = [sb(st, f"w2_{i}", [128, 4, D], BF16) for i in range(2)]
            hm, hmk = sb(st, "hm", [128, 4, TG], BF16)
            sil, silk = sb(st, "sil", [128, 512])
            xt, xk = sb(st, "xt2", [128, D])
            z, zk = sb(st, "z2", [128, D])
            xo, xok = sb(st, "xo", [128, D])
            small = {"stats": sb(st, "stats2", [128, 2, 6]), "mv": sb(st, "mv2", [128, 8])}
            for g0 in range(0, Tn, TG):
                ng = min(TG, Tn - g0)
                ntt = ng // 128
                P.dma(ftg[:, :, 0:ng], FT[:, g0:g0 + ng].rearrange("(kc p) t -> p kc t", p=128), reads=["FT"], writes=[ftgk])
                P.dma(gts[:, 0:ntt, :], GATE[g0:g0 + ng, :].rearrange("(a p) e -> p a e", p=128), reads=["GATE"], writes=[gtsk], eng="gpsimd")
                for ex in range(32):
                    w1t, w1k = w1[ex % 2]
                    w3t, w3k = w3[ex % 2]
                    w2t, w2k = w2[ex % 2]
                    P.dma(w1t[:], moe_w1_d[l, ex].rearrange("(kc p) n -> p kc n", p=128), writes=[w1k], eng="sync")
                    P.dma(w3t[:], moe_w3_d[l, ex].rearrange("(kc p) n -> p kc n", p=128), writes=[w3k], eng="sync")
                    P.dma(w2t[:], moe_w2_d[l, ex].rearrange("(kc p) n -> p kc n", p=128), writes=[w2k], eng="sync")
                    for n0 in range(0, ng, 512):
                        nn = min(512, ng - n0)
                        for oc in range(4):
                            p1, p1k = pbank()
                            p3, p3k = pbank()
                            for kc in range(8):
                                P.op(TE, lambda e, kc=kc, oc=oc, p1=p1, w1t=w1t, n0=n0, nn=nn: e.matmul(
                                    p1[:, 0:nn], lhsT=w1t[:, kc, oc * 128:(oc + 1) * 128], rhs=ftg[:, kc, n0:n0 + nn],
                                    start=(kc == 0), stop=(kc == 7)), [w1k, ftgk], p1k)
                            for kc in range(8):
                                P.op(TE, lambda e, kc=kc, oc=oc, p3=p3, w3t=w3t, n0=n0, nn=nn: e.matmul(
                                    p3[:, 0:nn], lhsT=w3t[:, kc, oc * 128:(oc + 1) * 128], rhs=ftg[:, kc, n0:n0 + nn],
                                    start=(kc == 0), stop=(kc == 7)), [w3k, ftgk], p3k)
                            P.op(A, lambda e, p1=p1, nn=nn: e.activation(out=sil[:, 0:nn], in_=p1[:, 0:nn], func=AF.Silu), p1k, [silk])
                            P.op(V, lambda e, p3=p3, nn=nn, oc=oc, n0=n0: e.tensor_tensor(out=hm[:, oc, n0:n0 + nn], in0=p3[:, 0:nn],
                                                                                         in1=sil[:, 0:nn], op=ALU.mult), p3k + [silk], [hmk])
                    for tt in range(ntt):
                        py, pyk = pbank(2)
                        for n in range(2):
                            for kc in range(4):
                                P.op(TE, lambda e, kc=kc, n=n, tt=tt, py=py, w2t=w2t: e.matmul(
                                    py[:, n * 512:(n + 1) * 512], lhsT=hm[:, kc, tt * 128:(tt + 1) * 128],
                                    rhs=w2t[:, kc, n * 512:(n + 1) * 512], start=(kc == 0), stop=(kc == 3)), [hmk, w2k], pyk)
                        eng_ = V if tt % 4 != 3 else G
                        if ex == 0:
                            P.op(V, lambda e, tt=tt, py=py, ex=ex: e.tensor_scalar_mul(out=acc[:, tt, :], in0=py, scalar1=gts[:, tt, ex:ex + 1]),
                                 pyk + [gtsk], [acck + str(tt)])
                        else:
                            P.op(V, lambda e, tt=tt, py=py, ex=ex: e.scalar_tensor_tensor(
                                out=acc[:, tt, :], in0=py, scalar=gts[:, tt, ex:ex + 1], in1=acc[:, tt, :], op0=ALU.mult, op1=ALU.add),
                                 pyk + [gtsk, acck + str(tt)], [acck + str(tt)])
                for tt in range(ntt):
                    t0 = g0 + tt * 128
                    s_ = 0 if t0 < S else 1
                    P.dma(xt[:], XA[t0:t0 + 128, :], reads=["XA"], writes=[xk])
                    P.op(G, lambda e, tt=tt, s_=s_: e.tensor_tensor(out=z[:], in0=acc[:, tt, :], in1=g5[s_][0][:], op=ALU.mult),
                         [acck + str(tt), g5[s_][1]], [zk])
                    P.op(V, lambda e: e.scalar_tensor_tensor(out=z[:], in0=xt[:], scalar=DN_ALPHA, in1=z[:], op0=ALU.mult, op1=ALU.add),
                         [xk, zk], [zk])
                    layer_norm_tile(small, z, zk, lng, lngk, lnb, lnbk, xo, xok)
                    dst = dst_fn(t0)
                    P.dma(dst, xo[:], reads=[xok], writes=["XB"], eng="gpsimd", final=final)
        P.barrier()

    def x0_src(t0, s_):
        return x_d[t0:t0 + 128, :] if s_ == 0 else ctx_d[t0 - S:t0 - S + 128, :]

    with scope() as st:
        win, wink = sb(st, "win", [128, 8, 2432], BF16)
        for kc in range(8):
            P.dma(win[:, kc, :], e_w_in_d[kc * 128:(kc + 1) * 128, :], writes=[wink], eng="sync" if kc % 2 else "gpsimd")
        bcol, bcolk = sb(st, "bcol", [128, 19])
        fm_cols = [(i * 128) for i in range(13)] + [1792 + i * 128 for i in range(5)]
        for j, c0 in enumerate(fm_cols):
            col_dma(bcol[:, j:j + 1], e_b_in_d[c0:c0 + 128].rearrange("(p o) -> p o", o=1), [bcolk])
        bv, bvk = load_bc(st, "bv", e_b_in_d[1664:1792], 128)
        xt_r = ring(st, "xt", [128, D])
        hT_r = ring(st, "hT", [128, 8, 512], BF16)
        gtmp_r = ring(st, "gtmp", [128, 512], BF16)
        utmp_r = ring(st, "utmp", [128, 512])
        cosT, cosk = sb(st, "cosT", [128, 512])
        sinT, sink = sb(st, "sinT", [128, 512])
        q1_r = ring(st, "q1", [128, 512])
        q2_r = ring(st, "q2", [128, 512])
        qo_r = ring(st, "qo", [128, 512], BF16, n=3)
        vt_r = ring(st, "vt", [128, 2, 65], BF16, init=1.0)
        groups = [(g0, 0, min(512, S - g0)) for g0 in range(0, S, 512)] + [(S + g0, 1, min(512, C - g0)) for g0 in range(0, C, 512)]
        for (g0, s_, ng) in groups:
            hT, hTk = hT_r()
            for tt in range(ng // 128):
                xt, xk = xt_r()
                P.dma(xt[:], x0_src(g0 + tt * 128, s_), writes=[xk], eng="sync")
                transpose_mod(xt, xk, hT, hTk, mcol[:, 0, s_, 1, :], mcol[:, 0, s_, 0, :], mcol_k, tt * 128)
            if k.debug_barrier:
                P.barrier()
            if s_ == 0:
                P.dma(cosT[:, 0:ng], rope_e_d[0, :, g0:g0 + ng], writes=[cosk], eng="gpsimd")
                P.dma(sinT[:, 0:ng], rope_e_d[1, :, g0:g0 + ng], writes=[sink], eng="gpsimd")

            def mm_chunk(c0, pb, pk):
                for kc in range(8):
                    P.op(TE, lambda e, kc=kc: e.matmul(pb[:, 0:ng], lhsT=win[:, kc, c0:c0 + 128], rhs=hT[:, kc, 0:ng],
                                                       start=(kc == 0), stop=(kc == 7)), [wink, hTk], pk)
            for j in range(4):
                pb, pk = pbank()
                mm_chunk(j * 128, pb, pk)
                gtmp, gtmpk = gtmp_r()
                P.op(A, lambda e, pb=pb, j=j: e.activation(out=gtmp[:, 0:ng], in_=pb[:, 0:ng], func=AF.Gelu_apprx_tanh,
                                                          bias=bcol[:, j:j + 1], scale=1.0), pk + [bcolk], [gtmpk])
                P.dma(G_s[j * 128:(j + 1) * 128, g0:g0 + ng], gtmp[:, 0:ng], reads=[gtmpk], writes=["G_s"], eng="gpsimd")
            for j in range(4):
                pb, pk = pbank()
                mm_chunk(512 + j * 128, pb, pk)
                utmp, utmpk = utmp_r()
                P.op(A, lambda e, pb=pb, j=j: e.activation(out=utmp[:, 0:ng], in_=pb[:, 0:ng], func=AF.Identity,
                                                          bias=bcol[:, 4 + j:5 + j], scale=1.0), pk + [bcolk], [utmpk])
                P.dma(U_s[j * 128:(j + 1) * 128, g0:g0 + ng], utmp[:, 0:ng], reads=[utmpk], writes=["U_s"], eng="gpsimd")
            for j in range(5):
                pb, pk = pbank()
                mm_chunk(1024 + j * 128, pb, pk)
                q1, q1k = q1_r()
                q2, q2k = q2_r()
                qo, qok = qo_r()
                if s_ == 0:
                    pr, prk = pbank()
                    mm_chunk(1792 + j * 128, pr, prk)
                    P.op(V, lambda e, pb=pb, j=j: e.scalar_tensor_tensor(out=q1[:, 0:ng], in0=pb[:, 0:ng], scalar=bcol[:, 8 + j:9 + j],
                                                                         in1=cosT[:, 0:ng], op0=ALU.add, op1=ALU.mult),
                         pk + [bcolk, cosk], [q1k])
                    P.op(V, lambda e, pr=pr, j=j: e.scalar_tensor_tensor(out=q2[:, 0:ng], in0=pr[:, 0:ng], scalar=bcol[:, 13 + j:14 + j],
                                                                         in1=sinT[:, 0:ng], op0=ALU.add, op1=ALU.mult),
                         prk + [bcolk, sink], [q2k])
                    P.op(G, lambda e: e.tensor_tensor(out=qo[:, 0:ng], in0=q1[:, 0:ng], in1=q2[:, 0:ng], op=ALU.add), [q1k, q2k], [qok])
                else:
                    P.op(A, lambda e, pb=pb, j=j: e.activation(out=qo[:, 0:ng], in_=pb[:, 0:ng], func=AF.Identity,
                                                              bias=bcol[:, 8 + j:9 + j], scale=1.0), pk + [bcolk], [qok])
                if j < 4:
                    dst = QT_s[2 * j:2 * j + 2, :, g0:g0 + ng].rearrange("h d t -> (h d) t")
                else:
                    dst = KT_s[:, :, g0:g0 + ng].rearrange("h d t -> (h d) t")
                P.dma(dst, qo[:, 0:ng], reads=[qok], writes=["QKT"], eng="gpsimd")
            for tt in range(ng // 128):
                vt, vtk = vt_r()
                pb, pk = pbank()
                for kc in range(8):
                    P.op(TE, lambda e, kc=kc, tt=tt, pb=pb: e.matmul(pb[:, 0:128], lhsT=hT[:, kc, tt * 128:(tt + 1) * 128],
                                                                     rhs=win[:, kc, 1664:1792], start=(kc == 0), stop=(kc == 7)),
                         [wink, hTk], pk)
                P.op(V, lambda e, pb=pb: e.tensor_tensor(out=vt[:, :, 0:64], in0=pb[:, 0:128].rearrange("p (h d) -> p h d", h=2),
                                                         in1=bv[:].rearrange("p (h d) -> p h d", h=2), op=ALU.add), pk + [bvk], [vtk])
                t0 = g0 + tt * 128
                P.dma(V_s[t0:t0 + 128], vt[:], reads=[vtk], writes=["V_s"], eng="gpsimd")
    P.barrier()
    if stop_after == "P1":
        return finish(k, st_all)

    SEG = 1024 if S % 1024 == 0 else 512
    with scope() as st:
        wbd32, wbd32k = sb(st, "wbd32", [128, 16, 128])
        wbd, wbdk = sb(st, "wbd", [128, 16, 128], BF16)
        P.op(V, lambda e: e.memset(wbd32[:], 0.0), [], [wbd32k])
        for d_ in range(2):
            for cc in range(4):
                for ai, wd in enumerate((e_wa_d, e_wx_d)):
                    ix = (d_ * 4 + cc) * 2 + ai
                    for hb in range(2):
                        P.dma(wbd32[hb * 64:(hb + 1) * 64, ix, hb * 64:(hb + 1) * 64], wd[d_, 2 * cc + hb], writes=[wbd32k], eng="gpsimd")
        P.op(V, lambda e: e.tensor_copy(out=wbd[:], in_=wbd32[:]), [wbd32k], [wbdk])
        cols, colsk = sb(st, "cols", [128, 64])
        col_dma(cols[:, 0:8], e_lam_d.rearrange("d (c p) -> p (d c)", p=128), [colsk])
        col_dma(cols[:, 8:16], e_ba_d.rearrange("d (c p) -> p (d c)", p=128), [colsk])
        col_dma(cols[:, 16:24], e_bx_d.rearrange("d (c p) -> p (d c)", p=128), [colsk])
        col_dma(cols[:, 24:28], e_conv_b_d.rearrange("(c p) -> p c", p=128), [colsk])
        for cc in range(4):
            col_dma(cols[:, 28 + 4 * cc:32 + 4 * cc], e_conv_w_d[:, cc * 128:(cc + 1) * 128].rearrange("k p -> p k"), [colsk])
        P.op(A, lambda e: e.activation(out=cols[:, 44:52], in_=cols[:, 0:8], func=AF.Exp, scale=-1.0), [colsk], [colsk])
        P.op(A, lambda e: e.activation(out=cols[:, 44:52], in_=cols[:, 44:52], func=AF.Ln, bias=1.0, scale=1.0), [colsk], [colsk])
        P.op(V, lambda e: e.tensor_scalar_mul(out=cols[:, 52:60], in0=cols[:, 44:52], scalar1=-16.0), [colsk], [colsk])
        P.op(V, lambda e: e.tensor_scalar_mul(out=cols[:, 44:52], in0=cols[:, 44:52], scalar1=-8.0), [colsk], [colsk])
        rings_ = {nm: ring(st, nm, [128, SEG + (3 if nm == "uh" else 0)], BF16 if nm in ("ucb", "gg", "mx") else F32)
                  for nm in ("uh", "uc", "ucb", "r", "i", "a", "b", "h", "hf", "gg", "mx")}
        state, statek = sb(st, "state", [128, 1])
        segs_l = [(s0, min(SEG, S - s0), 0) for s0 in range(0, S, SEG)]
        seg_c = (S, C, 1)
        for cc in range(4):
            rows = slice(cc * 128, (cc + 1) * 128)
            for d_ in range(2):
                order = [seg_c] + (segs_l if d_ == 0 else segs_l[::-1])
                P.op(V, lambda e: e.memset(state[:], 0.0), [], [statek])
                for (s0, n, s_) in order:
                    uh, uhk = rings_["uh"](); uc, uck = rings_["uc"](); ucb, ucbk = rings_["ucb"](); r_, rk = rings_["r"]()
                    i_, ik = rings_["i"](); a_, ak = rings_["a"](); b_, bk = rings_["b"](); h_, hk = rings_["h"]()
                    hf, hfk = rings_["hf"](); gg, ggk = rings_["gg"](); mx, mxk = rings_["mx"]()
                    lo = S if s_ == 1 else 0
                    hi = T if s_ == 1 else S
                    a0, a1 = max(lo, s0 - 1), min(hi, s0 + n + 2)
                    P.op(V, lambda e: e.memset(uh[:], 0.0), [], [uhk])
                    P.dma(uh[:, a0 - (s0 - 1):a1 - (s0 - 1)], U_s[rows, a0:a1], reads=["U_s"], writes=[uhk])
                    cw = 28 + 4 * cc
                    P.op(V, lambda e, n=n, cw=cw, cc=cc: e.tensor_scalar(out=uc[:, 0:n], in0=uh[:, 0:n], scalar1=cols[:, cw:cw + 1],
                                                                        scalar2=cols[:, 24 + cc:25 + cc], op0=ALU.mult, op1=ALU.add),
                         [uhk, colsk], [uck])
                    for kk in range(1, 4):
                        P.op(V, lambda e, n=n, cw=cw, kk=kk: e.scalar_tensor_tensor(out=uc[:, 0:n], in0=uh[:, kk:kk + n],
                                                                                    scalar=cols[:, cw + kk:cw + kk + 1], in1=uc[:, 0:n],
                                                                                    op0=ALU.mult, op1=ALU.add), [uhk, colsk, uck], [uck])
                    P.op(A, lambda e, n=n: e.copy(out=ucb[:, 0:n], in_=uc[:, 0:n]), [uck], [ucbk])
                    dc = d_ * 4 + cc
                    for n0 in range(0, n, 512):
                        nn = min(512, n - n0)
                        pa, pak = pbank()
                        px, pxk = pbank()
                        P.op(TE, lambda e, pa=pa, n0=n0, nn=nn, dc=dc: e.matmul(pa[:, 0:nn], lhsT=wbd[:, dc * 2, :], rhs=ucb[:, n0:n0 + nn],
                                                                                start=True, stop=True), [wbdk, ucbk], pak)
                        P.op(TE, lambda e, px=px, n0=n0, nn=nn, dc=dc: e.matmul(px[:, 0:nn], lhsT=wbd[:, dc * 2 + 1, :], rhs=ucb[:, n0:n0 + nn],
                                                                                start=True, stop=True), [wbdk, ucbk], pxk)
                        P.op(A, lambda e, pa=pa, n0=n0, nn=nn, dc=dc: e.activation(out=r_[:, n0:n0 + nn], in_=pa[:, 0:nn], func=AF.Sigmoid,
                                                                                   bias=cols[:, 8 + dc:9 + dc], scale=1.0), pak + [colsk], [rk])
                        P.op(A, lambda e, px=px, n0=n0, nn=nn, dc=dc: e.activation(out=i_[:, n0:n0 + nn], in_=px[:, 0:nn], func=AF.Sigmoid,
                                                                                   bias=cols[:, 16 + dc:17 + dc], scale=1.0), pxk + [colsk], [ik])
                    P.op(A, lambda e, n=n, dc=dc: e.activation(out=a_[:, 0:n], in_=r_[:, 0:n], func=AF.Exp, scale=cols[:, 44 + dc:45 + dc]),
                         [rk, colsk], [ak])
                    P.op(A, lambda e, n=n, dc=dc: e.activation(out=b_[:, 0:n], in_=r_[:, 0:n], func=AF.Exp, scale=cols[:, 52 + dc:53 + dc]),
                         [rk, colsk], [bk])
                    P.op(V, lambda e, n=n: e.tensor_scalar(out=b_[:, 0:n], in0=b_[:, 0:n], scalar1=-1.0, scalar2=1.0, op0=ALU.mult, op1=ALU.add),
                         [bk], [bk])
                    P.op(A, lambda e, n=n: e.sqrt(out=b_[:, 0:n], in_=b_[:, 0:n]), [bk], [bk])
                    P.op(V, lambda e, n=n: e.tensor_tensor(out=b_[:, 0:n], in0=b_[:, 0:n], in1=i_[:, 0:n], op=ALU.mult), [bk, ik], [bk])
                    P.op(G, lambda e, n=n: e.tensor_tensor(out=b_[:, 0:n], in0=b_[:, 0:n], in1=uc[:, 0:n], op=ALU.mult), [bk, uck], [bk])
                    if d_ == 0:
                        P.op(V, lambda e, n=n: e.tensor_tensor_scan(out=h_[:, 0:n], data0=a_[:, 0:n], data1=b_[:, 0:n], initial=state[:, 0:1],
                                                                    op0=ALU.mult, op1=ALU.add), [ak, bk, statek], [hk])
                        P.op(V, lambda e, n=n: e.tensor_copy(out=state[:], in_=h_[:, n - 1:n]), [hk], [statek])
                        P.dma(HF_s[rows, s0:s0 + n], h_[:, 0:n], reads=[hk], writes=["HF_s"], eng="gpsimd")
                    else:
                        P.op(V, lambda e, n=n: e.tensor_tensor_scan(out=h_[:, 0:n][:, ::-1], data0=a_[:, 0:n][:, ::-1], data1=b_[:, 0:n][:, ::-1],
                                                                    initial=state[:, 0:1], op0=ALU.mult, op1=ALU.add), [ak, bk, statek], [hk])
                        P.op(V, lambda e: e.tensor_copy(out=state[:], in_=h_[:, 0:1]), [hk], [statek])
                        P.dma(hf[:, 0:n], HF_s[rows, s0:s0 + n], reads=["HF_s"], writes=[hfk])
                        P.dma(gg[:, 0:n], G_s[rows, s0:s0 + n], reads=["G_s"], writes=[ggk])
                        P.op(G, lambda e, n=n: e.tensor_tensor(out=hf[:, 0:n], in0=hf[:, 0:n], in1=h_[:, 0:n], op=ALU.add), [hfk, hk], [hfk])
                        P.op(V, lambda e, n=n: e.tensor_tensor(out=mx[:, 0:n], in0=hf[:, 0:n], in1=gg[:, 0:n], op=ALU.mult), [hfk, ggk], [mxk])
                        P.dma(MIXA[rows, s0:s0 + n], mx[:, 0:n], reads=[mxk], writes=["MIXA"], eng="gpsimd")
    P.barrier()
    if stop_after == "P2":
        return finish(k, st_all)

    def attn_block(qT_ap, qk, keyts, nsub, dv, scale, pT, pTk, on_out):
        nk = len(keyts)
        nq = nsub * 128
        for i, (kT_ap, kkeys, v_ap, vkeys, mask) in enumerate(keyts):
            pb, pk = pbank()
            P.op(TE, lambda e, pb=pb, kT_ap=kT_ap: e.matmul(pb[:, 0:nq], lhsT=kT_ap, rhs=qT_ap, start=True, stop=True), kkeys + qk, pk)
            P.op(A, lambda e, pb=pb, i=i: e.activation(out=pT[:, i, 0:nq], in_=pb[:, 0:nq], func=AF.Exp, scale=scale), pk, [pTk + str(i)])
            if mask is not None:
                P.op(G, lambda e, i=i, mask=mask: e.tensor_tensor(out=pT[:, i, 0:nq], in0=pT[:, i, 0:nq], in1=mask[0][:, 0:nq], op=ALU.mult),
                     [pTk + str(i), mask[1]], [pTk + str(i)])
        for sub in range(nsub):
            po, pok = pbank()
            for i, (kT_ap, kkeys, v_ap, vkeys, mask) in enumerate(keyts):
                P.op(TE, lambda e, po=po, i=i, sub=sub, v_ap=v_ap: e.matmul(po[:, 0:dv + 1], lhsT=pT[:, i, sub * 128:(sub + 1) * 128], rhs=v_ap,
                                                                           start=(i == 0), stop=(i == nk - 1)), [pTk + str(i)] + vkeys, pok)
            on_out(sub, po[:, 0:dv + 1], pok)

    with scope() as st:
        es, esk = load_bc(st, "es", e_sink_d, 8)
        P.op(A, lambda e: e.activation(out=es[:], in_=es[:], func=AF.Exp), [esk], [esk])
        mprev, mprevk = sb(st, "mprev", [128, 512], BF16)
        mnext, mnextk = sb(st, "mnext", [128, 512], BF16)
        P.dma(mprev[:], mask_d[0], writes=[mprevk], eng="gpsimd")
        P.dma(mnext[:], mask_d[1], writes=[mnextk], eng="gpsimd")
        kt_sb, ktk = sb(st, "kt_sb", [64, T], BF16)
        v_sb, vk_ = sb(st, "v_sb", [128, T // 128, 65], BF16)
        qt_sb, qtk = sb(st, "qt_sb", [64, 4, 128], BF16)
        pT, pTk = sb(st, "pT", [128, 5, 512], BF16)
        den, denk = sb(st, "den", [128, 8])
        att, attk = sb(st, "att", [128, 256])
        for j in range(2):
            P.dma(kt_sb[:], KT_s[j], reads=["QKT"], writes=[ktk])
            P.dma(v_sb[:], V_s[:, j, :].rearrange("(a p) d -> p a d", p=128), reads=["V_s"], writes=[vk_], eng="gpsimd")
            for (t0, s_) in tok_tiles():
                P.dma(qt_sb[:], QT_s[4 * j:4 * j + 4, :, t0:t0 + 128].rearrange("h d t -> d h t"), reads=["QKT"], writes=[qtk])
                keyts = []

                def kt(tile_idx, mask):
                    keyts.append((kt_sb[:, tile_idx * 128:(tile_idx + 1) * 128], [ktk], v_sb[:, tile_idx, :], [vk_], mask))
                if s_ == 0:
                    n = t0 // 128
                    if n > 0:
                        kt(n - 1, (mprev, mprevk))
                    kt(n, None)
                    if n < NT_L - 1:
                        kt(n + 1, (mnext, mnextk))
                for c_ in range(NT_C):
                    kt(NT_L + c_, None)

                def on_out(sub, po, pok):
                    hh = 4 * j + sub
                    P.op(V, lambda e: e.tensor_tensor(out=den[:, 0:1], in0=po[:, 64:65], in1=es[:, hh:hh + 1], op=ALU.add), pok + [esk], [denk])
                    P.op(V, lambda e: e.reciprocal(out=den[:, 1:2], in_=den[:, 0:1]), [denk], [denk])
                    P.op(A, lambda e: e.activation(out=att[:, sub * 64:(sub + 1) * 64], in_=po[:, 0:64], func=AF.Identity, scale=den[:, 1:2]),
                         pok + [denk], [attk])
                attn_block(qt_sb[:].rearrange("d h t -> d (h t)"), [qtk], keyts, 4, 64, 0.125, pT, pTk, on_out)
                P.dma(ATT[t0:t0 + 128, j * 256:(j + 1) * 256], att[:], reads=[attk], writes=["ATT"], eng="gpsimd")
    P.barrier()
    if stop_after == "P3":
        return finish(k, st_all)

    def mix_loader_even(t0, s_, mixT, mixk, st):
        if "att" not in st.ld:
            st.ld["att"] = sb(st, "attin", [128, 512])
        att_in, attink = st.ld["att"]
        P.dma(mixT[:, 0:4, :], MIXA[:, t0:t0 + 128].rearrange("(c p) t -> p c t", p=128), reads=["MIXA"], writes=[mixk])
        P.dma(att_in[:], ATT[t0:t0 + 128, :], reads=["ATT"], writes=[attink], eng="gpsimd")
        pb, pk = pbank()
        for c_ in range(4):
            P.op(TE, lambda e, c_=c_, pb=pb: e.transpose(pb[:, c_ * 128:(c_ + 1) * 128], att_in[:, c_ * 128:(c_ + 1) * 128], ident[:]),
                 [attink, ident_k], pk)
        P.op(V, lambda e, pb=pb: e.tensor_copy(out=mixT[:, 4:8, :], in_=pb[:, 0:512].rearrange("p (c t) -> p c t", c=4)), pk, [mixk])

    mixer_epilogue(0, e_w_out_d, e_b_out_d, x0_src, mix_loader_even, True)
    if stop_after == "P4":
        return finish(k, st_all)
    moe_phase(0, True, lambda t0: XB[t0:t0 + 128, :], False)
    if stop_after == "P5":
        return finish(k, st_all)

    with scope() as st:
        NA = SH // 128
        xi_f, xik = sb(st, "xi", [128, NA])
        xi = xi_f.bitcast(I32)
        col_dma(xi, xh_idx_d.rearrange("(a p) -> p a", p=128), [xik])
        xg = [sb(st, f"xg{i}", [128, D]) for i in range(2)]
        for a in range(NA):
            gt_, gk_ = xg[a % 2]
            P.op("gpsimd", lambda e, a=a, gt_=gt_: e.indirect_dma_start(
                out=gt_[:, :], out_offset=None, in_=XBp[:, :],
                in_offset=bass.IndirectOffsetOnAxis(ap=xi[:, a:a + 1], axis=0),
                bounds_check=128 + T - 1, oob_is_err=False), [xik, "XB", "XBpad"], [gk_], dma=True)
            P.dma(XH[a * 128:(a + 1) * 128, :], gt_[:], reads=[gk_], writes=["XH"], eng="sync")
    P.barrier()

    def x1_src(t0, s_):
        return XH[64 + t0:64 + t0 + 128, :]

    MSCALE = 96.0 ** -0.5
    with scope() as st:
        owin, owink = sb(st, "owin", [128, 8, 1440], BF16)
        for kc in range(8):
            P.dma(owin[:, kc, :], o_w_in_d[kc * 128:(kc + 1) * 128, :], writes=[owink])
        wkr, wkrk = sb(st, "wkr", [128, 8, 32], BF16)
        P.dma(wkr[:], o_w_kpe_rot_d.rearrange("(kc p) n -> p kc n", p=128), writes=[wkrk])
        wuq, wuqk = sb(st, "wuq", [128, 2, 8, 192], BF16)
        P.dma(wuq[:], o_w_uq_d.rearrange("(kc p) h n -> p kc h n", p=128), writes=[wuqk])
        wuk, wukk = sb(st, "wuk", [128, 512], BF16)
        P.dma(wuk[:], o_w_uk_d, writes=[wukk])
        wuv, wuvk = sb(st, "wuv", [128, 512], BF16)
        P.dma(wuv[:], o_w_uv_d, writes=[wuvk])
        bq, bqk = load_bc(st, "bq", o_b_in_d[0:384], 384)
        qn, qnk = load_bc(st, "qn", o_q_norm_d, 256)
        kvn, kvnk = load_bc(st, "kvn", o_kv_norm_d, 128)
        zmk, zmkk = load_bc(st, "zmk", zmask_d, SH)
        oc, ock = sb(st, "oc", [128, 12])
        col_dma(oc[0:32, 0:1], o_b_in_d[384:416].rearrange("(p o) -> p o", o=1), [ock])
        col_dma(oc[0:32, 1:2], o_b_kpe_rot_d.rearrange("(p o) -> p o", o=1), [ock])
        col_dma(oc[:, 2:10], o_b_in_d[416:1440].rearrange("(c p) -> p c", p=128), [ock])
        xt_r = ring(st, "xt", [128, D])
        hT_r = ring(st, "hT", [128, 8, 512], BF16)
        tq_r = ring(st, "tq", [128, 384])
        sq_r = ring(st, "sq", [128, 384])
        rs_r = ring(st, "rs", [128, 8])
        cqnT_r = ring(st, "cqnT", [128, 2, 512], BF16)
        ckvT_r = ring(st, "ckvT", [128, 512], BF16)
        cos96, cos96k = sb(st, "cos96", [96, 512])
        sin96, sin96k = sb(st, "sin96", [96, 512])
        cos32, cos32k = sb(st, "cos32", [32, 512])
        sin32, sin32k = sb(st, "sin32", [32, 512])
        f1_r = ring(st, "f1", [128, 512])
        f2_r = ring(st, "f2", [128, 512])
        ob_r = ring(st, "ob", [128, 512], BF16, n=3)
        vt_r = ring(st, "vt1", [128, 8, 65], BF16, init=1.0)

        def norm_part(c0, c1, rcol, ncol, nbc, nbck):
            w = c1 - c0
            P.op(A, lambda e: e.activation(out=sq[:, c0:c1], in_=tq[:, c0:c1], func=AF.Square, accum_out=rs[:, rcol:rcol + 1]), [tqk], [sqk, rsk])
            P.op(V, lambda e: e.tensor_scalar(out=rs[:, rcol + 2:rcol + 3], in0=rs[:, rcol:rcol + 1], scalar1=1.0 / w, scalar2=LN_EPS,
                                              op0=ALU.mult, op1=ALU.add), [rsk], [rsk])
            P.op(A, lambda e: e.sqrt(out=rs[:, rcol + 4:rcol + 5], in_=rs[:, rcol + 2:rcol + 3]), [rsk], [rsk])
            P.op(V, lambda e: e.reciprocal(out=rs[:, rcol + 6:rcol + 7], in_=rs[:, rcol + 4:rcol + 5]), [rsk], [rsk])
            P.op(V, lambda e: e.scalar_tensor_tensor(out=sq[:, c0:c1], in0=tq[:, c0:c1], scalar=rs[:, rcol + 6:rcol + 7], in1=nbc[:],
                                                     op0=ALU.mult, op1=ALU.mult), [tqk, rsk, nbck], [sqk])

        groups = [(g0, 0, min(512, S - g0)) for g0 in range(0, S, 512)] + [(S + g0, 1, min(512, C - g0)) for g0 in range(0, C, 512)]
        for (g0, s_, ng) in groups:
            hT, hTk = hT_r()
            ckvT, ckvTk = ckvT_r()
            for tt in range(ng // 128):
                t0 = g0 + tt * 128
                xt, xk = xt_r()
                tq, tqk = tq_r()
                sq, sqk = sq_r()
                rs, rsk = rs_r()
                vt, vtk = vt_r()
                P.dma(xt[:], XB[t0:t0 + 128, :], reads=["XB"], writes=[xk])
                transpose_mod(xt, xk, hT, hTk, mcol[:, 1, s_, 1, :], mcol[:, 1, s_, 0, :], mcol_k, tt * 128)
                pb, pk = pbank()
                for kc in range(8):
                    P.op(TE, lambda e, kc=kc: e.matmul(pb[:, 0:128], lhsT=hT[:, kc, tt * 128:(tt + 1) * 128], rhs=owin[:, kc, 256:384],
                                                       start=(kc == 0), stop=(kc == 7)), [hTk, owink], pk)
                P.op(V, lambda e: e.tensor_tensor(out=tq[:, 256:384], in0=pb[:, 0:128], in1=bq[:, 256:384], op=ALU.add), pk + [bqk], [tqk])
                norm_part(256, 384, 1, 128, kvn, kvnk)
                pt, ptk = pbank()
                P.op(TE, lambda e: e.transpose(pt[:, 0:128], sq[:, 256:384], ident[:]), [sqk, ident_k], ptk)
                P.op(A, lambda e: e.copy(out=ckvT[:, tt * 128:(tt + 1) * 128], in_=pt[:, 0:128]), ptk, [ckvTk])
                pv, pvk = pbank()
                P.op(TE, lambda e: e.matmul(pv[:, 0:512], lhsT=ckvT[:, tt * 128:(tt + 1) * 128], rhs=wuv[:], start=True, stop=True), [ckvTk, wuvk], pvk)
                P.op(V, lambda e: e.tensor_copy(out=vt[:, :, 0:64], in_=pv[:, 0:512].rearrange("p (h d) -> p h d", h=8)), pvk, [vtk])
                P.dma(VM[t0:t0 + 128], vt[:], reads=[vtk], writes=["VM"], eng="gpsimd")
            for c_ in range(4):
                pkn, pknk = pbank()
                ob, obk = ob_r()
                P.op(TE, lambda e, c_=c_: e.matmul(pkn[:, 0:ng], lhsT=wuk[:, c_ * 128:(c_ + 1) * 128], rhs=ckvT[:, 0:ng], start=True, stop=True),
                     [wukk, ckvTk], pknk)
                P.op(A, lambda e: e.copy(out=ob[:, 0:ng], in_=pkn[:, 0:ng]), pknk, [obk])
                for hh in range(2):
                    P.dma(KM[2 * c_ + hh, 0:64, g0:g0 + ng], ob[hh * 64:(hh + 1) * 64, 0:ng], reads=[obk], writes=["KM"], eng="gpsimd")
            pp, ppk = pbank()
            ob, obk = ob_r()
            f1, f1k = f1_r()
            f2, f2k = f2_r()
            for kc in range(8):
                P.op(TE, lambda e, kc=kc: e.matmul(pp[0:32, 0:ng], lhsT=owin[:, kc, 384:416], rhs=hT[:, kc, 0:ng], start=(kc == 0), stop=(kc == 7)),
                     [owink, hTk], ppk)
            if s_ == 0:
                P.dma(cos32[:, 0:ng], rope_k_d[0, :, g0:g0 + ng], writes=[cos32k])
                P.dma(sin32[:, 0:ng], rope_k_d[1, :, g0:g0 + ng], writes=[sin32k])
                pr, prk = pbank()
                for kc in range(8):
                    P.op(TE, lambda e, kc=kc: e.matmul(pr[0:32, 0:ng], lhsT=wkr[:, kc, :], rhs=hT[:, kc, 0:ng], start=(kc == 0), stop=(kc == 7)),
                         [wkrk, hTk], prk)
                P.op(V, lambda e: e.scalar_tensor_tensor(out=f1[0:32, 0:ng], in0=pp[0:32, 0:ng], scalar=oc[0:32, 0:1], in1=cos32[:, 0:ng],
                                                         op0=ALU.add, op1=ALU.mult), ppk + [ock, cos32k], [f1k])
                P.op(V, lambda e: e.scalar_tensor_tensor(out=f2[0:32, 0:ng], in0=pr[0:32, 0:ng], scalar=oc[0:32, 1:2], in1=sin32[:, 0:ng],
                                                         op0=ALU.add, op1=ALU.mult), prk + [ock, sin32k], [f2k])
                P.op(G, lambda e: e.tensor_tensor(out=ob[0:32, 0:ng], in0=f1[0:32, 0:ng], in1=f2[0:32, 0:ng], op=ALU.add), [f1k, f2k], [obk])
            else:
                P.op(A, lambda e: e.activation(out=ob[0:32, 0:ng], in_=pp[0:32, 0:ng], func=AF.Identity, bias=oc[0:32, 0:1], scale=1.0),
                     ppk + [ock], [obk])
            for h in range(8):
                P.dma(KM[h, 64:96, g0:g0 + ng], ob[0:32, 0:ng], reads=[obk], writes=["KM"], eng="gpsimd")

        for g0 in range(0, SH, 512):
            ng = min(512, SH - g0)
            hT, hTk = hT_r()
            cqnT, cqnTk = cqnT_r()
            for tt in range(ng // 128):
                r0 = g0 + tt * 128
                xt, xk = xt_r()
                tq, tqk = tq_r()
                sq, sqk = sq_r()
                rs, rsk = rs_r()
                P.dma(xt[:], XH[r0:r0 + 128, :], reads=["XH"], writes=[xk])
                transpose_mod(xt, xk, hT, hTk, mcol[:, 1, 0, 1, :], mcol[:, 1, 0, 0, :], mcol_k, tt * 128)
                pb, pk = pbank()
                for kc in range(8):
                    P.op(TE, lambda e, kc=kc: e.matmul(pb[:, 0:256], lhsT=hT[:, kc, tt * 128:(tt + 1) * 128], rhs=owin[:, kc, 0:256],
                                                       start=(kc == 0), stop=(kc == 7)), [hTk, owink], pk)
                P.op(V, lambda e: e.tensor_tensor(out=tq[:, 0:256], in0=pb[:, 0:256], in1=bq[:, 0:256], op=ALU.add), pk + [bqk], [tqk])
                norm_part(0, 256, 0, 256, qn, qnk)
                pt, ptk = pbank()
                for c_ in range(2):
                    P.op(TE, lambda e, c_=c_: e.transpose(pt[:, c_ * 128:(c_ + 1) * 128], sq[:, c_ * 128:(c_ + 1) * 128], ident[:]), [sqk, ident_k], ptk)
                P.op(V, lambda e: e.tensor_copy(out=cqnT[:, :, tt * 128:(tt + 1) * 128], in_=pt[:, 0:256].rearrange("p (c t) -> p c t", c=2)),
                     ptk, [cqnTk])
            P.dma(cos96[:, 0:ng], rope_q_d[0, :, g0:g0 + ng], writes=[cos96k])
            P.dma(sin96[:, 0:ng], rope_q_d[1, :, g0:g0 + ng], writes=[sin96k])
            for h in range(8):
                pq, pqk = pbank()
                pr, prk = pbank()
                f1, f1k = f1_r()
                f2, f2k = f2_r()
                ob, obk = ob_r()
                for kc in range(2):
                    P.op(TE, lambda e, kc=kc: e.matmul(pq[0:96, 0:ng], lhsT=wuq[:, kc, h, 0:96], rhs=cqnT[:, kc, 0:ng], start=(kc == 0), stop=(kc == 1)),
                         [wuqk, cqnTk], pqk)
                for kc in range(2):
                    P.op(TE, lambda e, kc=kc: e.matmul(pr[0:96, 0:ng], lhsT=wuq[:, kc, h, 96:192], rhs=cqnT[:, kc, 0:ng], start=(kc == 0), stop=(kc == 1)),
                         [wuqk, cqnTk], prk)
                P.op(V, lambda e: e.tensor_tensor(out=f1[0:96, 0:ng], in0=pq[0:96, 0:ng], in1=cos96[:, 0:ng], op=ALU.mult), pqk + [cos96k], [f1k])
                P.op(V, lambda e: e.tensor_tensor(out=f2[0:96, 0:ng], in0=pr[0:96, 0:ng], in1=sin96[:, 0:ng], op=ALU.mult), prk + [sin96k], [f2k])
                P.op(G, lambda e: e.tensor_tensor(out=ob[0:96, 0:ng], in0=f1[0:96, 0:ng], in1=f2[0:96, 0:ng], op=ALU.add), [f1k, f2k], [obk])
                P.dma(QM[h, :, g0:g0 + ng], ob[0:96, 0:ng], reads=[obk], writes=["QM"], eng="gpsimd")
            for j in range(4):
                pa, pak = pbank()
                pg, pgk = pbank()
                f1, f1k = f1_r()
                f2, f2k = f2_r()
                ob, obk = ob_r()
                for kc in range(8):
                    P.op(TE, lambda e, kc=kc: e.matmul(pa[:, 0:ng], lhsT=owin[:, kc, 416 + j * 128:544 + j * 128], rhs=hT[:, kc, 0:ng],
                                                       start=(kc == 0), stop=(kc == 7)), [owink, hTk], pak)
                for kc in range(8):
                    P.op(TE, lambda e, kc=kc: e.matmul(pg[:, 0:ng], lhsT=owin[:, kc, 928 + j * 128:1056 + j * 128], rhs=hT[:, kc, 0:ng],
                                                       start=(kc == 0), stop=(kc == 7)), [owink, hTk], pgk)
                P.op(A, lambda e: e.activation(out=f1[:, 0:ng], in_=pg[:, 0:ng], func=AF.Sigmoid, bias=oc[:, 6 + j:7 + j], scale=1.0),
                     pgk + [ock], [f1k])
                P.op(V, lambda e: e.scalar_tensor_tensor(out=f2[:, 0:ng], in0=pa[:, 0:ng], scalar=oc[:, 2 + j:3 + j], in1=f1[:, 0:ng],
                                                         op0=ALU.add, op1=ALU.mult), pak + [ock, f1k], [f2k])
                P.op(G, lambda e: e.tensor_tensor(out=ob[:, 0:ng], in0=f2[:, 0:ng], in1=zmk[:, g0:g0 + ng], op=ALU.mult), [f2k, zmkk], [obk])
                P.dma(ZC[j * 128:(j + 1) * 128, g0:g0 + ng], ob[:, 0:ng], reads=[obk], writes=["ZC"], eng="gpsimd")
    P.barrier()
    if stop_after == "Q1":
        return finish(k, st_all)

    with scope() as st:
        identb, identbk = sb(st, "identb", [128, 128], BF16)
        P.op(V, lambda e: e.tensor_copy(out=identb[:], in_=ident[:]), [ident_k], [identbk])
        dwc, dwck = sb(st, "dwc", [128, 4, 32])
        for j in range(4):
            col_dma(dwc[:, j, 0:31], o_dw_w_d[:, j * 128:(j + 1) * 128].rearrange("k p -> p k"), [dwck])
        col_dma(dwc[:, :, 31], o_dw_b_d.rearrange("(c p) -> p c", p=128), [dwck])
        dg, dgk = sb(st, "dg", [128, 4, 31, 128], BF16)
        for j in range(4):
            for kk in range(31):
                P.op(V if kk % 2 else G, lambda e: e.tensor_scalar_mul(out=dg[:, j, kk, :], in0=identb[:], scalar1=dwc[:, j, kk:kk + 1]),
                     [identbk, dwck], [dgk])
        cg, cgk = load_bc(st, "cg", o_cln_g_d, 512)
        cb, cbk = load_bc(st, "cb", o_cln_b_d, 512)
        zw, zwk = sb(st, "zw", [128, 512 + 30], BF16)
        yc, yck = sb(st, "yc", [128, 4, 512])
        yt, ytk = sb(st, "yt", [128, 512])
        yo, yok = sb(st, "yo", [128, 512])
        small = {"stats": sb(st, "stats3", [128, 2, 6]), "mv": sb(st, "mv3", [128, 8])}
        for g0 in range(0, SQ, 512):
            ng = min(512, SQ - g0)
            for j in range(4):
                P.dma(zw[:, 0:ng + 30], ZC[j * 128:(j + 1) * 128, 49 + g0:49 + g0 + ng + 30], reads=["ZC"], writes=[zwk])
                pc, pck = pbank()
                for kk in range(31):
                    P.op(TE, lambda e, kk=kk: e.matmul(pc[:, 0:ng], lhsT=dg[:, j, kk, :], rhs=zw[:, kk:kk + ng], start=(kk == 0), stop=(kk == 30)),
                         [dgk, zwk], pck)
                P.op(A, lambda e: e.activation(out=yc[:, j, 0:ng], in_=pc[:, 0:ng], func=AF.Identity, bias=dwc[:, j, 31:32], scale=1.0),
                     pck + [dwck], [yck])
            for tt in range(ng // 128):
                pt, ptk = pbank()
                for j in range(4):
                    P.op(TE, lambda e, j=j: e.transpose(pt[:, j * 128:(j + 1) * 128], yc[:, j, tt * 128:(tt + 1) * 128], ident[:]), [yck, ident_k], ptk)
                P.op(V, lambda e: e.tensor_copy(out=yt[:], in_=pt[:, 0:512]), ptk, [ytk])
                layer_norm_tile(small, yt, ytk, cg, cgk, cb, cbk, yo, yok, width=512)
                P.op(A, lambda e: e.activation(out=yo[:], in_=yo[:], func=AF.Silu), [yok], [yok])
                t0 = g0 + tt * 128
                P.dma(CONV[t0:t0 + 128, :], yo[:], reads=[yok], writes=["CONV"], eng="gpsimd")
    P.barrier()
    if stop_after == "Q2":
        return finish(k, st_all)

    with scope() as st:
        NKT = T // 128
        km, kmk = sb(st, "km", [96, T], BF16)
        vm, vmk = sb(st, "vm", [128, NKT, 65], BF16)
        qm, qmk = sb(st, "qm", [96, 512], BF16)
        pT, pTk = sb(st, "pTm", [128, NKT, 512], BF16)
        den, denk = sb(st, "den1", [128, 2])
        att, attk = sb(st, "att1", [128, 64])
        for h in range(8):
            P.dma(km[:], KM[h], reads=["KM"], writes=[kmk])
            P.dma(vm[:], VM[:, h, :].rearrange("(a p) d -> p a d", p=128), reads=["VM"], writes=[vmk], eng="gpsimd")
            for g0 in range(0, SQ, 512):
                ng = min(512, SQ - g0)
                P.dma(qm[:, 0:ng], QM[h, :, 64 + g0:64 + g0 + ng], reads=["QM"], writes=[qmk])
                keyts = [(km[:, i * 128:(i + 1) * 128], [kmk], vm[:, i, :], [vmk], None) for i in range(NKT)]

                def on_out(sub, po, pok):
                    P.op(V, lambda e: e.reciprocal(out=den[:, 0:1], in_=po[:, 64:65]), pok, [denk])
                    P.op(A, lambda e: e.activation(out=att[:], in_=po[:, 0:64], func=AF.Identity, scale=den[:, 0:1]), pok + [denk], [attk])
                    t0 = g0 + sub * 128
                    P.dma(ATT[t0:t0 + 128, h * 64:(h + 1) * 64], att[:], reads=[attk], writes=["ATT"], eng="gpsimd")
                attn_block(qm[:, 0:ng], [qmk], keyts, ng // 128, 64, MSCALE, pT, pTk, on_out)
    P.barrier()
    if stop_after == "Q3":
        return finish(k, st_all)

    def mix_loader_odd(t0, s_, mixT, mixk, st):
        if "t" not in st.ld:
            st.ld["t"] = sb(st, "mixin", [128, D])
        mi, mik = st.ld["t"]
        P.dma(mi[:, 0:512], ATT[t0:t0 + 128, :], reads=["ATT"], writes=[mik])
        P.dma(mi[:, 512:1024], CONV[t0:t0 + 128, :], reads=["CONV"], writes=[mik], eng="gpsimd")
        pb, pk = pbank(2)
        for c_ in range(8):
            P.op(TE, lambda e, c_=c_: e.transpose(pb[:, c_ * 128:(c_ + 1) * 128], mi[:, c_ * 128:(c_ + 1) * 128], ident[:]), [mik, ident_k], pk)
        P.op(V, lambda e: e.tensor_copy(out=mixT[:, 0:4, :], in_=pb[:, 0:512].rearrange("p (c t) -> p c t", c=4)), pk, [mixk])
        P.op(A, lambda e: e.copy(out=mixT[:, 4:8, :], in_=pb[:, 512:1024].rearrange("p (c t) -> p c t", c=4)), pk, [mixk])

    mixer_epilogue(1, o_w_out_d, o_b_out_d, x1_src, mix_loader_odd, False, n_lat=SQ)
    if stop_after == "Q4":
        return finish(k, st_all)
    moe_phase(1, False, lambda t0: out_d[t0:t0 + 128, :], True, n_lat=SQ)
    return finish(k, st_all)


def finish(k, st_all):
    k.P.emit()
    st_all.close()
    return k


GRID_W = 64
ROPE_THETA = 10000.0


def _rot_perm(n_heads, hd):
    q = hd // 4
    idx = []
    for h in range(n_heads):
        b = h * hd
        idx += list(range(b + q, b + 2 * q)) + list(range(b, b + q)) + list(range(b + 3 * q, b + 4 * q)) + list(range(b + 2 * q, b + 3 * q))
    return np.array(idx)


def _rope_tables(S, hd, t=None):
    q = hd // 4
    if t is None:
        t = np.arange(S)
    S = len(t)
    row, col = t // GRID_W, t % GRID_W
    invf = ROPE_THETA ** (-np.arange(q, dtype=np.float64) / q)
    cos = np.zeros((hd, S)); sin = np.zeros((hd, S))
    for d in range(hd):
        pos = row if d < 2 * q else col
        dd = d % (2 * q)
        j = dd % q
        ang = pos.astype(np.float32).astype(np.float64) * np.float32(invf[j]).astype(np.float64)
        cos[d] = np.cos(ang)
        sin[d] = np.sin(ang) * (-1.0 if dd < q else 1.0)
    return cos.astype(np.float32), sin.astype(np.float32)


def prep_core_inputs(inp, b, S, h=0):
    f = lambda a: np.ascontiguousarray(np.asarray(a, dtype=np.float32))
    m = {}
    m["x"] = f(inp["x"][b])
    m["ctx"] = f(inp["ctx"][b])
    m["cvec"] = f(np.stack([inp["c"][b], inp["c_ctx"]]))
    m["ident"] = np.eye(128, dtype=np.float32)
    m["w_mod"] = f(inp["w_mod"]); m["b_mod"] = f(inp["b_mod"])
    m["ln_g"] = f(inp["ln_g"]); m["ln_b"] = f(inp["ln_b"])
    w_in = np.asarray(inp["e_w_in"][0]); b_in = np.asarray(inp["e_b_in"][0])
    perm = _rot_perm(10, 64) + 1024
    m["e_w_in"] = f(np.concatenate([w_in, w_in[:, perm]], axis=1))
    m["e_b_in"] = f(np.concatenate([b_in, b_in[perm]]))
    m["e_conv_w"] = f(inp["e_conv_w"][0]); m["e_conv_b"] = f(inp["e_conv_b"][0])
    m["e_lru_wa"] = f(inp["e_lru_wa"][0]); m["e_lru_ba"] = f(inp["e_lru_ba"][0])
    m["e_lru_wx"] = f(inp["e_lru_wx"][0]); m["e_lru_bx"] = f(inp["e_lru_bx"][0])
    m["e_lru_lambda"] = f(inp["e_lru_lambda"][0]); m["e_sink"] = f(inp["e_sink"][0])
    m["e_w_out"] = f(inp["e_w_out"][0]); m["e_b_out"] = f(inp["e_b_out"][0])
    c64, s64 = _rope_tables(S, 64)
    m["rope_e"] = f(np.stack([np.concatenate([c64, c64]), np.concatenate([s64, s64])]))
    j = np.arange(128)[:, None]; i = np.arange(128)[None, :]
    mp = (j >= i).astype(np.float32); mn = (j <= i).astype(np.float32)
    m["swa_mask"] = f(np.stack([np.tile(mp, (1, 4)), np.tile(mn, (1, 4))]))
    ow = np.asarray(inp["o_w_in"][0]); ob = np.asarray(inp["o_b_in"][0])
    m["o_w_in"] = f(ow); m["o_b_in"] = f(ob)
    m["o_q_norm"] = f(inp["o_q_norm"][0]); m["o_kv_norm"] = f(inp["o_kv_norm"][0])
    wuq = np.asarray(inp["o_w_uq"][0]).reshape(256, 8, 96)
    p32 = _rot_perm(1, 32)
    ext = np.zeros((256, 8, 192), np.float32)
    ext[:, :, 0:96] = wuq
    ext[:, :, 160:192] = wuq[:, :, 64:96][:, :, p32]
    m["o_w_uq"] = f(ext)
    m["o_w_uk"] = f(inp["o_w_uk"][0]); m["o_w_uv"] = f(inp["o_w_uv"][0])
    m["o_w_kpe_rot"] = f(ow[:, 384:416][:, p32]); m["o_b_kpe_rot"] = f(ob[384:416][p32])
    c32, s32 = _rope_tables(S, 32)
    m["rope_k"] = f(np.stack([c32, s32]))
    SQ = S // 2
    SH = SQ + 128
    tok = h * SQ - 64 + np.arange(SH)
    inside = (tok >= 0) & (tok < S)
    cq, sq_ = _rope_tables(S, 32, np.clip(tok, 0, S - 1))
    m["rope_q"] = f(np.stack([np.concatenate([np.ones((64, SH), np.float32), cq]), np.concatenate([np.zeros((64, SH), np.float32), sq_])]))
    m["zmask"] = f(inside.astype(np.float32))
    m["xh_idx"] = (64 + h * SQ + np.arange(SH)).astype(np.int32)
    m["o_dw_w"] = f(inp["o_dw_w"][0]); m["o_dw_b"] = f(inp["o_dw_b"][0])
    m["o_cln_g"] = f(inp["o_cln_g"][0]); m["o_cln_b"] = f(inp["o_cln_b"][0])
    m["o_w_out"] = f(inp["o_w_out"][0]); m["o_b_out"] = f(inp["o_b_out"][0])
    m["moe_w_gr"] = f(np.concatenate([inp["moe_w_group"], inp["moe_w_router"]], axis=2))
    m["moe_b_gr"] = f(np.concatenate([inp["moe_b_group"], inp["moe_b_router"]], axis=1))
    m["moe_w1"] = f(inp["moe_w1"]); m["moe_w3"] = f(inp["moe_w3"]); m["moe_w2"] = f(inp["moe_w2"])
    return m


_CACHE = {}


def kernel(**inputs):
    B, S, _ = inputs["x"].shape
    C = inputs["ctx"].shape[1]
    key = (S, C)
    if key not in _CACHE:
        _CACHE[key] = build(S, C)
    kk = _CACHE[key]
    shared = None
    in_maps = []
    for core in range(8):
        b, h = core % B, core // B
        m = prep_core_inputs(inputs, b, S, h)
        in_maps.append(m)
    res = run_bass_kernel_spmd(kk.nc, in_maps, core_ids=list(range(8)))
    SQ = S // 2
    out = np.empty((B, S, D), np.float32)
    for core in range(8):
        b, h = core % B, core // B
        out[b, h * SQ:(h + 1) * SQ] = np.asarray(res.results[core]["out"], dtype=np.float32)
    return out
```

```python
import contextlib
import numpy as np
import concourse.bass as bass
import concourse.mybir as mybir
from concourse.bass_utils import run_bass_kernel_spmd

F32 = mybir.dt.float32
BF16 = mybir.dt.bfloat16
I32 = mybir.dt.int32
AF = mybir.ActivationFunctionType
ALU = mybir.AluOpType
AX = mybir.AxisListType

D = 1024
MERGE_N = 1
PAIR_EXP = True
SEM_LIMIT = 30000
DN_ALPHA = 4.0 ** 0.25
LN_EPS = 1e-6


class _Rec:
    def __getattr__(self, name):
        def f(*a, **kw):
            self.call = (name, a, kw)
            return self
        return f


class Prog:
    ENGS = ("tensor", "vector", "scalar", "gpsimd", "sync")

    def __init__(self, nc, n_dma_sems=14):
        self.nc = nc
        self.ops = {e: [] for e in self.ENGS}
        self.sems = {}
        self.sem_order = []
        self.cnt = {e: 0 for e in self.ENGS}
        self.epoch = {e: 0 for e in self.ENGS}
        self.known = {e: {} for e in self.ENGS}
        self.last_w = {}
        self.readers = {}
        self.dma_pool = {e: [[f"d_{e}_{i}", 0] for i in range(n_dma_sems)] for e in ("sync", "gpsimd", "scalar")}
        self.dma_rr = {e: 0 for e in ("sync", "gpsimd", "scalar")}
        self.out_tokens = []
        self.last_tok = {}
        self.nops = 0

    def _sem(self, name):
        if name not in self.sems:
            self.sems[name] = None
            self.sem_order.append(name)
        return name

    limit = None

    defer_list = None

    def op(self, eng, fn, reads=(), writes=(), dma=False, final=False, late=False):
        if Prog.limit is not None and self.nops >= Prog.limit:
            return None
        if eng == "gpsimd" and not dma:
            eng = "vector"
        if not late:
            rec = _Rec()
            fn(rec)
            fn = rec.call
        if self.defer_list is not None:
            self.defer_list.append((eng, fn, tuple(reads), tuple(writes), dma, final))
            return None
        return self._op2(eng, fn, reads, writes, dma, final)

    def merge(self, lists):
        lists = [list(l) for l in lists]
        pos = [0] * len(lists)
        while True:
            done = True
            for i, l in enumerate(lists):
                if pos[i] < len(l):
                    self._op2(*l[pos[i]])
                    pos[i] += 1
                    done = False
            if done:
                break

    def _op2(self, eng, fn, reads, writes, dma, final):
        pr = [b for b in reads if b.startswith("pb")]
        if pr:
            reads = [b for b in reads if not b.startswith("pb")]
            writes = list(writes) + pr
        deps = {}

        def need(tok):
            if tok is None:
                return
            s, v, e = tok
            if eng == "tensor" and e == "tensor" and not dma:
                return
            if deps.get(s, 0) < v:
                deps[s] = v

        for b in reads:
            need(self.last_w.get(b))
        for b in writes:
            need(self.last_w.get(b))
            for t in self.readers.get(b, ()):
                need(t)
        if dma:
            pool = self.dma_pool[eng]
            i = self.dma_rr[eng]
            self.dma_rr[eng] = (i + 1) % len(pool)
            ent = pool[i]
            if ent[1] > 0:
                need((ent[0], ent[1], "dma"))
            ent[1] += 16
            tok = (self._sem(ent[0]), ent[1], "dma")
            inc = (ent[0], 16)
            self.last_tok[ent[0]] = tok
        else:
            if self.cnt[eng] >= SEM_LIMIT:
                self.epoch[eng] += 1
                self.cnt[eng] = 0
            self.cnt[eng] += 1
            sname = self._sem(f"e_{eng}_{self.epoch[eng]}")
            tok = (sname, self.cnt[eng], eng)
            inc = (sname, 1)
            self.last_tok[sname] = tok
        kn = self.known[eng]
        waits = []
        for s, v in deps.items():
            if kn.get(s, 0) < v:
                waits.append((s, v))
                kn[s] = v
        self.ops[eng].append((waits, fn, inc))
        for b in writes:
            self.last_w[b] = tok
            self.readers[b] = []
        for b in reads:
            self.readers.setdefault(b, []).append(tok)
        if final:
            self.out_tokens.append(tok)
        self.nops += 1
        return tok

    def dma(self, out, in_, reads=(), writes=(), eng="sync", final=False, **kw):
        eng = "gpsimd" if "DRam" in type(out.tensor).__name__ else "sync"
        if out.dtype != in_.dtype:
            eng = "gpsimd"
        return self.op(eng, lambda e: e.dma_start(out=out, in_=in_, **kw), reads, writes, dma=True, final=final)

    def vload(self, eng, name, ap, lo, hi, reads):
        kn = self.known[eng]
        waits = []
        for b in reads:
            t = self.last_w.get(b)
            if t is not None and kn.get(t[0], 0) < t[1]:
                waits.append((t[0], t[1]))
                kn[t[0]] = t[1]
        self.ops[eng].append((waits, ("__vload__", name, ap, lo, hi), None))

    def barrier(self):
        toks = list(self.last_tok.values())
        for eng in self.ENGS:
            kn = self.known[eng]
            waits = []
            for s, v, _ in toks:
                if kn.get(s, 0) < v:
                    waits.append((s, v))
                    kn[s] = v
            if waits:
                self.ops[eng].append((waits, None, None))
        self.last_w = {}
        self.readers = {}

    def emit(self):
        nc = self.nc
        with contextlib.ExitStack() as st:
            for name in self.sem_order:
                self.sems[name] = st.enter_context(nc.semaphore(name))
            block = st.enter_context(nc.Block())
            sems = self.sems
            out_tokens = self.out_tokens

            def run(engname):
                def body(e):
                    env = {}
                    for waits, fn, inc in self.ops[engname]:
                        for s, v in waits:
                            e.wait_ge(sems[s], v)
                        if fn is None:
                            continue
                        if callable(fn):
                            fn(e, env).then_inc(sems[inc[0]], inc[1])
                        elif fn[0] == "__vload__":
                            env[fn[1]] = e.value_load(fn[2], min_val=fn[3], max_val=fn[4])
                        else:
                            getattr(e, fn[0])(*fn[1], **fn[2]).then_inc(sems[inc[0]], inc[1])
                    if engname == "sync":
                        for s, v, _ in out_tokens:
                            e.wait_ge(sems[s], v)
                return body

            block.tensor(run("tensor"))
            block.vector(run("vector"))
            block.scalar(run("scalar"))
            block.gpsimd(run("gpsimd"))
            block.sync(run("sync"))


class K:
    def __init__(self, S, C, debug=False):
        self.S, self.C, self.T = S, C, S + C
        self.debug = debug
        self.nc = bass.Bass("TRN2", target_bir_lowering=False)
        self.P = Prog(self.nc)
        self.inputs = {}
        self.scr = {}
        self.pb_rr = 0
        self.uid = 0
        self.debug_barrier = False

    def inp(self, name, shape, dt=F32):
        ap = self.nc.dram_tensor(name, list(shape), dt, kind="ExternalInput").ap()
        self.inputs[name] = ap
        return ap

    def scratch(self, name, shape, dt=F32):
        kind = "ExternalOutput" if self.debug else "Internal"
        ap = self.nc.dram_tensor(name, list(shape), dt, kind=kind).ap()
        self.scr[name] = ap
        return ap


def build(S, C, debug=False, stop_after=None, skip_l0=False):
    k = K(S, C, debug)
    nc, P = k.nc, k.P
    T = S + C
    NT_L, NT_C = S // 128, C // 128
    SQ = S // 2
    SH = SQ + 128

    x_d = k.inp("x", [S, D])
    ctx_d = k.inp("ctx", [C, D])
    cc_d = k.inp("cvec", [2, D])
    ident_d = k.inp("ident", [128, 128])
    w_mod_d = k.inp("w_mod", [2, D, 6 * D])
    b_mod_d = k.inp("b_mod", [2, 6 * D])
    ln_g_d = k.inp("ln_g", [2, 2, D])
    ln_b_d = k.inp("ln_b", [2, 2, D])
    e_w_in_d = k.inp("e_w_in", [D, 2432])
    e_b_in_d = k.inp("e_b_in", [2432])
    e_conv_w_d = k.inp("e_conv_w", [4, 512])
    e_conv_b_d = k.inp("e_conv_b", [512])
    e_wa_d = k.inp("e_lru_wa", [2, 8, 64, 64])
    e_ba_d = k.inp("e_lru_ba", [2, 512])
    e_wx_d = k.inp("e_lru_wx", [2, 8, 64, 64])
    e_bx_d = k.inp("e_lru_bx", [2, 512])
    e_lam_d = k.inp("e_lru_lambda", [2, 512])
    e_sink_d = k.inp("e_sink", [8])
    e_w_out_d = k.inp("e_w_out", [D, D])
    e_b_out_d = k.inp("e_b_out", [D])
    rope_e_d = k.inp("rope_e", [2, 128, S])
    mask_d = k.inp("swa_mask", [2, 128, 512])
    o_w_in_d = k.inp("o_w_in", [D, 1440])
    o_b_in_d = k.inp("o_b_in", [1440])
    o_q_norm_d = k.inp("o_q_norm", [256])
    o_kv_norm_d = k.inp("o_kv_norm", [128])
    o_w_uq_d = k.inp("o_w_uq", [256, 8, 192])
    o_w_uk_d = k.inp("o_w_uk", [128, 512])
    o_w_uv_d = k.inp("o_w_uv", [128, 512])
    o_w_kpe_rot_d = k.inp("o_w_kpe_rot", [D, 32])
    o_b_kpe_rot_d = k.inp("o_b_kpe_rot", [32])
    rope_q_d = k.inp("rope_q", [2, 96, SH])
    rope_k_d = k.inp("rope_k", [2, 32, S])
    xh_idx_d = k.inp("xh_idx", [SH], I32)
    zmask_d = k.inp("zmask", [SH])
    o_dw_w_d = k.inp("o_dw_w", [31, 512])
    o_dw_b_d = k.inp("o_dw_b", [512])
    o_cln_g_d = k.inp("o_cln_g", [512])
    o_cln_b_d = k.inp("o_cln_b", [512])
    o_w_out_d = k.inp("o_w_out", [D, D])
    o_b_out_d = k.inp("o_b_out", [D])
    moe_wg_d = k.inp("moe_w_gr", [2, D, 36])
    moe_bg_d = k.inp("moe_b_gr", [2, 36])
    moe_w1_d = k.inp("moe_w1", [2, 32, D, 512])
    moe_w3_d = k.inp("moe_w3", [2, 32, D, 512])
    moe_w2_d = k.inp("moe_w2", [2, 32, 512, D])
    out_d = nc.dram_tensor("out", [SQ, D], F32, kind="ExternalOutput").ap()

    m_scr = k.scratch("m_scr", [2, 2, 6 * D])
    XA = k.scratch("XA", [T, D])
    XBp = k.scratch("XB", [128 + T, D])
    XB = XBp[128:, :]
    XH = k.scratch("XH", [SH, D])
    FT = k.scratch("FT", [D, T], BF16)
    GATE = k.scratch("GATE", [T, 32])
    G_s = k.scratch("G_s", [512, T], BF16)
    U_s = k.scratch("U_s", [512, T])
    HF_s = k.scratch("HF_s", [512, T])
    MIXA = k.scratch("MIXA", [512, T], BF16)
    QT_s = k.scratch("QT_s", [8, 64, T], BF16)
    KT_s = k.scratch("KT_s", [2, 64, T], BF16)
    V_s = k.scratch("V_s", [T, 2, 65], BF16)
    ATT = k.scratch("ATT", [T, 512])
    QM = k.scratch("QM", [8, 96, SH], BF16)
    KM = k.scratch("KM", [8, 96, T], BF16)
    VM = k.scratch("VM", [T, 8, 65], BF16)
    ZC = k.scratch("ZC", [512, SH], BF16)
    CONV = k.scratch("CONV", [SQ, 512])

    st_all = contextlib.ExitStack()
    ps = st_all.enter_context(nc.psum_tensor("ps", [128, 4096], F32))
    SB_WORDS = 52000
    big = st_all.enter_context(nc.sbuf_tensor("big", [128, SB_WORDS], F32))
    k.sb_ptr = 0

    def pbank(n=1):
        if n == 2 and k.pb_rr % 2 == 1:
            k.pb_rr += 1
        i = k.pb_rr % 8
        k.pb_rr += n
        return ps[:, i * 512:(i + n) * 512], [f"pb{i + j}" for j in range(n)]

    class scope:
        def __enter__(self):
            self.mark = k.sb_ptr
            return self

        def __exit__(self, *a):
            k.sb_ptr = self.mark
            return False

    def sb(st, name, shape, dt=F32):
        k.uid += 1
        nm = f"{name}_{k.uid}"
        nfree = int(np.prod(shape[1:]))
        nwords = nfree if dt == F32 else (nfree + 1) // 2
        off = k.sb_ptr
        k.sb_ptr += nwords
        assert k.sb_ptr <= SB_WORDS, (name, k.sb_ptr)
        ap = big[:, off:off + nwords]
        if dt != F32:
            ap = ap.bitcast(dt)[:, 0:nfree]
        ap = ap[0:shape[0]]
        if len(shape) == 3:
            ap = ap.rearrange("p (a b) -> p a b", a=shape[1])
        elif len(shape) == 4:
            ap = ap.rearrange("p (a b c) -> p a b c", a=shape[1], b=shape[2])
        elif len(shape) == 5:
            ap = ap.rearrange("p (a b c d) -> p a b c d", a=shape[1], b=shape[2], c=shape[3])
        return ap, nm

    V, A, G, TE = "vector", "scalar", "gpsimd", "tensor"

    def ring(st, name, shape, dt=F32, n=2, init=None):
        tiles = [sb(st, name, shape, dt) for _ in range(n)]
        if init is not None:
            for t_, k_ in tiles:
                P.op(V, lambda e, t_=t_: e.memset(t_[:], init), [], [k_])
        cnt = [0]

        def nxt():
            cnt[0] += 1
            return tiles[(cnt[0] - 1) % n]
        return nxt

    ident, ident_k = sb(None, "ident", [128, 128])
    P.dma(ident[:], ident_d, writes=[ident_k])
    ones_r, ones_k = sb(None, "ones", [1, 128])
    P.op(V, lambda e: e.memset(ones_r[:], 1.0), [], [ones_k])
    mcol, mcol_k = sb(None, "mcol", [128, 2, 2, 4, 8])

    def col_dma(dst_ap, src_ap, keys_w, eng="gpsimd", reads=()):
        P.dma(dst_ap, src_ap, writes=keys_w, reads=reads, eng=eng, allow_slow_non_contiguous=True)

    with scope() as st:
        csT, csT_k = sb(st, "csT", [128, 8, 2])
        craw, craw_k = sb(st, "craw", [128, 8, 2])
        for s_ in range(2):
            col_dma(craw[:, :, s_], cc_d[s_].rearrange("(c p) -> p c", p=128), [craw_k])
        P.op(A, lambda e: e.activation(out=csT[:], in_=craw[:], func=AF.Silu), [craw_k], [csT_k])
        bm, bm_k = sb(st, "bm", [2, 6 * D])
        mrow, mrow_k = sb(st, "mrow", [2, 6 * D])
        wm = [sb(st, f"wm{i}", [128, 8, 512]) for i in range(2)]
        for l in range(2):
            P.dma(bm[:], b_mod_d[l].partition_broadcast(2), writes=[bm_k], reads=[])
            for n in range(12):
                wt, wk = wm[n % 2]
                P.dma(wt[:], w_mod_d[l, :, n * 512:(n + 1) * 512].rearrange("(kc p) n -> p kc n", p=128), writes=[wk],
                      eng="sync" if n % 2 == 0 else "gpsimd")
                pb, pk = pbank()
                for kc in range(8):
                    P.op(TE, lambda e, kc=kc, wt=wt, pb=pb: e.matmul(pb[0:2, :], lhsT=csT[:, kc, :], rhs=wt[:, kc, :],
                                                                   start=(kc == 0), stop=(kc == 7)),
                         [csT_k, wk], pk)
                P.op(V, lambda e, pb=pb, n=n: e.tensor_tensor(out=mrow[:, n * 512:(n + 1) * 512], in0=pb[0:2, :],
                                                               in1=bm[:, n * 512:(n + 1) * 512], op=ALU.add),
                     pk + [bm_k], [mrow_k])
            P.dma(m_scr[l], mrow[:], reads=[mrow_k], writes=["m_scr"], eng="gpsimd")
        zpad, zpadk = sb(st, "zpad", [64, D])
        P.op(V, lambda e: e.memset(zpad[:], 0.0), [], [zpadk])
        P.dma(XBp[64:128, :], zpad[:], reads=[zpadk], writes=["XBpad"], eng="gpsimd")
        for l in range(2):
            for s_ in range(2):
                for j, idx in enumerate((0, 1, 3, 4)):
                    col_dma(mcol[:, l, s_, j, :], m_scr[l, s_, idx * D:(idx + 1) * D].rearrange("(c p) -> p c", p=128),
                            [mcol_k], reads=["m_scr"])
        P.op(V, lambda e: e.tensor_scalar_add(out=mcol[:, :, :, 1, :], in0=mcol[:, :, :, 1, :], scalar1=1.0), [mcol_k], [mcol_k])
        P.op(V, lambda e: e.tensor_scalar_add(out=mcol[:, :, :, 3, :], in0=mcol[:, :, :, 3, :], scalar1=1.0), [mcol_k], [mcol_k])
    P.barrier()

    def load_bc(st, name, row_ap, n):
        t, tk = sb(st, name, [128, n])
        P.dma(t[:], row_ap.partition_broadcast(128), writes=[tk], eng="gpsimd")
        return t, tk

    def tok_src(l_idx, which):
        raise NotImplementedError

    def transpose_mod(xt, xk, dst, dk, col_sc, col_sh, ck, tcol, out_parity=0):
        pb, pk = pbank(2)
        for kc in range(8):
            P.op(TE, lambda e, kc=kc, pb=pb: e.transpose(pb[:, kc * 128:(kc + 1) * 128], xt[:, kc * 128:(kc + 1) * 128], ident[:]),
                 [xk, ident_k], pk)
        for kc in range(8):
            if kc % 2 == 0:
                P.op(V, lambda e, kc=kc, pb=pb: e.tensor_scalar(out=dst[:, kc, tcol:tcol + 128], in0=pb[:, kc * 128:(kc + 1) * 128],
                                                               scalar1=col_sc[:, kc:kc + 1], scalar2=col_sh[:, kc:kc + 1],
                                                               op0=ALU.mult, op1=ALU.add), pk + [ck], [dk])
            else:
                P.op(A, lambda e, kc=kc, pb=pb: e.activation(out=dst[:, kc, tcol:tcol + 128], in_=pb[:, kc * 128:(kc + 1) * 128],
                                                            func=AF.Identity, scale=col_sc[:, kc:kc + 1], bias=col_sh[:, kc:kc + 1]),
                     pk + [ck], [dk])

    def layer_norm_tile(st_tiles, z, zk, g_bc, gk, b_bc, bk, out, ok, width=D):
        stats, sk = st_tiles["stats"]
        mv, mvk = st_tiles["mv"]
        nchunk = width // 512
        for c_ in range(nchunk):
            P.op(V, lambda e, c_=c_: e.bn_stats(out=stats[:, c_, :], in_=z[:, c_ * 512:(c_ + 1) * 512]), [zk], [sk])
        P.op(V, lambda e: e.bn_aggr(out=mv[:, 0:2], in_=stats[:, 0:nchunk, :]), [sk], [mvk])
        P.op(V, lambda e: e.tensor_scalar_add(out=mv[:, 2:3], in0=mv[:, 1:2], scalar1=LN_EPS), [mvk], [mvk])
        P.op(A, lambda e: e.sqrt(out=mv[:, 3:4], in_=mv[:, 2:3]), [mvk], [mvk])
        P.op(V, lambda e: e.reciprocal(out=mv[:, 4:5], in_=mv[:, 3:4]), [mvk], [mvk])
        P.op(V, lambda e: e.scalar_tensor_tensor(out=mv[:, 5:6], in0=mv[:, 0:1], scalar=-1.0, in1=mv[:, 4:5],
                                                 op0=ALU.mult, op1=ALU.mult), [mvk], [mvk])
        P.op(A, lambda e: e.activation(out=out[:, 0:width], in_=z[:, 0:width], func=AF.Identity, scale=mv[:, 4:5], bias=mv[:, 5:6]),
             [zk, mvk], [ok])
        P.op(G, lambda e: e.tensor_tensor(out=out[:, 0:width], in0=out[:, 0:width], in1=g_bc[:, 0:width], op=ALU.mult), [ok, gk], [ok])
        P.op(V, lambda e: e.tensor_tensor(out=out[:, 0:width], in0=out[:, 0:width], in1=b_bc[:, 0:width], op=ALU.add), [ok, bk], [ok])

    def tok_tiles(n_lat_only=False, n_lat=None):
        r = [(i * 128, 0) for i in range(NT_L if n_lat is None else n_lat // 128)]
        if not n_lat_only:
            r += [(S + i * 128, 1) for i in range(NT_C)]
        return r

    def mixer_epilogue(l, w_out_d, b_out_d, x_src, mixT_loader, with_ctx, n_lat=None):
        with scope() as st:
            wo, wok = sb(st, "wo", [128, 8, D], BF16)
            P.dma(wo[:], w_out_d.rearrange("(kc p) n -> p kc n", p=128), writes=[wok], eng="gpsimd")
            wr, wrk = sb(st, "wr", [128, 8, 36])
            P.dma(wr[:], moe_wg_d[l].rearrange("(kc p) n -> p kc n", p=128), writes=[wrk])
            br, brk = load_bc(st, "br", moe_bg_d[l], 36)
            lng, lngk = load_bc(st, "lng", ln_g_d[l, 0], D)
            lnb, lnbk = load_bc(st, "lnb", ln_b_d[l, 0], D)
            streams = (0, 1) if with_ctx else (0,)
            gate_bc, gb_bc = {}, {}
            bo, bok = load_bc(st, "bo", b_out_d, D)
            for s_ in streams:
                gate_bc[s_] = load_bc(st, f"gate{s_}", m_scr[l, s_, 2 * D:3 * D], D)
                gb_bc[s_] = sb(st, f"gb{s_}", [128, D])
                P.op(V, lambda e, s_=s_: e.tensor_tensor(out=gb_bc[s_][0][:], in0=gate_bc[s_][0][:], in1=bo[:], op=ALU.mult),
                     [gate_bc[s_][1], bok], [gb_bc[s_][1]])
            NBUF = 2
            bufs = []
            for bi in range(NBUF):
                bufs.append(dict(
                    xt=sb(st, "xt", [128, D]), mixT=sb(st, "mixT", [128, 8, 128], BF16), tmp=sb(st, "tmp", [128, D]),
                    z=sb(st, "z", [128, D]), x1=sb(st, "x1", [128, D]), fT=sb(st, "fT", [128, 8, 128]),
                    fTb=sb(st, "fTb", [128, 8, 128], BF16),
                    small={"stats": sb(st, "stats", [128, 2, 6]), "mv": sb(st, "mv", [128, 8])},
                    lg=sb(st, "lg", [128, 36]), rt=sb(st, "rt", [128, 64]), gt=sb(st, "gt", [128, 32]), ld={}))
            tiles_ = tok_tiles(not with_ctx, n_lat)
            pending = []
            for ti_, (t0, s_) in enumerate(tiles_):
                P.defer_list = []
                pending.append(P.defer_list)
                B_ = bufs[ti_ % NBUF]
                xt, xk = B_["xt"]; mixT, mixk = B_["mixT"]; tmp, tmpk = B_["tmp"]; z, zk = B_["z"]; x1, x1k = B_["x1"]
                fT, fTk = B_["fT"]; fTb, fTbk = B_["fTb"]; small = B_["small"]; lg, lgk = B_["lg"]; rt, rtk = B_["rt"]; gt, gtk = B_["gt"]
                st.ld = B_["ld"]
                P.dma(xt[:], x_src(t0, s_), writes=[xk])
                mixT_loader(t0, s_, mixT, mixk, st)
                pb, pk = pbank(2)
                for n in range(2):
                    for kc in range(8):
                        P.op(TE, lambda e, kc=kc, n=n, pb=pb: e.matmul(pb[:, n * 512:(n + 1) * 512], lhsT=mixT[:, kc, :],
                                                                       rhs=wo[:, kc, n * 512:(n + 1) * 512], start=(kc == 0), stop=(kc == 7)),
                             [mixk, wok], pk)
                gbc, gbck = gate_bc[s_]
                P.op(V, lambda e, pb=pb, gbc=gbc: e.tensor_tensor(out=tmp[:], in0=pb, in1=gbc[:], op=ALU.mult), pk + [gbck], [tmpk])
                P.op(V, lambda e: e.scalar_tensor_tensor(out=z[:], in0=xt[:], scalar=DN_ALPHA, in1=tmp[:], op0=ALU.mult, op1=ALU.add),
                     [xk, tmpk], [zk])
                P.op(G, lambda e, s_=s_: e.tensor_tensor(out=z[:], in0=z[:], in1=gb_bc[s_][0][:], op=ALU.add), [zk, gb_bc[s_][1]], [zk])
                layer_norm_tile(small, z, zk, lng, lngk, lnb, lnbk, x1, x1k)
                P.dma(XA[t0:t0 + 128, :], x1[:], reads=[x1k], writes=["XA"], eng="gpsimd")
                transpose_mod(x1, x1k, fT, fTk, mcol[:, l, s_, 3, :], mcol[:, l, s_, 2, :], mcol_k, 0)
                P.op(G, lambda e: e.tensor_copy(out=fTb[:], in_=fT[:]), [fTk], [fTbk])
                P.dma(FT[:, t0:t0 + 128].rearrange("(kc p) t -> p kc t", p=128), fTb[:], reads=[fTbk], writes=["FT"], eng="gpsimd")
                pb2, pk2 = pbank()
                for kc in range(8):
                    P.op(TE, lambda e, kc=kc, pb2=pb2: e.matmul(pb2[:, 0:36], lhsT=fT[:, kc, :], rhs=wr[:, kc, :], start=(kc == 0), stop=(kc == 7)),
                         [fTk, wrk], pk2)
                P.op(V, lambda e, pb2=pb2: e.tensor_tensor(out=lg[:], in0=pb2[:, 0:36], in1=br[:], op=ALU.add), pk2 + [brk], [lgk])
                routing(lg, lgk, rt, rtk, gt, gtk)
                P.dma(GATE[t0:t0 + 128, :], gt[:], reads=[gtk], writes=["GATE"], eng="gpsimd")
                P.defer_list = None
                if len(pending) == MERGE_N or ti_ == len(tiles_) - 1:
                    P.merge(pending)
                    pending = []
        P.barrier()

    def routing(lg, lgk, rt, rtk, gt, gtk):
        ops = P.op
        ops(V, lambda e: e.reduce_max(out=rt[:, 0:1], in_=lg[:, 0:4], axis=AX.X), [lgk], [rtk])
        ops(V, lambda e: e.tensor_scalar_mul(out=rt[:, 1:2], in0=rt[:, 0:1], scalar1=-1.0), [rtk], [rtk])
        ops(A, lambda e: e.activation(out=rt[:, 56:60], in_=lg[:, 0:4], func=AF.Exp, bias=rt[:, 1:2], scale=1.0, accum_out=rt[:, 2:3]),
            [lgk, rtk], [rtk])
        ops(V, lambda e: e.reciprocal(out=rt[:, 3:4], in_=rt[:, 2:3]), [rtk], [rtk])
        ops(V, lambda e: e.tensor_scalar(out=rt[:, 4:8], in0=lg[:, 0:4], scalar1=rt[:, 0:1], scalar2=None, op0=ALU.is_ge), [lgk, rtk], [rtk])
        ops(V, lambda e: e.tensor_scalar_mul(out=rt[:, 8:16], in0=lg[:, 4:12], scalar1=rt[:, 4:5]), [lgk, rtk], [rtk])
        for g_ in range(1, 4):
            ops(V, lambda e, g_=g_: e.scalar_tensor_tensor(out=rt[:, 8:16], in0=lg[:, 4 + 8 * g_:12 + 8 * g_], scalar=rt[:, 4 + g_:5 + g_],
                                                           in1=rt[:, 8:16], op0=ALU.mult, op1=ALU.add), [lgk, rtk], [rtk])
        ops(V, lambda e: e.max(out=rt[:, 16:24], in_=rt[:, 8:16]), [rtk], [rtk])
        ops(V, lambda e: e.tensor_tensor(out=rt[:, 24:25], in0=rt[:, 17:18], in1=rt[:, 16:17], op=ALU.subtract), [rtk], [rtk])
        ops(A, lambda e: e.activation(out=rt[:, 24:25], in_=rt[:, 24:25], func=AF.Exp), [rtk], [rtk])
        ops(V, lambda e: e.tensor_scalar_add(out=rt[:, 25:26], in0=rt[:, 24:25], scalar1=1.0), [rtk], [rtk])
        ops(V, lambda e: e.reciprocal(out=rt[:, 26:27], in_=rt[:, 25:26]), [rtk], [rtk])
        ops(V, lambda e: e.tensor_tensor(out=rt[:, 27:28], in0=rt[:, 26:27], in1=rt[:, 3:4], op=ALU.mult), [rtk], [rtk])
        ops(V, lambda e: e.tensor_tensor(out=rt[:, 28:29], in0=rt[:, 27:28], in1=rt[:, 24:25], op=ALU.mult), [rtk], [rtk])
        ops(V, lambda e: e.tensor_scalar(out=rt[:, 32:40], in0=rt[:, 8:16], scalar1=rt[:, 16:17], scalar2=None, op0=ALU.is_ge), [rtk], [rtk])
        ops(V, lambda e: e.tensor_scalar(out=rt[:, 40:48], in0=rt[:, 8:16], scalar1=rt[:, 17:18], scalar2=None, op0=ALU.is_ge), [rtk], [rtk])
        ops(V, lambda e: e.tensor_tensor(out=rt[:, 40:48], in0=rt[:, 40:48], in1=rt[:, 32:40], op=ALU.subtract), [rtk], [rtk])
        ops(V, lambda e: e.tensor_scalar_mul(out=rt[:, 48:56], in0=rt[:, 32:40], scalar1=rt[:, 27:28]), [rtk], [rtk])
        ops(V, lambda e: e.scalar_tensor_tensor(out=rt[:, 48:56], in0=rt[:, 40:48], scalar=rt[:, 28:29], in1=rt[:, 48:56],
                                                op0=ALU.mult, op1=ALU.add), [rtk], [rtk])
        for g_ in range(4):
            ops(V, lambda e, g_=g_: e.tensor_scalar_mul(out=gt[:, 8 * g_:8 * g_ + 8], in0=rt[:, 48:56], scalar1=rt[:, 4 + g_:5 + g_]),
                [rtk], [gtk])

    def moe_phase(l, with_ctx, dst_fn, final, n_lat=None):
        Tn = T if with_ctx else (S if n_lat is None else n_lat)
        TG = 1024
        with scope() as st:
            lng, lngk = load_bc(st, "lng2", ln_g_d[l, 1], D)
            lnb, lnbk = load_bc(st, "lnb2", ln_b_d[l, 1], D)
            g5 = {0: load_bc(st, "g5l", m_scr[l, 0, 5 * D:6 * D], D)}
            if with_ctx:
                g5[1] = load_bc(st, "g5c", m_scr[l, 1, 5 * D:6 * D], D)
            ftg, ftgk = sb(st, "ftg", [128, 8, TG], BF16)
            gts, gtsk = sb(st, "gts", [128, 8, 32])
            acc, acck = sb(st, "acc", [128, 8, D])
            w1 = [sb(st, f"w1_{i}", [128, 8, 512], BF16) for i in range(2)]
            w3 = [sb(st, f"w3_{i}", [128, 8, 512], BF16) for i in range(2)]
            w2 = [sb(st, f"w2_{i}", [128, 4, D], BF16) for i in range(2)]
            hm, hmk = sb(st, "hm", [128, 4, TG], BF16)
            sil, silk = sb(st, "sil", [128, 512])
            xt, xk = sb(st, "xt2", [128, D])
            z, zk = sb(st, "z2", [128, D])
            xo, xok = sb(st, "xo", [128, D])
            small = {"stats": sb(st, "stats2", [128, 2, 6]), "mv": sb(st, "mv2", [128, 8])}
            for g0 in range(0, Tn, TG):
                ng = min(TG, Tn - g0)
                ntt = ng // 128
                P.dma(ftg[:, :, 0:ng], FT[:, g0:g0 + ng].rearrange("(kc p) t -> p kc t", p=128), reads=["FT"], writes=[ftgk])
                P.dma(gts[:, 0:ntt, :], GATE[g0:g0 + ng, :].rearrange("(a p) e -> p a e", p=128), reads=["GATE"], writes=[gtsk], eng="gpsimd")
                for ex in range(32):
                    w1t, w1k = w1[ex % 2]
                    w3t, w3k = w3[ex % 2]
                    w2t, w2k = w2[ex % 2]
                    P.dma(w1t[:], moe_w1_d[l, ex].rearrange("(kc p) n -> p kc n", p=128), writes=[w1k], eng="sync")
                    P.dma(w3t[:], moe_w3_d[l, ex].rearrange("(kc p) n -> p kc n", p=128), writes=[w3k], eng="sync")
                    P.dma(w2t[:], moe_w2_d[l, ex].rearrange("(kc p) n -> p kc n", p=128), writes=[w2k], eng="sync")
                    for n0 in range(0, ng, 512):
                        nn = min(512, ng - n0)
                        for oc in range(4):
                            p1, p1k = pbank()
                            p3, p3k = pbank()
                            for kc in range(8):
                                P.op(TE, lambda e, kc=kc, oc=oc, p1=p1, w1t=w1t, n0=n0, nn=nn: e.matmul(
                                    p1[:, 0:nn], lhsT=w1t[:, kc, oc * 128:(oc + 1) * 128], rhs=ftg[:, kc, n0:n0 + nn],
                                    start=(kc == 0), stop=(kc == 7)), [w1k, ftgk], p1k)
                            for kc in range(8):
                                P.op(TE, lambda e, kc=kc, oc=oc, p3=p3, w3t=w3t, n0=n0, nn=nn: e.matmul(
                                    p3[:, 0:nn], lhsT=w3t[:, kc, oc * 128:(oc + 1) * 128], rhs=ftg[:, kc, n0:n0 + nn],
                                    start=(kc == 0), stop=(kc == 7)), [w3k, ftgk], p3k)
                            P.op(A, lambda e, p1=p1, nn=nn: e.activation(out=sil[:, 0:nn], in_=p1[:, 0:nn], func=AF.Silu), p1k, [silk])
                            P.op(V, lambda e, p3=p3, nn=nn, oc=oc, n0=n0: e.tensor_tensor(out=hm[:, oc, n0:n0 + nn], in0=p3[:, 0:nn],
                                                                                         in1=sil[:, 0:nn], op=ALU.mult), p3k + [silk], [hmk])
                    for tt in range(ntt):
                        py, pyk = pbank(2)
                        for n in range(2):
                            for kc in range(4):
                                P.op(TE, lambda e, kc=kc, n=n, tt=tt, py=py, w2t=w2t: e.matmul(
                                    py[:, n * 512:(n + 1) * 512], lhsT=hm[:, kc, tt * 128:(tt + 1) * 128],
                                    rhs=w2t[:, kc, n * 512:(n + 1) * 512], start=(kc == 0), stop=(kc == 3)), [hmk, w2k], pyk)
                        eng_ = V if tt % 4 != 3 else G
                        if ex == 0:
                            P.op(V, lambda e, tt=tt, py=py, ex=ex: e.tensor_scalar_mul(out=acc[:, tt, :], in0=py, scalar1=gts[:, tt, ex:ex + 1]),
                                 pyk + [gtsk], [acck + str(tt)])
                        else:
                            P.op(V, lambda e, tt=tt, py=py, ex=ex: e.scalar_tensor_tensor(
                                out=acc[:, tt, :], in0=py, scalar=gts[:, tt, ex:ex + 1], in1=acc[:, tt, :], op0=ALU.mult, op1=ALU.add),
                                 pyk + [gtsk, acck + str(tt)], [acck + str(tt)])
                for tt in range(ntt):
                    t0 = g0 + tt * 128
                    s_ = 0 if t0 < S else 1
                    P.dma(xt[:], XA[t0:t0 + 128, :], reads=["XA"], writes=[xk])
                    P.op(G, lambda e, tt=tt, s_=s_: e.tensor_tensor(out=z[:], in0=acc[:, tt, :], in1=g5[s_][0][:], op=ALU.mult),
                         [acck + str(tt), g5[s_][1]], [zk])
                    P.op(V, lambda e: e.scalar_tensor_tensor(out=z[:], in0=xt[:], scalar=DN_ALPHA, in1=z[:], op0=ALU.mult, op1=ALU.add),
                         [xk, zk], [zk])
                    layer_norm_tile(small, z, zk, lng, lngk, lnb, lnbk, xo, xok)
                    dst = dst_fn(t0)
                    P.dma(dst, xo[:], reads=[xok], writes=["XB"], eng="gpsimd", final=final)
        P.barrier()

    def x0_src(t0, s_):
        return x_d[t0:t0 + 128, :] if s_ == 0 else ctx_d[t0 - S:t0 - S + 128, :]

    with scope() as st:
        win, wink = sb(st, "win", [128, 8, 2432], BF16)
        for kc in range(8):
            P.dma(win[:, kc, :], e_w_in_d[kc * 128:(kc + 1) * 128, :], writes=[wink], eng="sync" if kc % 2 else "gpsimd")
        bcol, bcolk = sb(st, "bcol", [128, 19])
        fm_cols = [(i * 128) for i in range(13)] + [1792 + i * 128 for i in range(5)]
        for j, c0 in enumerate(fm_cols):
            col_dma(bcol[:, j:j + 1], e_b_in_d[c0:c0 + 128].rearrange("(p o) -> p o", o=1), [bcolk])
        bv, bvk = load_bc(st, "bv", e_b_in_d[1664:1792], 128)
        xt_r = ring(st, "xt", [128, D])
        hT_r = ring(st, "hT", [128, 8, 512], BF16)
        gtmp_r = ring(st, "gtmp", [128, 512], BF16)
        utmp_r = ring(st, "utmp", [128, 512])
        cosT, cosk = sb(st, "cosT", [128, 512])
        sinT, sink = sb(st, "sinT", [128, 512])
        q1_r = ring(st, "q1", [128, 512])
        q2_r = ring(st, "q2", [128, 512])
        qo_r = ring(st, "qo", [128, 512], BF16, n=3)
        vt_r = ring(st, "vt", [128, 2, 65], BF16, init=1.0)
        groups = [(g0, 0, min(512, S - g0)) for g0 in range(0, S, 512)] + [(S + g0, 1, min(512, C - g0)) for g0 in range(0, C, 512)]
        for (g0, s_, ng) in groups:
            hT, hTk = hT_r()
            for tt in range(ng // 128):
                xt, xk = xt_r()
                P.dma(xt[:], x0_src(g0 + tt * 128, s_), writes=[xk], eng="sync")
                transpose_mod(xt, xk, hT, hTk, mcol[:, 0, s_, 1, :], mcol[:, 0, s_, 0, :], mcol_k, tt * 128)
            if k.debug_barrier:
                P.barrier()
            if s_ == 0:
                P.dma(cosT[:, 0:ng], rope_e_d[0, :, g0:g0 + ng], writes=[cosk], eng="gpsimd")
                P.dma(sinT[:, 0:ng], rope_e_d[1, :, g0:g0 + ng], writes=[sink], eng="gpsimd")

            def mm_chunk(c0, pb, pk):
                for kc in range(8):
                    P.op(TE, lambda e, kc=kc: e.matmul(pb[:, 0:ng], lhsT=win[:, kc, c0:c0 + 128], rhs=hT[:, kc, 0:ng],
                                                       start=(kc == 0), stop=(kc == 7)), [wink, hTk], pk)
            for j in range(4):
                pb, pk = pbank()
                mm_chunk(j * 128, pb, pk)
                gtmp, gtmpk = gtmp_r()
                P.op(A, lambda e, pb=pb, j=j: e.activation(out=gtmp[:, 0:ng], in_=pb[:, 0:ng], func=AF.Gelu_apprx_tanh,
                                                          bias=bcol[:, j:j + 1], scale=1.0), pk + [bcolk], [gtmpk])
                P.dma(G_s[j * 128:(j + 1) * 128, g0:g0 + ng], gtmp[:, 0:ng], reads=[gtmpk], writes=["G_s"], eng="gpsimd")
            for j in range(4):
                pb, pk = pbank()
                mm_chunk(512 + j * 128, pb, pk)
                utmp, utmpk = utmp_r()
                P.op(A, lambda e, pb=pb, j=j: e.activation(out=utmp[:, 0:ng], in_=pb[:, 0:ng], func=AF.Identity,
                                                          bias=bcol[:, 4 + j:5 + j], scale=1.0), pk + [bcolk], [utmpk])
                P.dma(U_s[j * 128:(j + 1) * 128, g0:g0 + ng], utmp[:, 0:ng], reads=[utmpk], writes=["U_s"], eng="gpsimd")
            for j in range(5):
                pb, pk = pbank()
                mm_chunk(1024 + j * 128, pb, pk)
                q1, q1k = q1_r()
                q2, q2k = q2_r()
                qo, qok = qo_r()
                if s_ == 0:
                    pr, prk = pbank()
                    mm_chunk(1792 + j * 128, pr, prk)
                    P.op(V, lambda e, pb=pb, j=j: e.scalar_tensor_tensor(out=q1[:, 0:ng], in0=pb[:, 0:ng], scalar=bcol[:, 8 + j:9 + j],
                                                                         in1=cosT[:, 0:ng], op0=ALU.add, op1=ALU.mult),
                         pk + [bcolk, cosk], [q1k])
                    P.op(V, lambda e, pr=pr, j=j: e.scalar_tensor_tensor(out=q2[:, 0:ng], in0=pr[:, 0:ng], scalar=bcol[:, 13 + j:14 + j],
                                                                         in1=sinT[:, 0:ng], op0=ALU.add, op1=ALU.mult),
                         prk + [bcolk, sink], [q2k])
                    P.op(G, lambda e: e.tensor_tensor(out=qo[:, 0:ng], in0=q1[:, 0:ng], in1=q2[:, 0:ng], op=ALU.add), [q1k, q2k], [qok])
                else:
                    P.op(A, lambda e, pb=pb, j=j: e.activation(out=qo[:, 0:ng], in_=pb[:, 0:ng], func=AF.Identity,
                                                              bias=bcol[:, 8 + j:9 + j], scale=1.0), pk + [bcolk], [qok])
                if j < 4:
                    dst = QT_s[2 * j:2 * j + 2, :, g0:g0 + ng].rearrange("h d t -> (h d) t")
                else:
                    dst = KT_s[:, :, g0:g0 + ng].rearrange("h d t -> (h d) t")
                P.dma(dst, qo[:, 0:ng], reads=[qok], writes=["QKT"], eng="gpsimd")
            for tt in range(ng // 128):
                vt, vtk = vt_r()
                pb, pk = pbank()
                for kc in range(8):
                    P.op(TE, lambda e, kc=kc, tt=tt, pb=pb: e.matmul(pb[:, 0:128], lhsT=hT[:, kc, tt * 128:(tt + 1) * 128],
                                                                     rhs=win[:, kc, 1664:1792], start=(kc == 0), stop=(kc == 7)),
                         [wink, hTk], pk)
                P.op(V, lambda e, pb=pb: e.tensor_tensor(out=vt[:, :, 0:64], in0=pb[:, 0:128].rearrange("p (h d) -> p h d", h=2),
                                                         in1=bv[:].rearrange("p (h d) -> p h d", h=2), op=ALU.add), pk + [bvk], [vtk])
                t0 = g0 + tt * 128
                P.dma(V_s[t0:t0 + 128], vt[:], reads=[vtk], writes=["V_s"], eng="gpsimd")
    P.barrier()
    if stop_after == "P1":
        return finish(k, st_all)

    SEG = 1024 if S % 1024 == 0 else 512
    with scope() as st:
        wbd32, wbd32k = sb(st, "wbd32", [128, 16, 128])
        wbd, wbdk = sb(st, "wbd", [128, 16, 128], BF16)
        P.op(V, lambda e: e.memset(wbd32[:], 0.0), [], [wbd32k])
        for d_ in range(2):
            for cc in range(4):
                for ai, wd in enumerate((e_wa_d, e_wx_d)):
                    ix = (d_ * 4 + cc) * 2 + ai
                    for hb in range(2):
                        P.dma(wbd32[hb * 64:(hb + 1) * 64, ix, hb * 64:(hb + 1) * 64], wd[d_, 2 * cc + hb], writes=[wbd32k], eng="gpsimd")
        P.op(V, lambda e: e.tensor_copy(out=wbd[:], in_=wbd32[:]), [wbd32k], [wbdk])
        cols, colsk = sb(st, "cols", [128, 64])
        col_dma(cols[:, 0:8], e_lam_d.rearrange("d (c p) -> p (d c)", p=128), [colsk])
        col_dma(cols[:, 8:16], e_ba_d.rearrange("d (c p) -> p (d c)", p=128), [colsk])
        col_dma(cols[:, 16:24], e_bx_d.rearrange("d (c p) -> p (d c)", p=128), [colsk])
        col_dma(cols[:, 24:28], e_conv_b_d.rearrange("(c p) -> p c", p=128), [colsk])
        for cc in range(4):
            col_dma(cols[:, 28 + 4 * cc:32 + 4 * cc], e_conv_w_d[:, cc * 128:(cc + 1) * 128].rearrange("k p -> p k"), [colsk])
        P.op(A, lambda e: e.activation(out=cols[:, 44:52], in_=cols[:, 0:8], func=AF.Exp, scale=-1.0), [colsk], [colsk])
        P.op(A, lambda e: e.activation(out=cols[:, 44:52], in_=cols[:, 44:52], func=AF.Ln, bias=1.0, scale=1.0), [colsk], [colsk])
        P.op(V, lambda e: e.tensor_scalar_mul(out=cols[:, 52:60], in0=cols[:, 44:52], scalar1=-16.0), [colsk], [colsk])
        P.op(V, lambda e: e.tensor_scalar_mul(out=cols[:, 44:52], in0=cols[:, 44:52], scalar1=-8.0), [colsk], [colsk])
        rings_ = {nm: ring(st, nm, [128, SEG + (3 if nm == "uh" else 0)], BF16 if nm in ("ucb", "gg", "mx") else F32)
                  for nm in ("uh", "uc", "ucb", "r", "i", "a", "b", "h", "hf", "gg", "mx")}
        state, statek = sb(st, "state", [128, 1])
        segs_l = [(s0, min(SEG, S - s0), 0) for s0 in range(0, S, SEG)]
        seg_c = (S, C, 1)
        for cc in range(4):
            rows = slice(cc * 128, (cc + 1) * 128)
            for d_ in range(2):
                order = [seg_c] + (segs_l if d_ == 0 else segs_l[::-1])
                P.op(V, lambda e: e.memset(state[:], 0.0), [], [statek])
                for (s0, n, s_) in order:
                    uh, uhk = rings_["uh"](); uc, uck = rings_["uc"](); ucb, ucbk = rings_["ucb"](); r_, rk = rings_["r"]()
                    i_, ik = rings_["i"](); a_, ak = rings_["a"](); b_, bk = rings_["b"](); h_, hk = rings_["h"]()
                    hf, hfk = rings_["hf"](); gg, ggk = rings_["gg"](); mx, mxk = rings_["mx"]()
                    lo = S if s_ == 1 else 0
                    hi = T if s_ == 1 else S
                    a0, a1 = max(lo, s0 - 1), min(hi, s0 + n + 2)
                    P.op(V, lambda e: e.memset(uh[:], 0.0), [], [uhk])
                    P.dma(uh[:, a0 - (s0 - 1):a1 - (s0 - 1)], U_s[rows, a0:a1], reads=["U_s"], writes=[uhk])
                    cw = 28 + 4 * cc
                    P.op(V, lambda e, n=n, cw=cw, cc=cc: e.tensor_scalar(out=uc[:, 0:n], in0=uh[:, 0:n], scalar1=cols[:, cw:cw + 1],
                                                                        scalar2=cols[:, 24 + cc:25 + cc], op0=ALU.mult, op1=ALU.add),
                         [uhk, colsk], [uck])
                    for kk in range(1, 4):
                        P.op(V, lambda e, n=n, cw=cw, kk=kk: e.scalar_tensor_tensor(out=uc[:, 0:n], in0=uh[:, kk:kk + n],
                                                                                    scalar=cols[:, cw + kk:cw + kk + 1], in1=uc[:, 0:n],
                                                                                    op0=ALU.mult, op1=ALU.add), [uhk, colsk, uck], [uck])
                    P.op(A, lambda e, n=n: e.copy(out=ucb[:, 0:n], in_=uc[:, 0:n]), [uck], [ucbk])
                    dc = d_ * 4 + cc
                    for n0 in range(0, n, 512):
                        nn = min(512, n - n0)
                        pa, pak = pbank()
                        px, pxk = pbank()
                        P.op(TE, lambda e, pa=pa, n0=n0, nn=nn, dc=dc: e.matmul(pa[:, 0:nn], lhsT=wbd[:, dc * 2, :], rhs=ucb[:, n0:n0 + nn],
                                                                                start=True, stop=True), [wbdk, ucbk], pak)
                        P.op(TE, lambda e, px=px, n0=n0, nn=nn, dc=dc: e.matmul(px[:, 0:nn], lhsT=wbd[:, dc * 2 + 1, :], rhs=ucb[:, n0:n0 + nn],
                                                                                start=True, stop=True), [wbdk, ucbk], pxk)
                        P.op(A, lambda e, pa=pa, n0=n0, nn=nn, dc=dc: e.activation(out=r_[:, n0:n0 + nn], in_=pa[:, 0:nn], func=AF.Sigmoid,
                                                                                   bias=cols[:, 8 + dc:9 + dc], scale=1.0), pak + [colsk], [rk])
                        P.op(A, lambda e, px=px, n0=n0, nn=nn, dc=dc: e.activation(out=i_[:, n0:n0 + nn], in_=px[:, 0:nn], func=AF.Sigmoid,
                                                                                   bias=cols[:, 16 + dc:17 + dc], scale=1.0), pxk + [colsk], [ik])
                    P.op(A, lambda e, n=n, dc=dc: e.activation(out=a_[:, 0:n], in_=r_[:, 0:n], func=AF.Exp, scale=cols[:, 44 + dc:45 + dc]),
                         [rk, colsk], [ak])
                    P.op(A, lambda e, n=n, dc=dc: e.activation(out=b_[:, 0:n], in_=r_[:, 0:n], func=AF.Exp, scale=cols[:, 52 + dc:53 + dc]),
                         [rk, colsk], [bk])
                    P.op(V, lambda e, n=n: e.tensor_scalar(out=b_[:, 0:n], in0=b_[:, 0:n], scalar1=-1.0, scalar2=1.0, op0=ALU.mult, op1=ALU.add),
                         [bk], [bk])
                    P.op(A, lambda e, n=n: e.sqrt(out=b_[:, 0:n], in_=b_[:, 0:n]), [bk], [bk])
                    P.op(V, lambda e, n=n: e.tensor_tensor(out=b_[:, 0:n], in0=b_[:, 0:n], in1=i_[:, 0:n], op=ALU.mult), [bk, ik], [bk])
                    P.op(G, lambda e, n=n: e.tensor_tensor(out=b_[:, 0:n], in0=b_[:, 0:n], in1=uc[:, 0:n], op=ALU.mult), [bk, uck], [bk])
                    if d_ == 0:
                        P.op(V, lambda e, n=n: e.tensor_tensor_scan(out=h_[:, 0:n], data0=a_[:, 0:n], data1=b_[:, 0:n], initial=state[:, 0:1],
                                                                    op0=ALU.mult, op1=ALU.add), [ak, bk, statek], [hk])
                        P.op(V, lambda e, n=n: e.tensor_copy(out=state[:], in_=h_[:, n - 1:n]), [hk], [statek])
                        P.dma(HF_s[rows, s0:s0 + n], h_[:, 0:n], reads=[hk], writes=["HF_s"], eng="gpsimd")
                    else:
                        P.op(V, lambda e, n=n: e.tensor_tensor_scan(out=h_[:, 0:n][:, ::-1], data0=a_[:, 0:n][:, ::-1], data1=b_[:, 0:n][:, ::-1],
                                                                    initial=state[:, 0:1], op0=ALU.mult, op1=ALU.add), [ak, bk, statek], [hk])
                        P.op(V, lambda e: e.tensor_copy(out=state[:], in_=h_[:, 0:1]), [hk], [statek])
                        P.dma(hf[:, 0:n], HF_s[rows, s0:s0 + n], reads=["HF_s"], writes=[hfk])
                        P.dma(gg[:, 0:n], G_s[rows, s0:s0 + n], reads=["G_s"], writes=[ggk])
                        P.op(G, lambda e, n=n: e.tensor_tensor(out=hf[:, 0:n], in0=hf[:, 0:n], in1=h_[:, 0:n], op=ALU.add), [hfk, hk], [hfk])
                        P.op(V, lambda e, n=n: e.tensor_tensor(out=mx[:, 0:n], in0=hf[:, 0:n], in1=gg[:, 0:n], op=ALU.mult), [hfk, ggk], [mxk])
                        P.dma(MIXA[rows, s0:s0 + n], mx[:, 0:n], reads=[mxk], writes=["MIXA"], eng="gpsimd")
    P.barrier()
    if stop_after == "P2":
        return finish(k, st_all)

    def attn_block(qT_ap, qk, keyts, nsub, dv, scale, pT, pTk, on_out):
        nk = len(keyts)
        nq = nsub * 128
        i = 0
        while i < nk:
            if PAIR_EXP and nq == 512 and i + 1 < nk:
                pb, pk = pbank(2)
                for j_ in range(2):
                    P.op(TE, lambda e, j_=j_: e.matmul(pb[:, j_ * 512:(j_ + 1) * 512], lhsT=keyts[i + j_][0], rhs=qT_ap, start=True, stop=True),
                         keyts[i + j_][1] + qk, pk)
                P.op(A, lambda e: e.activation(out=pT[:, i:i + 2, :], in_=pb[:, 0:1024].rearrange("p (a n) -> p a n", a=2), func=AF.Exp, scale=scale),
                     pk, [pTk + str(i), pTk + str(i + 1)])
                i += 2
            else:
                pb, pk = pbank()
                P.op(TE, lambda e: e.matmul(pb[:, 0:nq], lhsT=keyts[i][0], rhs=qT_ap, start=True, stop=True), keyts[i][1] + qk, pk)
                P.op(A, lambda e: e.activation(out=pT[:, i, 0:nq], in_=pb[:, 0:nq], func=AF.Exp, scale=scale), pk, [pTk + str(i)])
                i += 1
        for i, (kT_ap, kkeys, v_ap, vkeys, mask) in enumerate(keyts):
            if mask is not None:
                P.op(G, lambda e, i=i, mask=mask: e.tensor_tensor(out=pT[:, i, 0:nq], in0=pT[:, i, 0:nq], in1=mask[0][:, 0:nq], op=ALU.mult),
                     [pTk + str(i), mask[1]], [pTk + str(i)])
        for sub in range(nsub):
            po, pok = pbank()
            for i, (kT_ap, kkeys, v_ap, vkeys, mask) in enumerate(keyts):
                P.op(TE, lambda e, po=po, i=i, sub=sub, v_ap=v_ap: e.matmul(po[:, 0:dv + 1], lhsT=pT[:, i, sub * 128:(sub + 1) * 128], rhs=v_ap,
                                                                           start=(i == 0), stop=(i == nk - 1)), [pTk + str(i)] + vkeys, pok)
            on_out(sub, po[:, 0:dv + 1], pok)

    with scope() as st:
        es, esk = load_bc(st, "es", e_sink_d, 8)
        P.op(A, lambda e: e.activation(out=es[:], in_=es[:], func=AF.Exp), [esk], [esk])
        mprev, mprevk = sb(st, "mprev", [128, 512], BF16)
        mnext, mnextk = sb(st, "mnext", [128, 512], BF16)
        P.dma(mprev[:], mask_d[0], writes=[mprevk], eng="gpsimd")
        P.dma(mnext[:], mask_d[1], writes=[mnextk], eng="gpsimd")
        kt_sb, ktk = sb(st, "kt_sb", [64, T], BF16)
        v_sb, vk_ = sb(st, "v_sb", [128, T // 128, 65], BF16)
        qt_sb, qtk = sb(st, "qt_sb", [64, 4, 128], BF16)
        pT, pTk = sb(st, "pT", [128, 5, 512], BF16)
        den, denk = sb(st, "den", [128, 8])
        att, attk = sb(st, "att", [128, 256])
        for j in range(2):
            P.dma(kt_sb[:], KT_s[j], reads=["QKT"], writes=[ktk])
            P.dma(v_sb[:], V_s[:, j, :].rearrange("(a p) d -> p a d", p=128), reads=["V_s"], writes=[vk_], eng="gpsimd")
            for (t0, s_) in tok_tiles():
                P.dma(qt_sb[:], QT_s[4 * j:4 * j + 4, :, t0:t0 + 128].rearrange("h d t -> d h t"), reads=["QKT"], writes=[qtk])
                keyts = []

                def kt(tile_idx, mask):
                    keyts.append((kt_sb[:, tile_idx * 128:(tile_idx + 1) * 128], [ktk], v_sb[:, tile_idx, :], [vk_], mask))
                if s_ == 0:
                    n = t0 // 128
                    if n > 0:
                        kt(n - 1, (mprev, mprevk))
                    kt(n, None)
                    if n < NT_L - 1:
                        kt(n + 1, (mnext, mnextk))
                for c_ in range(NT_C):
                    kt(NT_L + c_, None)

                def on_out(sub, po, pok):
                    hh = 4 * j + sub
                    P.op(V, lambda e: e.tensor_tensor(out=den[:, 0:1], in0=po[:, 64:65], in1=es[:, hh:hh + 1], op=ALU.add), pok + [esk], [denk])
                    P.op(V, lambda e: e.reciprocal(out=den[:, 1:2], in_=den[:, 0:1]), [denk], [denk])
                    P.op(V, lambda e: e.tensor_scalar_mul(out=att[:, sub * 64:(sub + 1) * 64], in0=po[:, 0:64], scalar1=den[:, 1:2]),
                         pok + [denk], [attk])
                attn_block(qt_sb[:].rearrange("d h t -> d (h t)"), [qtk], keyts, 4, 64, 0.125, pT, pTk, on_out)
                P.dma(ATT[t0:t0 + 128, j * 256:(j + 1) * 256], att[:], reads=[attk], writes=["ATT"], eng="gpsimd")
    P.barrier()
    if stop_after == "P3":
        return finish(k, st_all)

    def mix_loader_even(t0, s_, mixT, mixk, st):
        if "att" not in st.ld:
            st.ld["att"] = sb(st, "attin", [128, 512])
        att_in, attink = st.ld["att"]
        P.dma(mixT[:, 0:4, :], MIXA[:, t0:t0 + 128].rearrange("(c p) t -> p c t", p=128), reads=["MIXA"], writes=[mixk])
        P.dma(att_in[:], ATT[t0:t0 + 128, :], reads=["ATT"], writes=[attink], eng="gpsimd")
        pb, pk = pbank()
        for c_ in range(4):
            P.op(TE, lambda e, c_=c_, pb=pb: e.transpose(pb[:, c_ * 128:(c_ + 1) * 128], att_in[:, c_ * 128:(c_ + 1) * 128], ident[:]),
                 [attink, ident_k], pk)
        P.op(V, lambda e, pb=pb: e.tensor_copy(out=mixT[:, 4:8, :], in_=pb[:, 0:512].rearrange("p (c t) -> p c t", c=4)), pk, [mixk])

    mixer_epilogue(0, e_w_out_d, e_b_out_d, x0_src, mix_loader_even, True)
    if stop_after == "P4":
        return finish(k, st_all)
    moe_phase(0, True, lambda t0: XB[t0:t0 + 128, :], False)
    if stop_after == "P5":
        return finish(k, st_all)

    with scope() as st:
        NA = SH // 128
        xi_f, xik = sb(st, "xi", [128, NA])
        xi = xi_f.bitcast(I32)
        col_dma(xi, xh_idx_d.rearrange("(a p) -> p a", p=128), [xik])
        xg = [sb(st, f"xg{i}", [128, D]) for i in range(2)]
        for a in range(NA):
            gt_, gk_ = xg[a % 2]
            P.op("gpsimd", lambda e, a=a, gt_=gt_: e.indirect_dma_start(
                out=gt_[:, :], out_offset=None, in_=XBp[:, :],
                in_offset=bass.IndirectOffsetOnAxis(ap=xi[:, a:a + 1], axis=0),
                bounds_check=128 + T - 1, oob_is_err=False), [xik, "XB", "XBpad"], [gk_], dma=True)
            P.dma(XH[a * 128:(a + 1) * 128, :], gt_[:], reads=[gk_], writes=["XH"], eng="sync")
    P.barrier()

    def x1_src(t0, s_):
        return XH[64 + t0:64 + t0 + 128, :]

    MSCALE = 96.0 ** -0.5
    with scope() as st:
        owin, owink = sb(st, "owin", [128, 8, 1440], BF16)
        for kc in range(8):
            P.dma(owin[:, kc, :], o_w_in_d[kc * 128:(kc + 1) * 128, :], writes=[owink])
        wkr, wkrk = sb(st, "wkr", [128, 8, 32], BF16)
        P.dma(wkr[:], o_w_kpe_rot_d.rearrange("(kc p) n -> p kc n", p=128), writes=[wkrk])
        wuq, wuqk = sb(st, "wuq", [128, 2, 8, 192], BF16)
        P.dma(wuq[:], o_w_uq_d.rearrange("(kc p) h n -> p kc h n", p=128), writes=[wuqk])
        wuk, wukk = sb(st, "wuk", [128, 512], BF16)
        P.dma(wuk[:], o_w_uk_d, writes=[wukk])
        wuv, wuvk = sb(st, "wuv", [128, 512], BF16)
        P.dma(wuv[:], o_w_uv_d, writes=[wuvk])
        bq, bqk = load_bc(st, "bq", o_b_in_d[0:384], 384)
        qn, qnk = load_bc(st, "qn", o_q_norm_d, 256)
        kvn, kvnk = load_bc(st, "kvn", o_kv_norm_d, 128)
        zmk, zmkk = load_bc(st, "zmk", zmask_d, SH)
        oc, ock = sb(st, "oc", [128, 12])
        col_dma(oc[0:32, 0:1], o_b_in_d[384:416].rearrange("(p o) -> p o", o=1), [ock])
        col_dma(oc[0:32, 1:2], o_b_kpe_rot_d.rearrange("(p o) -> p o", o=1), [ock])
        col_dma(oc[:, 2:10], o_b_in_d[416:1440].rearrange("(c p) -> p c", p=128), [ock])
        xt_r = ring(st, "xt", [128, D])
        hT_r = ring(st, "hT", [128, 8, 512], BF16)
        tq_r = ring(st, "tq", [128, 384])
        sq_r = ring(st, "sq", [128, 384])
        rs_r = ring(st, "rs", [128, 8])
        cqnT_r = ring(st, "cqnT", [128, 2, 512], BF16)
        ckvT_r = ring(st, "ckvT", [128, 512], BF16)
        cos96, cos96k = sb(st, "cos96", [96, 512])
        sin96, sin96k = sb(st, "sin96", [96, 512])
        cos32, cos32k = sb(st, "cos32", [32, 512])
        sin32, sin32k = sb(st, "sin32", [32, 512])
        f1_r = ring(st, "f1", [128, 512])
        f2_r = ring(st, "f2", [128, 512])
        ob_r = ring(st, "ob", [128, 512], BF16, n=3)
        vt_r = ring(st, "vt1", [128, 8, 65], BF16, init=1.0)

        def norm_part(c0, c1, rcol, ncol, nbc, nbck):
            w = c1 - c0
            P.op(A, lambda e: e.activation(out=sq[:, c0:c1], in_=tq[:, c0:c1], func=AF.Square, accum_out=rs[:, rcol:rcol + 1]), [tqk], [sqk, rsk])
            P.op(V, lambda e: e.tensor_scalar(out=rs[:, rcol + 2:rcol + 3], in0=rs[:, rcol:rcol + 1], scalar1=1.0 / w, scalar2=LN_EPS,
                                              op0=ALU.mult, op1=ALU.add), [rsk], [rsk])
            P.op(A, lambda e: e.sqrt(out=rs[:, rcol + 4:rcol + 5], in_=rs[:, rcol + 2:rcol + 3]), [rsk], [rsk])
            P.op(V, lambda e: e.reciprocal(out=rs[:, rcol + 6:rcol + 7], in_=rs[:, rcol + 4:rcol + 5]), [rsk], [rsk])
            P.op(V, lambda e: e.scalar_tensor_tensor(out=sq[:, c0:c1], in0=tq[:, c0:c1], scalar=rs[:, rcol + 6:rcol + 7], in1=nbc[:],
                                                     op0=ALU.mult, op1=ALU.mult), [tqk, rsk, nbck], [sqk])

        groups = [(g0, 0, min(512, S - g0)) for g0 in range(0, S, 512)] + [(S + g0, 1, min(512, C - g0)) for g0 in range(0, C, 512)]
        for (g0, s_, ng) in groups:
            hT, hTk = hT_r()
            ckvT, ckvTk = ckvT_r()
            for tt in range(ng // 128):
                t0 = g0 + tt * 128
                xt, xk = xt_r()
                tq, tqk = tq_r()
                sq, sqk = sq_r()
                rs, rsk = rs_r()
                vt, vtk = vt_r()
                P.dma(xt[:], XB[t0:t0 + 128, :], reads=["XB"], writes=[xk])
                transpose_mod(xt, xk, hT, hTk, mcol[:, 1, s_, 1, :], mcol[:, 1, s_, 0, :], mcol_k, tt * 128)
                pb, pk = pbank()
                for kc in range(8):
                    P.op(TE, lambda e, kc=kc: e.matmul(pb[:, 0:128], lhsT=hT[:, kc, tt * 128:(tt + 1) * 128], rhs=owin[:, kc, 256:384],
                                                       start=(kc == 0), stop=(kc == 7)), [hTk, owink], pk)
                P.op(V, lambda e: e.tensor_tensor(out=tq[:, 256:384], in0=pb[:, 0:128], in1=bq[:, 256:384], op=ALU.add), pk + [bqk], [tqk])
                norm_part(256, 384, 1, 128, kvn, kvnk)
                pt, ptk = pbank()
                P.op(TE, lambda e: e.transpose(pt[:, 0:128], sq[:, 256:384], ident[:]), [sqk, ident_k], ptk)
                P.op(A, lambda e: e.copy(out=ckvT[:, tt * 128:(tt + 1) * 128], in_=pt[:, 0:128]), ptk, [ckvTk])
                pv, pvk = pbank()
                P.op(TE, lambda e: e.matmul(pv[:, 0:512], lhsT=ckvT[:, tt * 128:(tt + 1) * 128], rhs=wuv[:], start=True, stop=True), [ckvTk, wuvk], pvk)
                P.op(V, lambda e: e.tensor_copy(out=vt[:, :, 0:64], in_=pv[:, 0:512].rearrange("p (h d) -> p h d", h=8)), pvk, [vtk])
                P.dma(VM[t0:t0 + 128], vt[:], reads=[vtk], writes=["VM"], eng="gpsimd")
            for c_ in range(4):
                pkn, pknk = pbank()
                ob, obk = ob_r()
                P.op(TE, lambda e, c_=c_: e.matmul(pkn[:, 0:ng], lhsT=wuk[:, c_ * 128:(c_ + 1) * 128], rhs=ckvT[:, 0:ng], start=True, stop=True),
                     [wukk, ckvTk], pknk)
                P.op(A, lambda e: e.copy(out=ob[:, 0:ng], in_=pkn[:, 0:ng]), pknk, [obk])
                for hh in range(2):
                    P.dma(KM[2 * c_ + hh, 0:64, g0:g0 + ng], ob[hh * 64:(hh + 1) * 64, 0:ng], reads=[obk], writes=["KM"], eng="gpsimd")
            pp, ppk = pbank()
            ob, obk = ob_r()
            f1, f1k = f1_r()
            f2, f2k = f2_r()
            for kc in range(8):
                P.op(TE, lambda e, kc=kc: e.matmul(pp[0:32, 0:ng], lhsT=owin[:, kc, 384:416], rhs=hT[:, kc, 0:ng], start=(kc == 0), stop=(kc == 7)),
                     [owink, hTk], ppk)
            if s_ == 0:
                P.dma(cos32[:, 0:ng], rope_k_d[0, :, g0:g0 + ng], writes=[cos32k])
                P.dma(sin32[:, 0:ng], rope_k_d[1, :, g0:g0 + ng], writes=[sin32k])
                pr, prk = pbank()
                for kc in range(8):
                    P.op(TE, lambda e, kc=kc: e.matmul(pr[0:32, 0:ng], lhsT=wkr[:, kc, :], rhs=hT[:, kc, 0:ng], start=(kc == 0), stop=(kc == 7)),
                         [wkrk, hTk], prk)
                P.op(V, lambda e: e.scalar_tensor_tensor(out=f1[0:32, 0:ng], in0=pp[0:32, 0:ng], scalar=oc[0:32, 0:1], in1=cos32[:, 0:ng],
                                                         op0=ALU.add, op1=ALU.mult), ppk + [ock, cos32k], [f1k])
                P.op(V, lambda e: e.scalar_tensor_tensor(out=f2[0:32, 0:ng], in0=pr[0:32, 0:ng], scalar=oc[0:32, 1:2], in1=sin32[:, 0:ng],
                                                         op0=ALU.add, op1=ALU.mult), prk + [ock, sin32k], [f2k])
                P.op(G, lambda e: e.tensor_tensor(out=ob[0:32, 0:ng], in0=f1[0:32, 0:ng], in1=f2[0:32, 0:ng], op=ALU.add), [f1k, f2k], [obk])
            else:
                P.op(A, lambda e: e.activation(out=ob[0:32, 0:ng], in_=pp[0:32, 0:ng], func=AF.Identity, bias=oc[0:32, 0:1], scale=1.0),
                     ppk + [ock], [obk])
            for h in range(8):
                P.dma(KM[h, 64:96, g0:g0 + ng], ob[0:32, 0:ng], reads=[obk], writes=["KM"], eng="gpsimd")

        for g0 in range(0, SH, 512):
            ng = min(512, SH - g0)
            hT, hTk = hT_r()
            cqnT, cqnTk = cqnT_r()
            for tt in range(ng // 128):
                r0 = g0 + tt * 128
                xt, xk = xt_r()
                tq, tqk = tq_r()
                sq, sqk = sq_r()
                rs, rsk = rs_r()
                P.dma(xt[:], XH[r0:r0 + 128, :], reads=["XH"], writes=[xk])
                transpose_mod(xt, xk, hT, hTk, mcol[:, 1, 0, 1, :], mcol[:, 1, 0, 0, :], mcol_k, tt * 128)
                pb, pk = pbank()
                for kc in range(8):
                    P.op(TE, lambda e, kc=kc: e.matmul(pb[:, 0:256], lhsT=hT[:, kc, tt * 128:(tt + 1) * 128], rhs=owin[:, kc, 0:256],
                                                       start=(kc == 0), stop=(kc == 7)), [hTk, owink], pk)
                P.op(V, lambda e: e.tensor_tensor(out=tq[:, 0:256], in0=pb[:, 0:256], in1=bq[:, 0:256], op=ALU.add), pk + [bqk], [tqk])
                norm_part(0, 256, 0, 256, qn, qnk)
                pt, ptk = pbank()
                for c_ in range(2):
                    P.op(TE, lambda e, c_=c_: e.transpose(pt[:, c_ * 128:(c_ + 1) * 128], sq[:, c_ * 128:(c_ + 1) * 128], ident[:]), [sqk, ident_k], ptk)
                P.op(V, lambda e: e.tensor_copy(out=cqnT[:, :, tt * 128:(tt + 1) * 128], in_=pt[:, 0:256].rearrange("p (c t) -> p c t", c=2)),
                     ptk, [cqnTk])
            P.dma(cos96[:, 0:ng], rope_q_d[0, :, g0:g0 + ng], writes=[cos96k])
            P.dma(sin96[:, 0:ng], rope_q_d[1, :, g0:g0 + ng], writes=[sin96k])
            for h in range(8):
                pq, pqk = pbank()
                pr, prk = pbank()
                f1, f1k = f1_r()
                f2, f2k = f2_r()
                ob, obk = ob_r()
                for kc in range(2):
                    P.op(TE, lambda e, kc=kc: e.matmul(pq[0:96, 0:ng], lhsT=wuq[:, kc, h, 0:96], rhs=cqnT[:, kc, 0:ng], start=(kc == 0), stop=(kc == 1)),
                         [wuqk, cqnTk], pqk)
                for kc in range(2):
                    P.op(TE, lambda e, kc=kc: e.matmul(pr[0:96, 0:ng], lhsT=wuq[:, kc, h, 96:192], rhs=cqnT[:, kc, 0:ng], start=(kc == 0), stop=(kc == 1)),
                         [wuqk, cqnTk], prk)
                P.op(V, lambda e: e.tensor_tensor(out=f1[0:96, 0:ng], in0=pq[0:96, 0:ng], in1=cos96[:, 0:ng], op=ALU.mult), pqk + [cos96k], [f1k])
                P.op(V, lambda e: e.tensor_tensor(out=f2[0:96, 0:ng], in0=pr[0:96, 0:ng], in1=sin96[:, 0:ng], op=ALU.mult), prk + [sin96k], [f2k])
                P.op(G, lambda e: e.tensor_tensor(out=ob[0:96, 0:ng], in0=f1[0:96, 0:ng], in1=f2[0:96, 0:ng], op=ALU.add), [f1k, f2k], [obk])
                P.dma(QM[h, :, g0:g0 + ng], ob[0:96, 0:ng], reads=[obk], writes=["QM"], eng="gpsimd")
            for j in range(4):
                pa, pak = pbank()
                pg, pgk = pbank()
                f1, f1k = f1_r()
                f2, f2k = f2_r()
                ob, obk = ob_r()
                for kc in range(8):
                    P.op(TE, lambda e, kc=kc: e.matmul(pa[:, 0:ng], lhsT=owin[:, kc, 416 + j * 128:544 + j * 128], rhs=hT[:, kc, 0:ng],
                                                       start=(kc == 0), stop=(kc == 7)), [owink, hTk], pak)
                for kc in range(8):
                    P.op(TE, lambda e, kc=kc: e.matmul(pg[:, 0:ng], lhsT=owin[:, kc, 928 + j * 128:1056 + j * 128], rhs=hT[:, kc, 0:ng],
                                                       start=(kc == 0), stop=(kc == 7)), [owink, hTk], pgk)
                P.op(A, lambda e: e.activation(out=f1[:, 0:ng], in_=pg[:, 0:ng], func=AF.Sigmoid, bias=oc[:, 6 + j:7 + j], scale=1.0),
                     pgk + [ock], [f1k])
                P.op(V, lambda e: e.scalar_tensor_tensor(out=f2[:, 0:ng], in0=pa[:, 0:ng], scalar=oc[:, 2 + j:3 + j], in1=f1[:, 0:ng],
                                                         op0=ALU.add, op1=ALU.mult), pak + [ock, f1k], [f2k])
                P.op(G, lambda e: e.tensor_tensor(out=ob[:, 0:ng], in0=f2[:, 0:ng], in1=zmk[:, g0:g0 + ng], op=ALU.mult), [f2k, zmkk], [obk])
                P.dma(ZC[j * 128:(j + 1) * 128, g0:g0 + ng], ob[:, 0:ng], reads=[obk], writes=["ZC"], eng="gpsimd")
    P.barrier()
    if stop_after == "Q1":
        return finish(k, st_all)

    with scope() as st:
        identb, identbk = sb(st, "identb", [128, 128], BF16)
        P.op(V, lambda e: e.tensor_copy(out=identb[:], in_=ident[:]), [ident_k], [identbk])
        dwc, dwck = sb(st, "dwc", [128, 4, 32])
        for j in range(4):
            col_dma(dwc[:, j, 0:31], o_dw_w_d[:, j * 128:(j + 1) * 128].rearrange("k p -> p k"), [dwck])
        col_dma(dwc[:, :, 31], o_dw_b_d.rearrange("(c p) -> p c", p=128), [dwck])
        dg, dgk = sb(st, "dg", [128, 4, 31, 128], BF16)
        for j in range(4):
            for kk in range(31):
                P.op(V if kk % 2 else G, lambda e: e.tensor_scalar_mul(out=dg[:, j, kk, :], in0=identb[:], scalar1=dwc[:, j, kk:kk + 1]),
                     [identbk, dwck], [dgk])
        cg, cgk = load_bc(st, "cg", o_cln_g_d, 512)
        cb, cbk = load_bc(st, "cb", o_cln_b_d, 512)
        zw, zwk = sb(st, "zw", [128, 512 + 30], BF16)
        yc, yck = sb(st, "yc", [128, 4, 512])
        yt, ytk = sb(st, "yt", [128, 512])
        yo, yok = sb(st, "yo", [128, 512])
        small = {"stats": sb(st, "stats3", [128, 2, 6]), "mv": sb(st, "mv3", [128, 8])}
        for g0 in range(0, SQ, 512):
            ng = min(512, SQ - g0)
            for j in range(4):
                P.dma(zw[:, 0:ng + 30], ZC[j * 128:(j + 1) * 128, 49 + g0:49 + g0 + ng + 30], reads=["ZC"], writes=[zwk])
                pc, pck = pbank()
                for kk in range(31):
                    P.op(TE, lambda e, kk=kk: e.matmul(pc[:, 0:ng], lhsT=dg[:, j, kk, :], rhs=zw[:, kk:kk + ng], start=(kk == 0), stop=(kk == 30)),
                         [dgk, zwk], pck)
                P.op(A, lambda e: e.activation(out=yc[:, j, 0:ng], in_=pc[:, 0:ng], func=AF.Identity, bias=dwc[:, j, 31:32], scale=1.0),
                     pck + [dwck], [yck])
            for tt in range(ng // 128):
                pt, ptk = pbank()
                for j in range(4):
                    P.op(TE, lambda e, j=j: e.transpose(pt[:, j * 128:(j + 1) * 128], yc[:, j, tt * 128:(tt + 1) * 128], ident[:]), [yck, ident_k], ptk)
                P.op(V, lambda e: e.tensor_copy(out=yt[:], in_=pt[:, 0:512]), ptk, [ytk])
                layer_norm_tile(small, yt, ytk, cg, cgk, cb, cbk, yo, yok, width=512)
                P.op(A, lambda e: e.activation(out=yo[:], in_=yo[:], func=AF.Silu), [yok], [yok])
                t0 = g0 + tt * 128
                P.dma(CONV[t0:t0 + 128, :], yo[:], reads=[yok], writes=["CONV"], eng="gpsimd")
    P.barrier()
    if stop_after == "Q2":
        return finish(k, st_all)

    with scope() as st:
        NKT = T // 128
        km, kmk = sb(st, "km", [96, T], BF16)
        vm, vmk = sb(st, "vm", [128, NKT, 65], BF16)
        qm, qmk = sb(st, "qm", [96, 512], BF16)
        pT, pTk = sb(st, "pTm", [128, NKT, 512], BF16)
        den, denk = sb(st, "den1", [128, 2])
        att, attk = sb(st, "att1", [128, 64])
        for h in range(8):
            P.dma(km[:], KM[h], reads=["KM"], writes=[kmk])
            P.dma(vm[:], VM[:, h, :].rearrange("(a p) d -> p a d", p=128), reads=["VM"], writes=[vmk], eng="gpsimd")
            for g0 in range(0, SQ, 512):
                ng = min(512, SQ - g0)
                P.dma(qm[:, 0:ng], QM[h, :, 64 + g0:64 + g0 + ng], reads=["QM"], writes=[qmk])
                keyts = [(km[:, i * 128:(i + 1) * 128], [kmk], vm[:, i, :], [vmk], None) for i in range(NKT)]

                def on_out(sub, po, pok):
                    P.op(V, lambda e: e.reciprocal(out=den[:, 0:1], in_=po[:, 64:65]), pok, [denk])
                    P.op(V, lambda e: e.tensor_scalar_mul(out=att[:], in0=po[:, 0:64], scalar1=den[:, 0:1]), pok + [denk], [attk])
                    t0 = g0 + sub * 128
                    P.dma(ATT[t0:t0 + 128, h * 64:(h + 1) * 64], att[:], reads=[attk], writes=["ATT"], eng="gpsimd")
                attn_block(qm[:, 0:ng], [qmk], keyts, ng // 128, 64, MSCALE, pT, pTk, on_out)
    P.barrier()
    if stop_after == "Q3":
        return finish(k, st_all)

    def mix_loader_odd(t0, s_, mixT, mixk, st):
        if "t" not in st.ld:
            st.ld["t"] = sb(st, "mixin", [128, D])
        mi, mik = st.ld["t"]
        P.dma(mi[:, 0:512], ATT[t0:t0 + 128, :], reads=["ATT"], writes=[mik])
        P.dma(mi[:, 512:1024], CONV[t0:t0 + 128, :], reads=["CONV"], writes=[mik], eng="gpsimd")
        pb, pk = pbank(2)
        for c_ in range(8):
            P.op(TE, lambda e, c_=c_: e.transpose(pb[:, c_ * 128:(c_ + 1) * 128], mi[:, c_ * 128:(c_ + 1) * 128], ident[:]), [mik, ident_k], pk)
        P.op(V, lambda e: e.tensor_copy(out=mixT[:, 0:4, :], in_=pb[:, 0:512].rearrange("p (c t) -> p c t", c=4)), pk, [mixk])
        P.op(A, lambda e: e.copy(out=mixT[:, 4:8, :], in_=pb[:, 512:1024].rearrange("p (c t) -> p c t", c=4)), pk, [mixk])

    mixer_epilogue(1, o_w_out_d, o_b_out_d, x1_src, mix_loader_odd, False, n_lat=SQ)
    if stop_after == "Q4":
        return finish(k, st_all)
    moe_phase(1, False, lambda t0: out_d[t0:t0 + 128, :], True, n_lat=SQ)
    return finish(k, st_all)


def finish(k, st_all):
    k.P.emit()
    st_all.close()
    return k


GRID_W = 64
ROPE_THETA = 10000.0


def _rot_perm(n_heads, hd):
    q = hd // 4
    idx = []
    for h in range(n_heads):
        b = h * hd
        idx += list(range(b + q, b + 2 * q)) + list(range(b, b + q)) + list(range(b + 3 * q, b + 4 * q)) + list(range(b + 2 * q, b + 3 * q))
    return np.array(idx)


def _rope_tables(S, hd, t=None):
    q = hd // 4
    if t is None:
        t = np.arange(S)
    S = len(t)
    row, col = t // GRID_W, t % GRID_W
    invf = ROPE_THETA ** (-np.arange(q, dtype=np.float64) / q)
    cos = np.zeros((hd, S)); sin = np.zeros((hd, S))
    for d in range(hd):
        pos = row if d < 2 * q else col
        dd = d % (2 * q)
        j = dd % q
        ang = pos.astype(np.float32).astype(np.float64) * np.float32(invf[j]).astype(np.float64)
        cos[d] = np.cos(ang)
        sin[d] = np.sin(ang) * (-1.0 if dd < q else 1.0)
    return cos.astype(np.float32), sin.astype(np.float32)


def prep_core_inputs(inp, b, S, h=0):
    f = lambda a: np.ascontiguousarray(np.asarray(a, dtype=np.float32))
    m = {}
    m["x"] = f(inp["x"][b])
    m["ctx"] = f(inp["ctx"][b])
    m["cvec"] = f(np.stack([inp["c"][b], inp["c_ctx"]]))
    m["ident"] = np.eye(128, dtype=np.float32)
    m["w_mod"] = f(inp["w_mod"]); m["b_mod"] = f(inp["b_mod"])
    m["ln_g"] = f(inp["ln_g"]); m["ln_b"] = f(inp["ln_b"])
    w_in = np.asarray(inp["e_w_in"][0]); b_in = np.asarray(inp["e_b_in"][0])
    perm = _rot_perm(10, 64) + 1024
    m["e_w_in"] = f(np.concatenate([w_in, w_in[:, perm]], axis=1))
    m["e_b_in"] = f(np.concatenate([b_in, b_in[perm]]))
    m["e_conv_w"] = f(inp["e_conv_w"][0]); m["e_conv_b"] = f(inp["e_conv_b"][0])
    m["e_lru_wa"] = f(inp["e_lru_wa"][0]); m["e_lru_ba"] = f(inp["e_lru_ba"][0])
    m["e_lru_wx"] = f(inp["e_lru_wx"][0]); m["e_lru_bx"] = f(inp["e_lru_bx"][0])
    m["e_lru_lambda"] = f(inp["e_lru_lambda"][0]); m["e_sink"] = f(inp["e_sink"][0])
    m["e_w_out"] = f(inp["e_w_out"][0]); m["e_b_out"] = f(inp["e_b_out"][0])
    c64, s64 = _rope_tables(S, 64)
    m["rope_e"] = f(np.stack([np.concatenate([c64, c64]), np.concatenate([s64, s64])]))
    j = np.arange(128)[:, None]; i = np.arange(128)[None, :]
    mp = (j >= i).astype(np.float32); mn = (j <= i).astype(np.float32)
    m["swa_mask"] = f(np.stack([np.tile(mp, (1, 4)), np.tile(mn, (1, 4))]))
    ow = np.asarray(inp["o_w_in"][0]); ob = np.asarray(inp["o_b_in"][0])
    m["o_w_in"] = f(ow); m["o_b_in"] = f(ob)
    m["o_q_norm"] = f(inp["o_q_norm"][0]); m["o_kv_norm"] = f(inp["o_kv_norm"][0])
    wuq = np.asarray(inp["o_w_uq"][0]).reshape(256, 8, 96)
    p32 = _rot_perm(1, 32)
    ext = np.zeros((256, 8, 192), np.float32)
    ext[:, :, 0:96] = wuq
    ext[:, :, 160:192] = wuq[:, :, 64:96][:, :, p32]
    m["o_w_uq"] = f(ext)
    m["o_w_uk"] = f(inp["o_w_uk"][0]); m["o_w_uv"] = f(inp["o_w_uv"][0])
    m["o_w_kpe_rot"] = f(ow[:, 384:416][:, p32]); m["o_b_kpe_rot"] = f(ob[384:416][p32])
    c32, s32 = _rope_tables(S, 32)
    m["rope_k"] = f(np.stack([c32, s32]))
    SQ = S // 2
    SH = SQ + 128
    tok = h * SQ - 64 + np.arange(SH)
    inside = (tok >= 0) & (tok < S)
    cq, sq_ = _rope_tables(S, 32, np.clip(tok, 0, S - 1))
    m["rope_q"] = f(np.stack([np.concatenate([np.ones((64, SH), np.float32), cq]), np.concatenate([np.zeros((64, SH), np.float32), sq_])]))
    m["zmask"] = f(inside.astype(np.float32))
    m["xh_idx"] = (64 + h * SQ + np.arange(SH)).astype(np.int32)
    m["o_dw_w"] = f(inp["o_dw_w"][0]); m["o_dw_b"] = f(inp["o_dw_b"][0])
    m["o_cln_g"] = f(inp["o_cln_g"][0]); m["o_cln_b"] = f(inp["o_cln_b"][0])
    m["o_w_out"] = f(inp["o_w_out"][0]); m["o_b_out"] = f(inp["o_b_out"][0])
    m["moe_w_gr"] = f(np.concatenate([inp["moe_w_group"], inp["moe_w_router"]], axis=2))
    m["moe_b_gr"] = f(np.concatenate([inp["moe_b_group"], inp["moe_b_router"]], axis=1))
    m["moe_w1"] = f(inp["moe_w1"]); m["moe_w3"] = f(inp["moe_w3"]); m["moe_w2"] = f(inp["moe_w2"])
    return m


_CACHE = {}


def kernel(**inputs):
    B, S, _ = inputs["x"].shape
    C = inputs["ctx"].shape[1]
    key = (S, C)
    if key not in _CACHE:
        _CACHE[key] = build(S, C)
    kk = _CACHE[key]
    shared = None
    in_maps = []
    for core in range(8):
        b, h = core % B, core // B
        m = prep_core_inputs(inputs, b, S, h)
        in_maps.append(m)
    res = run_bass_kernel_spmd(kk.nc, in_maps, core_ids=list(range(8)))
    SQ = S // 2
    out = np.empty((B, S, D), np.float32)
    for core in range(8):
        b, h = core % B, core // B
        out[b, h * SQ:(h + 1) * SQ] = np.asarray(res.results[core]["out"], dtype=np.float32)
    return out
```
